# Optimizing a Trainium2 kernel written in Bass

```python
import jax, jax.numpy as jnp
from jax import lax
import numpy as np

D_MODEL = 2048
BATCH = 32
SEQ = 256
DEPTH = 2
DEC_BATCH = 2
DEC_SEQ = 4096
PAST_LEN = 512

GRID_W = 64
N_HEADS = 8
HEAD_DK = D_MODEL // (2 * N_HEADS)
HEAD_DV = D_MODEL // N_HEADS
QK_DIM = N_HEADS * HEAD_DK
PROJ_DIM = 2 * QK_DIM + 2 * D_MODEL + 4 * N_HEADS
CHUNK = 64
GATE_CAP = 15.0
POOL_WINDOWS = (2, 4, 8, 16)
POOL_GROUPS = 4
POOL_GC = D_MODEL // POOL_GROUPS
D_FF = 256 * ((8 * D_MODEL // 3 + 255) // 256)
N_EXPERTS = 8
TOP_K = 2
N_MLSTM = (DEPTH + 1) // 2
N_POOL = DEPTH // 2
EPS = 1e-6

kernel_name = 'hybrid_mlstm_pool_diffusion_step'


def _rms(x, g):
    xf = x.astype(jnp.float32)
    y = xf * lax.rsqrt(jnp.mean(xf * xf, axis=-1, keepdims=True) + EPS)
    return (y * g.astype(jnp.float32)).astype(x.dtype)


def _ada(cvec, w, b):
    m = jnp.matmul(jax.nn.silu(cvec), w) + b
    m = m.reshape(m.shape[0], 6, 1, D_MODEL)
    return [m[:, i] for i in range(6)]


def _soft_cap(x):
    return GATE_CAP * jnp.tanh(x / GATE_CAP)


def _mlstm_scan(q, k, v, i_log, f_log, C0, n0, m0):
    B, T, H, _ = q.shape
    nc = T // CHUNK

    def chunks(a):
        a = a.reshape((B, nc, CHUNK, H) + a.shape[3:])
        return jnp.swapaxes(jnp.swapaxes(a, 0, 1), 2, 3)

    xs = (chunks(q.astype(jnp.float32)), chunks(k.astype(jnp.float32)), chunks(v.astype(jnp.float32)),
          chunks(i_log.astype(jnp.float32)), chunks(f_log.astype(jnp.float32)))
    tril = jnp.tril(jnp.ones((CHUNK, CHUNK), dtype=bool))

    def step(carry, inp):
        C, n, m = carry
        qc, kc, vc, ic, fc = inp
        b = jnp.cumsum(fc, axis=-1)
        a = b + m[..., None]
        dmat = jnp.where(tril, b[..., :, None] - b[..., None, :] + ic[..., None, :], -jnp.inf)
        m_row = jnp.maximum(a, jnp.max(dmat, axis=-1))
        w_inter = jnp.exp(a - m_row)
        s = jnp.einsum('bhtk,bhsk->bhts', qc, kc) * jnp.exp(dmat - m_row[..., None])
        num = jnp.einsum('bhts,bhsv->bhtv', s, vc) + w_inter[..., None] * jnp.einsum('bhtk,bhkv->bhtv', qc, C)
        den = jnp.sum(s, axis=-1) + w_inter * jnp.einsum('bhtk,bhk->bht', qc, n)
        h = num / jnp.maximum(jnp.abs(den), jnp.exp(-m_row))[..., None]
        b_last = b[..., -1]
        g = b_last[..., None] - b + ic
        m_new = jnp.maximum(b_last + m, jnp.max(g, axis=-1))
        wk = jnp.exp(g - m_new[..., None])
        decay = jnp.exp(b_last + m - m_new)
        C_new = decay[..., None, None] * C + jnp.einsum('bhs,bhsk,bhsv->bhkv', wk, kc, vc)
        n_new = decay[..., None] * n + jnp.einsum('bhs,bhsk->bhk', wk, kc)
        return (C_new, n_new, m_new), h

    (C, n, m), hs = lax.scan(step, (C0.astype(jnp.float32), n0.astype(jnp.float32), m0.astype(jnp.float32)), xs)
    h = jnp.swapaxes(jnp.swapaxes(hs, 2, 3), 0, 1).reshape(B, T, H, v.shape[-1])
    return h, C, n, m


def _mlstm_mixer(h, C0, n0, m0, w_in, b_gates, head_norm, w_out):
    B, T, _ = h.shape
    proj = jnp.matmul(h, w_in)
    q = proj[..., :QK_DIM].reshape(B, T, N_HEADS, HEAD_DK)
    k = proj[..., QK_DIM:2 * QK_DIM].reshape(B, T, N_HEADS, HEAD_DK) * (HEAD_DK ** -0.5)
    v = proj[..., 2 * QK_DIM:2 * QK_DIM + D_MODEL].reshape(B, T, N_HEADS, HEAD_DV)
    o = proj[..., 2 * QK_DIM + D_MODEL:2 * QK_DIM + 2 * D_MODEL]
    g = (proj[..., 2 * QK_DIM + 2 * D_MODEL:].astype(jnp.float32) + b_gates.astype(jnp.float32))
    g = g.reshape(B, T, 2, 2, N_HEADS)
    i_log = _soft_cap(g[:, :, :, 0])
    f_log = jax.nn.log_sigmoid(_soft_cap(g[:, :, :, 1]))
    h_f, Cf, nf, mf = _mlstm_scan(q, k, v, i_log[:, :, 0], f_log[:, :, 0], C0[:, 0], n0[:, 0], m0[:, 0])
    rev = lambda a: jnp.flip(a, axis=1)
    h_b, Cb, nb, mb = _mlstm_scan(rev(q), rev(k), rev(v), rev(i_log[:, :, 1]), rev(f_log[:, :, 1]),
                                  C0[:, 1], n0[:, 1], m0[:, 1])
    hsum = h_f + rev(h_b)
    hn = hsum * lax.rsqrt(jnp.mean(hsum * hsum, axis=-1, keepdims=True) + EPS)
    hn = hn.reshape(B, T, D_MODEL) * head_norm.astype(jnp.float32)
    y = (jax.nn.sigmoid(o.astype(jnp.float32)) * hn).astype(h.dtype)
    return (jnp.matmul(y, w_out), jnp.stack([Cf, Cb], axis=1), jnp.stack([nf, nb], axis=1),
            jnp.stack([mf, mb], axis=1))


def _box_mean(x, w, axis):
    T = x.shape[axis]
    pad = [(0, 0)] * x.ndim
    pad[axis] = (1, 0)
    cs = jnp.pad(jnp.cumsum(x, axis=axis), pad)
    idx = jnp.arange(T)
    lo = jnp.clip(idx - w // 2, 0, T)
    hi = jnp.clip(idx + w // 2, 0, T)
    s = jnp.take(cs, hi, axis=axis) - jnp.take(cs, lo, axis=axis)
    shape = [1] * x.ndim
    shape[axis] = T
    return s / (hi - lo).astype(x.dtype).reshape(shape)


def _pool_mixer(h, w_pool, scale, on_grid):
    B, T, _ = h.shape
    hg = h.reshape(B, T, POOL_GROUPS, POOL_GC)
    outs = []
    for gi, w in enumerate(POOL_WINDOWS):
        xg = hg[:, :, gi].astype(jnp.float32)
        if on_grid:
            rows = T // GRID_W
            xq = xg.reshape(B, rows, GRID_W, POOL_GC)
            pooled = _box_mean(_box_mean(xq, w, 1), w, 2).reshape(B, T, POOL_GC)
        else:
            pooled = _box_mean(xg, w, 1)
        outs.append(pooled - xg)
    d = jnp.stack(outs, axis=2).astype(h.dtype)
    y = jnp.einsum('btgc,gcd->btgd', d, w_pool).reshape(B, T, D_MODEL)
    return y * scale


def _swiglu(x, w1, w3, w2):
    return jnp.matmul(jax.nn.silu(jnp.matmul(x, w1)) * jnp.matmul(x, w3), w2)


def _moe(x, router, w1, w3, w2):
    logits = jnp.einsum('btd,de->bte', x, router).astype(jnp.float32)
    top_v, top_i = lax.top_k(logits, TOP_K)
    gates = jax.nn.softmax(top_v, axis=-1)
    combine = jnp.sum(jax.nn.one_hot(top_i, N_EXPERTS, dtype=jnp.float32) * gates[..., None], axis=-2)
    y = jnp.zeros_like(x)
    for e in range(N_EXPERTS):
        y = y + combine[..., e:e + 1].astype(x.dtype) * _swiglu(x, w1[e], w3[e], w2[e])
    return y


def setup_inputs(seed: int = 0) -> dict:
    key = jax.random.key(seed)
    ks = jax.random.split(key, 32)
    f32 = jnp.float32
    D = D_MODEL

    def nrm(k, shape, s):
        return s * jax.random.normal(k, shape, f32)

    gate_base = jnp.zeros((2, 2, N_HEADS), f32).at[:, 1].set(3.0).reshape(4 * N_HEADS)
    return {
        'x_prompt': nrm(ks[0], (BATCH, SEQ, D), 1.0),
        'x_sample': nrm(ks[1], (DEC_BATCH, DEC_SEQ, D), 1.0),
        'state_mlstm_C': nrm(ks[2], (DEC_BATCH, N_MLSTM, 2, N_HEADS, HEAD_DK, HEAD_DV), 0.1),
        'state_mlstm_n': nrm(ks[3], (DEC_BATCH, N_MLSTM, 2, N_HEADS, HEAD_DK), 0.1),
        'state_mlstm_m': nrm(ks[4], (DEC_BATCH, N_MLSTM, 2, N_HEADS), 1.0),
        'c': nrm(ks[5], (DEC_BATCH, D), 1.0),
        'c_ctx': nrm(ks[6], (D,), 1.0),
        'w_ada': nrm(ks[7], (DEPTH, D, 6 * D), 0.5 * D ** -0.5),
        'b_ada': nrm(ks[8], (DEPTH, 6 * D), 0.02),
        'norm_mix': 1.0 + nrm(ks[9], (DEPTH, D), 0.02),
        'norm_ffn': 1.0 + nrm(ks[10], (DEPTH, D), 0.02),
        'norm_final': 1.0 + nrm(ks[11], (D,), 0.02),
        'mlstm_w_in': nrm(ks[12], (N_MLSTM, D, PROJ_DIM), D ** -0.5),
        'mlstm_b_gates': gate_base + nrm(ks[13], (N_MLSTM, 4 * N_HEADS), 0.1),
        'mlstm_head_norm': 1.0 + nrm(ks[14], (N_MLSTM, D), 0.02),
        'mlstm_w_out': nrm(ks[15], (N_MLSTM, D, D), D ** -0.5),
        'pool_w': nrm(ks[16], (N_POOL, POOL_GROUPS, POOL_GC, POOL_GC), POOL_GC ** -0.5),
        'pool_scale': 1.0 + nrm(ks[17], (N_POOL, D), 0.1),
        'ffn_w1': nrm(ks[18], (N_MLSTM, D, D_FF), D ** -0.5),
        'ffn_w3': nrm(ks[19], (N_MLSTM, D, D_FF), D ** -0.5),
        'ffn_w2': nrm(ks[20], (N_MLSTM, D_FF, D), D_FF ** -0.5),
        'moe_router': nrm(ks[21], (N_POOL, D, N_EXPERTS), D ** -0.5),
        'moe_w1': nrm(ks[22], (N_POOL, N_EXPERTS, D, D_FF), D ** -0.5),
        'moe_w3': nrm(ks[23], (N_POOL, N_EXPERTS, D, D_FF), D ** -0.5),
        'moe_w2': nrm(ks[24], (N_POOL, N_EXPERTS, D_FF, D), D_FF ** -0.5),
    }


def reference(x_prompt, x_sample, state_mlstm_C, state_mlstm_n, state_mlstm_m, c, c_ctx,
              w_ada, b_ada, norm_mix, norm_ffn, norm_final,
              mlstm_w_in, mlstm_b_gates, mlstm_head_norm, mlstm_w_out,
              pool_w, pool_scale, ffn_w1, ffn_w3, ffn_w2,
              moe_router, moe_w1, moe_w3, moe_w2):
    xp, xs = x_prompt, x_sample
    bp = xp.shape[0]
    new_C, new_n, new_m = [], [], []
    for layer in range(DEPTH):
        j = layer // 2
        mp = _ada(c_ctx[None, :], w_ada[layer], b_ada[layer])
        ms = _ada(c, w_ada[layer], b_ada[layer])
        hp = _rms(xp, norm_mix[layer]) * (1.0 + mp[1]) + mp[0]
        hs = _rms(xs, norm_mix[layer]) * (1.0 + ms[1]) + ms[0]
        if layer % 2 == 0:
            zC = jnp.zeros((bp, 2, N_HEADS, HEAD_DK, HEAD_DV), jnp.float32)
            zn = jnp.zeros((bp, 2, N_HEADS, HEAD_DK), jnp.float32)
            zm = jnp.zeros((bp, 2, N_HEADS), jnp.float32)
            yp, Cp, n_p, m_p = _mlstm_mixer(hp, zC, zn, zm, mlstm_w_in[j], mlstm_b_gates[j],
                                            mlstm_head_norm[j], mlstm_w_out[j])
            ys, _, _, _ = _mlstm_mixer(hs, state_mlstm_C[:, j], state_mlstm_n[:, j], state_mlstm_m[:, j],
                                       mlstm_w_in[j], mlstm_b_gates[j], mlstm_head_norm[j], mlstm_w_out[j])
            new_C.append(Cp)
            new_n.append(n_p)
            new_m.append(m_p)
        else:
            yp = _pool_mixer(hp, pool_w[j], pool_scale[j], False)
            ys = _pool_mixer(hs, pool_w[j], pool_scale[j], True)
        xp = xp + mp[2] * yp
        xs = xs + ms[2] * ys
        hp = _rms(xp, norm_ffn[layer]) * (1.0 + mp[4]) + mp[3]
        hs = _rms(xs, norm_ffn[layer]) * (1.0 + ms[4]) + ms[3]
        if layer % 2 == 0:
            fp = _swiglu(hp, ffn_w1[j], ffn_w3[j], ffn_w2[j])
            fs = _swiglu(hs, ffn_w1[j], ffn_w3[j], ffn_w2[j])
        else:
            fp = _moe(hp, moe_router[j], moe_w1[j], moe_w3[j], moe_w2[j])
            fs = _moe(hs, moe_router[j], moe_w1[j], moe_w3[j], moe_w2[j])
        xp = xp + mp[5] * fp
        xs = xs + ms[5] * fs
    y_prompt = _rms(xp, norm_final)
    y_sample = _rms(xs, norm_final)
    state_C_out = jnp.stack(new_C, axis=1)
    state_n_out = jnp.stack(new_n, axis=1)
    state_m_out = jnp.stack(new_m, axis=1)
    return (y_prompt, y_sample, state_C_out, state_n_out, state_m_out)
```

```python
import numpy as np
from contextlib import ExitStack
import concourse.bass as bass
import concourse.mybir as mybir
from concourse.bass_utils import run_bass_kernel_spmd

F32 = mybir.dt.float32
BF16 = mybir.dt.bfloat16
I32 = mybir.dt.int32
AF = mybir.ActivationFunctionType
ALU = mybir.AluOpType

D = 2048
KD = 16
DFF = 5632
NFC = 44
NH = 8
NP = 1024
NS = 4096
T0 = NP + NS
T1 = 2048
EPS = 1e-6
NEG = -30000.0
ENG = ('pe', 'act', 'dve', 'pool', 'sp')


class Sched:
    def __init__(self, nc, es):
        self.nc = nc
        self.es = es
        self.eobj = {'pe': nc.tensor, 'act': nc.scalar, 'dve': nc.vector, 'pool': nc.gpsimd, 'sp': nc.sync}
        self.ops = {e: [] for e in ENG}
        self.cnt = {e: 0 for e in ENG}
        self.dcnt = {}
        self.seen = {e: {} for e in ENG}
        self.lastw = {}
        self.readers = {}
        self.sems = {}
        self.pend = {e: ([], []) for e in ENG}
        self.nsb = 0
        self.capture = None

    def sem(self, name):
        if name not in self.sems:
            self.sems[name] = self.es.enter_context(self.nc.semaphore(name))
        return self.sems[name]

    def op(self, eng, fn, reads=(), writes=(), dma=None, inc=True):
        if self.capture is not None:
            self.capture.append((eng, fn, tuple(reads), tuple(writes), dma, inc))
            return None
        waits = {}

        def need(tok):
            s, v = tok
            if waits.get(s, 0) < v:
                waits[s] = v
        for b in reads:
            if b in self.lastw:
                need(self.lastw[b])
        for b in writes:
            if b in self.lastw:
                need(self.lastw[b])
            for r in self.readers.get(b, ()):
                need(r)
        wl = []
        for s, v in waits.items():
            if s in self.dcnt:
                v = 16 * self.dcnt[s]
            if self.seen[eng].get(s, 0) < v:
                self.seen[eng][s] = v
                wl.append((s, v))
        tok = None
        incspec = None
        if dma is not None:
            self.dcnt[dma] = self.dcnt.get(dma, 0) + 1
            tok = (dma, 16 * self.dcnt[dma])
            incspec = (dma, 16)
        elif inc:
            self.cnt[eng] += 1
            tok = ('E' + eng, self.cnt[eng])
            incspec = ('E' + eng, 1)
        self.ops[eng].append((wl, fn, incspec))
        pr, pw = self.pend[eng]
        if tok is None:
            pr.extend(reads)
            pw.extend(writes)
        else:
            if dma is None:
                allr = list(reads) + pr
                allw = list(writes) + pw
                self.pend[eng] = ([], [])
            else:
                allr, allw = reads, writes
            for b in allw:
                self.lastw[b] = tok
                self.readers[b] = []
            for b in allr:
                self.readers.setdefault(b, []).append(tok)
        return tok

    def interleave(self, fns):
        lists = []
        for f in fns:
            self.capture = []
            f()
            lists.append(self.capture)
        self.capture = None
        n = max(len(l) for l in lists) if lists else 0
        for i in range(n):
            for l in lists:
                if i < len(l):
                    self.op(*l[i])

    def flush(self, final=False):
        nc = self.nc
        for s in list(self.dcnt) + ['E' + e for e in ENG]:
            self.sem(s)
        tails = {}
        for e in ENG:
            tl = []
            for e2 in ENG:
                if e2 != e and self.cnt[e2] > self.seen[e].get('E' + e2, 0):
                    tl.append(('E' + e2, self.cnt[e2]))
                    self.seen[e]['E' + e2] = self.cnt[e2]
            for s, c in self.dcnt.items():
                if 16 * c > self.seen[e].get(s, 0):
                    tl.append((s, 16 * c))
                    self.seen[e][s] = 16 * c
            tails[e] = tl
        ops = self.ops
        sems = self.sems

        def run(e, name):
            for wl, fn, incspec in ops[name]:
                for s, v in wl:
                    e.wait_ge(sems[s], v)
                ins = fn(e)
                if incspec is not None:
                    ins.then_inc(sems[incspec[0]], incspec[1])
            for s, v in tails[name]:
                e.wait_ge(sems[s], v)
        with nc.Block() as block:
            @block.tensor
            def _(e):
                run(e, 'pe')

            @block.scalar
            def _(e):
                run(e, 'act')

            @block.vector
            def _(e):
                run(e, 'dve')

            @block.gpsimd
            def _(e):
                run(e, 'pool')

            @block.sync
            def _(e):
                run(e, 'sp')
        self.ops = {e: [] for e in ENG}

    def sb(self, st, shape, dt, name=None):
        self.nsb += 1
        return st.enter_context(self.nc.sbuf_tensor(f"{name or 't'}_{self.nsb}", list(shape), dt))

    def ps(self, st, shape, dt, name=None):
        self.nsb += 1
        return st.enter_context(self.nc.psum_tensor(f"{name or 'p'}_{self.nsb}", list(shape), dt))

    def dma(self, q, out, in_, reads, writes, sem, slow=False):
        if slow:
            return self.op(q, lambda e, o=out, i=in_: e.dma_start(out=o, in_=i, allow_slow_non_contiguous=True),
                           reads, writes, dma=sem)
        return self.op(q, lambda e, o=out, i=in_: e.dma_start(out=o, in_=i), reads, writes, dma=sem)

    def mm(self, out, lhsT, rhs, start, stop, reads, writes):
        return self.op('pe', lambda e, o=out, l=lhsT, r=rhs, a=start, b=stop: e.matmul(o, l, r, start=a, stop=b),
                       reads, writes, inc=stop)

    def tr(self, out, in_, ident, reads, writes, inc=True):
        return self.op('pe', lambda e, o=out, i=in_, d=ident: e.transpose(o, i, d), reads, writes, inc=inc)

    def act(self, out, in_, func, reads, writes, bias=None, scale=None, accum=None, eng='act'):
        def f(e, o=out, i=in_, fu=func, b=bias, s=scale, a=accum):
            kw = {}
            if b is not None:
                kw['bias'] = b
            if s is not None:
                kw['scale'] = s
            if a is not None:
                kw['accum_out'] = a
            return e.activation(o, i, fu, **kw)
        return self.op('act', f, reads, writes)

    def tt(self, eng, out, in0, in1, op, reads, writes):
        return self.op(eng, lambda e, o=out, a=in0, b=in1, p=op: e.tensor_tensor(out=o, in0=a, in1=b, op=p), reads, writes)

    def ts(self, eng, out, in0, s1, op0, reads, writes, s2=None, op1=None):
        def f(e, o=out, a=in0, x1=s1, x2=s2, p0=op0, p1=op1):
            if p1 is None:
                return e.tensor_scalar(out=o, in0=a, scalar1=x1, scalar2=None, op0=p0)
            return e.tensor_scalar(out=o, in0=a, scalar1=x1, scalar2=x2, op0=p0, op1=p1)
        return self.op(eng, f, reads, writes)

    def stt(self, out, in0, scalar, in1, op0, op1, reads, writes):
        return self.op('dve', lambda e, o=out, a=in0, s=scalar, b=in1, p0=op0, p1=op1:
                       e.scalar_tensor_tensor(out=o, in0=a, scalar=s, in1=b, op0=p0, op1=p1), reads, writes)

    def copy(self, eng, out, in_, reads, writes):
        if eng == 'act':
            return self.op('act', lambda e, o=out, i=in_: e.copy(o, i), reads, writes)
        return self.op(eng, lambda e, o=out, i=in_: e.tensor_copy(out=o, in_=i), reads, writes)

    def memset(self, eng, ap, val, writes):
        return self.op(eng, lambda e, a=ap, v=val: e.memset(a, v), (), writes)


def build_program(dbg=False):
    nc = bass.Bass("TRN2", target_bir_lowering=False)
    es = ExitStack()
    S = Sched(nc, es)

    def din(name, shape, dt=F32):
        return nc.dram_tensor(name, list(shape), dt, kind="ExternalInput").ap()

    def dout(name, shape, dt=F32):
        return nc.dram_tensor(name, list(shape), dt, kind="ExternalOutput").ap()

    def dscr(name, shape, dt=F32):
        kind = "ExternalOutput" if (dbg and name in DBG_OUT) else "Internal"
        return nc.dram_tensor(name, list(shape), dt, kind=kind).ap()

    xp = din("xp", [NP, D])
    xs = din("xs", [NS, D])
    sC = din("sC", [16, 128, 256])
    snl = din("snl", [128, 16])
    sml = din("sml", [2, 8, 1])
    c2 = din("c2", [128, KD, 2])
    w_ada = din("w_ada", [2, D, 6 * D])
    b_ada = din("b_ada", [2, 6 * D])
    norm_mix = din("norm_mix", [2, D])
    norm_ffn = din("norm_ffn", [2, D])
    norm_final = din("norm_final", [1, D])
    w_in = din("w_in", [D, 6176])
    bgl = din("bgl", [8, 4])
    head_norm = din("head_norm", [1, D])
    w_out = din("w_out", [D, D])
    pool_w = din("pool_w", [4, 512, 512])
    pool_scale = din("pool_scale", [1, D])
    ffn_w1 = din("ffn_w1", [D, DFF])
    ffn_w3 = din("ffn_w3", [D, DFF])
    ffn_w2 = din("ffn_w2", [DFF, D])
    router = din("router", [D, 8])
    moe_w1 = din("moe_w1", [8, D, DFF])
    moe_w3 = din("moe_w3", [8, D, DFF])
    moe_w2 = din("moe_w2", [8, DFF, D])
    identf_d = din("identf", [128, 128])
    cmask_d = din("cmask", [2, 64, 512])
    bmask_d = din("bmask", [8, 512])
    idx_win = din("idx_win", [16, 128, 1], I32)
    idx_xs = din("idx_xs", [16, 128, 1], I32)
    pm_s = din("pm_s", [160, 128, 128])
    ic_s = din("ic_s", [32, 128])
    pm_p = din("pm_p", [16, 128, 128])
    ic_p = din("ic_p", [8, 128])

    yp = dout("yp", [NP, D])
    ys = dout("ys", [1024, D])
    nC = dout("nC", [4, 16, 128, 256])
    nn = dout("nn", [4, 16, 128])
    nm = dout("nm", [4, 2, 8])

    mod = dscr("mod", [2, 2, 6 * D])
    qT_d = dscr("qT_d", [NH, 128, T0], BF16)
    kT_d = dscr("kT_d", [NH, 128, T0], BF16)
    k_d = dscr("k_d", [T0, 1024], BF16)
    v_d = dscr("v_d", [T0, D], BF16)
    og_d = dscr("og_d", [T0, D])
    gi_d = dscr("gi_d", [4, 8, T0])
    hdl = [dscr("hd0", [T0, D]), dscr("hd1", [T0, D])]
    TW = NP + 2048
    x1 = dscr("x1", [TW, D])
    x2 = dscr("x2", [TW, D])
    x3 = dscr("x3", [T1, D])

    identf = S.sb(es, [128, 128], F32, "identf")
    identb = S.sb(es, [128, 128], BF16, "identb")
    S.dma('sp', identf[:], identf_d[:, :], [], ['identf'], 'dconst')
    S.copy('dve', identb[:], identf[:], ['identf'], ['identb'])

    rowsel = {0: 0, 1: 1}

    def load_bc(tile_ap, src_row_ap, key, sem='dmod'):
        S.dma('sp', tile_ap, src_row_ap.partition_broadcast(128), [], [key], sem)

    with ExitStack() as st:
        c2t = S.sb(st, [128, KD, 2], F32)
        c2s = S.sb(st, [128, KD, 2], BF16)
        S.dma('sp', c2t[:], c2[:, :, :], [], ['c2t'], 'dconst')
        S.act(c2s[:], c2t[:], AF.Silu, ['c2t'], ['c2s'])
        wa = [S.sb(st, [128, KD, 512], BF16, f"wa{i}") for i in range(2)]
        bt = [S.sb(st, [2, 512], F32, f"bt{i}") for i in range(2)]
        mo = [S.sb(st, [2, 512], F32, f"mo{i}") for i in range(2)]
        pa = [S.ps(st, [128, 512], F32, f"pa{i}") for i in range(2)]
        it = 0
        for l in range(2):
            wl = w_ada[l].rearrange("(k p) n -> p k n", p=128)
            for nci in range(24):
                b = it % 2
                it += 1
                n0 = nci * 512
                S.dma('pool', wa[b][:], wl[:, :, n0:n0 + 512], [], [f'wa{b}'], f'dwa{b}')
                S.dma('sp', bt[b][:], b_ada[l:l + 1, n0:n0 + 512].partition_broadcast(2), [], [f'bt{b}'], f'dbt{b}')
                for k in range(KD):
                    S.mm(pa[b][0:2, :], c2s[:, k, :], wa[b][:, k, :], k == 0, k == KD - 1,
                         ['c2s', f'wa{b}'], [f'pa{b}'])
                S.tt('dve', mo[b][:], pa[b][0:2, :], bt[b][:], ALU.add, [f'pa{b}', f'bt{b}'], [f'mo{b}'])
                S.dma('sp', mod[l, :, n0:n0 + 512], mo[b][:], [f'mo{b}'], ['mod'], 'dmodw')
        S.flush()

    def make_GS(st, l, which, gvec_ap):
        base = 0 if which == 0 else 3
        gt = S.sb(st, [128, D], F32, "gt")
        load_bc(gt[:], gvec_ap, 'gt')
        G, Sh = [], []
        for row in range(2):
            g_ = S.sb(st, [128, D], F32, f"G{row}")
            s_ = S.sb(st, [128, D], F32, f"S{row}")
            load_bc(g_[:], mod[l, row:row + 1, (base + 1) * D:(base + 2) * D], f'G{row}')
            load_bc(s_[:], mod[l, row:row + 1, base * D:(base + 1) * D], f'S{row}')
            S.stt(g_[:], g_[:], 1.0, gt[:], ALU.add, ALU.mult, [f'G{row}', 'gt'], [f'G{row}'])
            G.append(g_)
            Sh.append(s_)
        return G, Sh

    class NormBufs:
        def __init__(self, st):
            self.xin = [S.sb(st, [128, D], F32, f"xin{i}") for i in range(2)]
            self.junk = S.sb(st, [128, D], BF16, "junk")
            self.ss = [S.sb(st, [128, 1], F32, f"ss{i}") for i in range(2)]
            self.tmp = [S.sb(st, [128, D], F32, f"ntmp{i}") for i in range(2)]
            self.h = [S.sb(st, [128, D], BF16, f"nh{i}") for i in range(2)]
            self.ptr = [S.ps(st, [128, 1024], BF16, f"ptr{i}") for i in range(2)]
            self.n = 0

    def norm_tile(nb, x_src_ap, G, Sh, hT_dst, col0, loaded=False, xkey=None, keep32=False):
        b = nb.n % 2
        nb.n += 1
        if not loaded:
            S.dma('sp', nb.xin[b][:], x_src_ap, [xkey] if xkey else [], [f'xin{b}'], f'dxin{b}')
        S.act(nb.junk[:], nb.xin[b][:], AF.Square, [f'xin{b}'], ['junk', f'ss{b}'], accum=nb.ss[b][:])
        S.ts('dve', nb.ss[b][:], nb.ss[b][:], 1.0 / D, ALU.mult, [f'ss{b}'], [f'ss{b}'], s2=EPS, op1=ALU.add)
        S.act(nb.ss[b][:], nb.ss[b][:], AF.Sqrt, [f'ss{b}'], [f'ss{b}'])
        S.op('dve', lambda e, o=nb.ss[b][:]: e.reciprocal(out=o, in_=o), [f'ss{b}'], [f'ss{b}'])
        S.stt(nb.tmp[b][:], nb.xin[b][:], nb.ss[b][:, 0:1], G[:], ALU.mult, ALU.mult,
              [f'xin{b}', f'ss{b}', 'G0', 'G1'], [f'ntmp{b}'])
        if keep32:
            S.tt('pool', nb.tmp[b][:], nb.tmp[b][:], Sh[:], ALU.add, [f'ntmp{b}', 'S0', 'S1'], [f'ntmp{b}'])
            S.copy('pool', nb.h[b][:], nb.tmp[b][:], [f'ntmp{b}'], [f'nh{b}'])
        else:
            S.tt('pool', nb.h[b][:], nb.tmp[b][:], Sh[:], ALU.add, [f'ntmp{b}', 'S0', 'S1'], [f'nh{b}'])
        if hT_dst is not None:
            ht, hkey = hT_dst
            for half in range(2):
                for kk in range(8):
                    k = half * 8 + kk
                    S.tr(nb.ptr[half][:, kk * 128:(kk + 1) * 128], nb.h[b][:, k * 128:(k + 1) * 128], identb[:],
                         [f'nh{b}', 'identb'], [f'ptr{half}'], inc=(kk == 7))
                S.copy('act' if half == 0 else 'dve',
                       ht[:, half * 8:(half + 1) * 8, col0:col0 + 128],
                       nb.ptr[half][:].rearrange("p (k t) -> p k t", k=8), [f'ptr{half}'], [hkey])
        return b

    def x0_rows(t0, n):
        if t0 < NP:
            return xp[t0:t0 + n, :]
        return xs[t0 - NP:t0 - NP + n, :]

    with ExitStack() as st:
        G, Sh = make_GS(st, 0, 0, norm_mix[0:1, :])
        nb = NormBufs(st)
        hT = S.sb(st, [128, KD, 1024], BF16, "hT")
        wc = [S.sb(st, [128, KD, 512], BF16, f"wc{i}") for i in range(2)]
        ev = [S.sb(st, [128, 512], BF16, f"ev{i}") for i in range(2)]
        evf = [S.sb(st, [128, 512], F32, f"evf{i}") for i in range(2)]
        pm = [S.ps(st, [128, 512], F32, f"pm{i}") for i in range(2)]
        bg = S.sb(st, [8, 4], F32, "bg")
        gtmp = S.sb(st, [8, 512], F32, "gtmp")
        S.dma('sp', bg[:], bgl[:, :], [], ['bg'], 'dconst')
        S.ts('dve', bg[:], bg[:], 1.0 / 15.0, ALU.mult, ['bg'], ['bg'])
        w_in_r = w_in.rearrange("(k p) n -> p k n", p=128)
        wi = 0
        ei = 0
        for blk in range(T0 // 1024):
            row = 0 if blk == 0 else 1
            for tl in range(8):
                t0 = blk * 1024 + tl * 128
                norm_tile(nb, x0_rows(t0, 128), G[row], Sh[row], (hT, 'hT'), tl * 128)
            for cg in range(13):
                wb = wi % 2
                wi += 1
                ncol = 512 if cg < 12 else 32
                S.dma('pool', wc[wb][:, :, 0:ncol], w_in_r[:, :, cg * 512:cg * 512 + ncol], [], [f'wc{wb}'], f'dwc{wb}')
                if cg < 4:
                    for hh in range(4):
                        head = (cg % 2) * 4 + hh
                        for tb in range(2):
                            pb = ei % 2
                            ei += 1
                            for k in range(KD):
                                S.mm(pm[pb][:, :], wc[wb][:, k, hh * 128:(hh + 1) * 128], hT[:, k, tb * 512:(tb + 1) * 512],
                                     k == 0, k == KD - 1, [f'wc{wb}', 'hT'], [f'pm{pb}'])
                            dst = qT_d if cg < 2 else kT_d
                            if cg < 2:
                                S.copy('act', ev[pb][:], pm[pb][:], [f'pm{pb}'], [f'ev{pb}'])
                            else:
                                S.op('act', lambda e, o=ev[pb][:], i=pm[pb][:]: e.mul(o, i, 128.0 ** -0.5),
                                     [f'pm{pb}'], [f'ev{pb}'])
                            c0 = blk * 1024 + tb * 512
                            S.dma('sp', dst[head, :, c0:c0 + 512], ev[pb][:], [f'ev{pb}'], ['qk_d'], 'dqkw')
                if 2 <= cg < 12:
                    for tl in range(8):
                        pb = ei % 2
                        ei += 1
                        for k in range(KD):
                            S.mm(pm[pb][:, :], hT[:, k, tl * 128:(tl + 1) * 128], wc[wb][:, k, :],
                                 k == 0, k == KD - 1, [f'wc{wb}', 'hT'], [f'pm{pb}'])
                        r0 = blk * 1024 + tl * 128
                        if cg < 4:
                            S.op('act', lambda e, o=ev[pb][:], i=pm[pb][:]: e.mul(o, i, 128.0 ** -0.5),
                                 [f'pm{pb}'], [f'ev{pb}'])
                            S.dma('sp', k_d[r0:r0 + 128, (cg - 2) * 512:(cg - 1) * 512], ev[pb][:], [f'ev{pb}'], ['qk_d'], 'dqkw')
                        elif cg < 8:
                            S.copy('dve', ev[pb][:], pm[pb][:], [f'pm{pb}'], [f'ev{pb}'])
                            S.dma('sp', v_d[r0:r0 + 128, (cg - 4) * 512:(cg - 3) * 512], ev[pb][:], [f'ev{pb}'], ['qk_d'], 'dqkw')
                        else:
                            S.act(evf[pb][:], pm[pb][:], AF.Sigmoid, [f'pm{pb}'], [f'evf{pb}'])
                            S.dma('sp', og_d[r0:r0 + 128, (cg - 8) * 512:(cg - 7) * 512], evf[pb][:], [f'evf{pb}'], ['qk_d'], 'dqkw')
                if cg == 12:
                    for grp in range(4):
                        for tb in range(2):
                            pb = ei % 2
                            ei += 1
                            for k in range(KD):
                                S.mm(pm[pb][0:8, :], wc[wb][:, k, grp * 8:(grp + 1) * 8], hT[:, k, tb * 512:(tb + 1) * 512],
                                     k == 0, k == KD - 1, [f'wc{wb}', 'hT'], [f'pm{pb}'])
                            S.act(gtmp[:], pm[pb][0:8, :], AF.Tanh, [f'pm{pb}', 'bg'], ['gtmp'],
                                  bias=bg[:, grp:grp + 1], scale=1.0 / 15.0)
                            if grp % 2 == 0:
                                S.ts('dve', evf[pb][0:8, :], gtmp[:], 15.0, ALU.mult, ['gtmp'], [f'evf{pb}'])
                            else:
                                S.act(gtmp[:], gtmp[:], AF.Exp, ['gtmp'], ['gtmp'], scale=-15.0)
                                S.act(gtmp[:], gtmp[:], AF.Ln, ['gtmp'], ['gtmp'], bias=1.0)
                                S.ts('dve', evf[pb][0:8, :], gtmp[:], -1.0, ALU.mult, ['gtmp'], [f'evf{pb}'])
                            c0 = blk * 1024 + tb * 512
                            S.dma('sp', gi_d[grp, :, c0:c0 + 512], evf[pb][0:8, :], [f'evf{pb}'], ['qk_d'], 'dqkw')
        S.flush()


    if STOP_AFTER <= 2:
        return nc, es, S

    with ExitStack() as st:
        ones8 = S.sb(st, [8, 128], F32, "ones8")
        bmask = S.sb(st, [8, 512], F32, "bmask")
        S.memset('dve', ones8[:], 1.0, ['ones8'])
        onesb = S.sb(st, [128, 1], BF16, "onesb")
        S.memset('dve', onesb[:], 1.0, ['onesb'])
        S.dma('sp', bmask[:], bmask_d[:, :], [], ['bmask'], 'dconst')
        bmask3 = bmask[:].rearrange("p (h t) -> p h t", h=8)
        cm = []
        B = []
        for d in range(2):
            c_ = S.sb(st, [64, 512], F32, f"cm{d}")
            S.dma('sp', c_[:], cmask_d[d, :, :], [], [f'cm{d}'], 'dconst')
            cm.append(c_)
            b = {}
            b['I'] = S.sb(st, [8, 256], F32, f"I{d}")
            b['F'] = S.sb(st, [8, 256], F32, f"F{d}")
            b['qT'] = S.sb(st, [128, 8, 256], BF16, f"qT{d}")
            b['kT'] = S.sb(st, [128, 8, 256], BF16, f"kT{d}")
            for nm_ in ('b', 'c', 'M', 'nM', 'wk', 'wi', 'fl', 't'):
                b[nm_] = S.sb(st, [8, 64], F32, f"r{nm_}{d}")
            b['m'] = [S.sb(st, [8, 1], F32, f"m{i}{d}") for i in range(2)]
            b['mfin'] = [S.sb(st, [8, 1], F32, f"mfin{i}{d}") for i in range(2)]
            b['BD1'] = S.sb(st, [8, 512], F32, f"BD1{d}")
            b['BD2'] = S.sb(st, [8, 512], F32, f"BD2{d}")
            b['dg'] = S.sb(st, [8, 8], F32, f"dg{d}")
            b['E'] = S.sb(st, [64, 512], F32, f"E{d}")
            for nm_, shp, dt_ in (('v', [64, 8, 257], BF16), ('k', [64, 8, 128], BF16), ('tok', [64, 32], F32),
                                  ('S', [64, 512], BF16), ('qp', [128, 512], BF16), ('kw', [64, 8, 128], BF16),
                                  ('decb', [128, 8], F32), ('Hc', [64, 8, 256], F32)):
                b[nm_] = [S.sb(st, shp, dt_, f"{nm_}{i}{d}") for i in range(2)]
            b['Cn'] = S.sb(st, [128, 8, 256], F32, f"Cn{d}")
            b['nst'] = S.sb(st, [128, 8], F32, f"nst{d}")
            b['Cnb'] = [S.sb(st, [128, 8, 256], BF16, f"Cnb{i}{d}") for i in range(2)]
            b['nb'] = [S.sb(st, [128, 8], BF16, f"nb{i}{d}") for i in range(2)]
            b['dd'] = S.sb(st, [64, 8], F32, f"dd{d}")
            b['pA'] = S.ps(st, [128, 512], F32, f"pA{d}")
            b['pS'] = S.ps(st, [128, 512], F32, f"pS{d}")
            b['pN'] = [S.ps(st, [128, 512], F32, f"pN{i}{d}") for i in range(2)]
            b['ci'] = 0
            b['mi'] = 0
            for i in range(2):
                S.memset('pool', b['v'][i][:, :, 256:257], 1.0, [f'v{i}{d}'])
            B.append(b)

        def K_(n, d):
            return f'{n}{d}'

        def group_load(d, g0):
            b = B[d]
            S.dma('sp', b['I'][:], gi_d[2 * d, :, g0:g0 + 256], ['qk_d'], [K_('I', d)], K_('dgl', d))
            S.dma('sp', b['F'][:], gi_d[2 * d + 1, :, g0:g0 + 256], ['qk_d'], [K_('F', d)], K_('dgl', d))
            S.dma('sp', b['qT'][:], qT_d[:, :, g0:g0 + 256].rearrange("h p t -> p h t"), ['qk_d'], [K_('qT', d)], K_('dgl', d))
            S.dma('sp', b['kT'][:], kT_d[:, :, g0:g0 + 256].rearrange("h p t -> p h t"), ['qk_d'], [K_('kT', d)], K_('dgl', d))

        def pre(d, u, si):
            q, g0, jc, first, lastc, newgrp = u
            b = B[d]
            r0 = g0 + jc * 64
            cs = slice(jc * 64, jc * 64 + 64)
            last = 63 if d == 0 else 0
            k = lambda n: K_(n, d)
            ks = lambda n: f'{n}{si}{d}'

            def rv(ap):
                return ap[:, ::-1] if d == 1 else ap
            if first:
                if q < 4:
                    S.memset('pool', b['m'][b['mi']][:], 0.0, [k('m')])
                else:
                    S.dma('sp', b['m'][b['mi']][:], sml[d], [], [k('m')], K_('dst', d))
            if newgrp:
                group_load(d, g0)
            mcur = b['m'][b['mi']]
            mnew = b['m'][1 - b['mi']]
            S.dma('sp', b['v'][si][:, :, 0:256], v_d[r0:r0 + 64, :].rearrange("t (h v) -> t h v", h=8), ['qk_d'], [ks('v')], ks('dcl'))
            S.dma('sp', b['k'][si][:], k_d[r0:r0 + 64, :].rearrange("t (h v) -> t h v", h=8), ['qk_d'], [ks('k')], ks('dcl'))
            S.op('dve', lambda e, o=rv(b['b'][:]), a=ones8[:, 0:64], x=rv(b['F'][:, cs]):
                 e.tensor_tensor_scan(out=o, data0=a, data1=x, initial=0.0, op0=ALU.mult, op1=ALU.add),
                 ['ones8', k('F')], [k('b')])
            S.tt('dve', b['c'][:], b['I'][:, cs], b['b'][:], ALU.subtract, [k('I'), k('b')], [k('c')])
            S.op('dve', lambda e, o=rv(b['M'][:]), a=ones8[:, 0:64], x=rv(b['c'][:]), i=mcur[:, 0:1]:
                 e.tensor_tensor_scan(out=o, data0=a, data1=x, initial=i, op0=ALU.mult, op1=ALU.max),
                 ['ones8', k('c'), k('m')], [k('M')])
            S.ts('dve', b['nM'][:], b['M'][:], -1.0, ALU.mult, [k('M')], [k('nM')])
            S.act(b['wk'][:], b['c'][:], AF.Exp, [k('c'), k('nM')], [k('wk')], bias=b['nM'][:, last:last + 1])
            S.act(b['wi'][:], b['nM'][:], AF.Exp, [k('nM'), k('m')], [k('wi')], bias=mcur[:, 0:1])
            S.tt('dve', b['t'][:], b['nM'][:], b['b'][:], ALU.subtract, [k('nM'), k('b')], [k('t')])
            S.act(b['fl'][:], b['t'][:], AF.Exp, [k('t')], [k('fl')])
            S.tt('dve', mnew[:], b['b'][:, last:last + 1], b['M'][:, last:last + 1], ALU.add, [k('b'), k('M'), k('m')], [k('m')])
            if lastc and q < 4:
                S.copy('dve', b['mfin'][q % 2][:], mnew[:], [k('m')], [f'mfin{q % 2}{d}'])
            S.tt('dve', b['BD1'][:].rearrange("p (h t) -> p h t", h=8), bmask3,
                 b['nM'][:].unsqueeze(1).to_broadcast([8, 8, 64]), ALU.mult, ['bmask', k('nM')], [k('BD1')])
            S.tt('dve', b['BD2'][:].rearrange("p (h t) -> p h t", h=8), bmask3,
                 b['wi'][:].unsqueeze(1).to_broadcast([8, 8, 64]), ALU.mult, ['bmask', k('wi')], [k('BD2')])
            S.ts('dve', b['dg'][:], identf[0:8, 0:8], b['wi'][:, last:last + 1], ALU.mult, ['identf', k('wi')], [k('dg')])
            for j, nm_ in enumerate(('c', 'wk', 'wi', 'fl')):
                S.tr(b['pA'][0:64, j * 8:(j + 1) * 8], b[nm_][:], identf[0:8, 0:8], [k(nm_), 'identf'], [k('pA')], inc=False)
            S.mm(b['pA'][:, 32:40], ones8[:, :], b['dg'][:], True, True, ['ones8', k('dg')], [k('pA')])
            S.copy('act', b['tok'][si][:], b['pA'][0:64, 0:32], [k('pA')], [ks('tok')])
            S.copy('act', b['decb'][si][:], b['pA'][:, 32:40], [k('pA')], [ks('decb')])
            S.mm(b['pA'][0:64, :], ones8[:, 0:64], b['BD1'][:], True, False, ['ones8', k('BD1')], [k('pA')])
            S.mm(b['pA'][0:64, :], b['c'][:], bmask[:], False, False, [k('c'), 'bmask'], [k('pA')])
            S.mm(b['pA'][0:64, :], identf[0:64, 0:64], cm[d][:], False, True, ['identf', f'cm{d}'], [k('pA')])
            S.act(b['E'][:], b['pA'][0:64, :], AF.Exp, [k('pA')], [k('E')])
            for h in range(8):
                S.op('pe', lambda e, o=b['pA'][0:64, h * 64:(h + 1) * 64], l=b['kT'][:, h, cs], r=b['qT'][:, h, cs]:
                     e.matmul(o, l, r, start=True, stop=True), [k('kT'), k('qT')], [k('pA')], inc=(h == 7))
            S.tt('dve', b['S'][si][:], b['pA'][0:64, :], b['E'][:], ALU.mult, [k('pA'), k('E')], [ks('S')])
            S.mm(b['pA'][:, :], ones8[:, :], b['BD2'][:], True, True, ['ones8', k('BD2')], [k('pA')])
            S.tt('dve', b['qp'][si][:].rearrange("p (h t) -> p h t", h=8), b['qT'][:, :, cs],
                 b['pA'][:, :].rearrange("p (h t) -> p h t", h=8), ALU.mult, [k('qT'), k('pA')], [ks('qp')])
            S.tt('pool', b['kw'][si][:], b['k'][si][:], b['tok'][si][:, 8:16].unsqueeze(2).to_broadcast([64, 8, 128]), ALU.mult,
                 [ks('k'), ks('tok')], [ks('kw')])
            b['mi'] = 1 - b['mi']

        def post(d, u, si):
            q, g0, jc, first, lastc, newgrp = u
            b = B[d]
            r0 = g0 + jc * 64
            k = lambda n: K_(n, d)
            ks = lambda n: f'{n}{si}{d}'
            jo = b['ci']
            jn = 1 - jo
            ko = lambda n: f'{n}{jo}{d}'
            kn = lambda n: f'{n}{jn}{d}'
            if first:
                if q < 4:
                    S.memset('pool', b['Cn'][:], 0.0, [k('Cn')])
                    S.memset('pool', b['nst'][:], 0.0, [k('nst')])
                else:
                    S.dma('sp', b['Cn'][:], sC[d * 8:(d + 1) * 8].rearrange("h p v -> p h v"), [], [k('Cn')], K_('dst', d))
                    S.dma('sp', b['nst'][:], snl[:, d * 8:(d + 1) * 8], [], [k('nst')], K_('dst', d))
                S.copy('pool', b['Cnb'][jo][:], b['Cn'][:], [k('Cn')], [ko('Cnb')])
                S.copy('pool', b['nb'][jo][:], b['nst'][:], [k('nst')], [ko('nb')])
            S.tt('dve', b['Cn'][:], b['Cn'][:], b['decb'][si][:].unsqueeze(2).to_broadcast([128, 8, 256]), ALU.mult,
                 [k('Cn'), ks('decb')], [k('Cn')])
            for h in range(8):
                S.op('pe', lambda e, o=b['pS'][:, 8 + h:9 + h], l=b['kw'][si][:, h, :], r=onesb[0:64, 0:1]:
                     e.matmul(o, l, r, start=True, stop=True), [ks('kw'), 'onesb'], [k('pSn')], inc=(h == 7))
            S.tt('dve', b['nst'][:], b['nst'][:], b['decb'][si][:], ALU.mult, [k('nst'), ks('decb')], [k('nst')])
            S.tt('dve', b['nst'][:], b['nst'][:], b['pS'][:, 8:16], ALU.add, [k('nst'), k('pSn')], [k('nst')])
            for pr in range(4):
                pz = b['pN'][pr % 2]
                pk = f'pN{pr % 2}{d}'
                for hh in range(2):
                    h = pr * 2 + hh
                    S.op('pe', lambda e, o=pz[:, hh * 256:(hh + 1) * 256], l=b['kw'][si][:, h, :], r=b['v'][si][:, h, 0:256]:
                         e.matmul(o, l, r, start=True, stop=True), [ks('kw'), ks('v')], [pk], inc=(hh == 1))
                S.tt('dve', b['Cn'][:, pr * 2:pr * 2 + 2, :], b['Cn'][:, pr * 2:pr * 2 + 2, :],
                     pz[:, :].rearrange("p (h v) -> p h v", h=2), ALU.add, [k('Cn'), pk], [k('Cn')])
            S.copy('pool', b['Cnb'][jn][:], b['Cn'][:], [k('Cn')], [kn('Cnb')])
            S.copy('pool', b['nb'][jn][:], b['nst'][:], [k('nst')], [kn('nb')])
            for h in range(8):
                hs = slice(h * 64, (h + 1) * 64)
                S.op('pe', lambda e, o=b['pS'][0:64, h:h + 1], l=b['S'][si][:, hs], r=onesb[0:64, 0:1]:
                     e.matmul(o, l, r, start=True, stop=False), [ks('S'), 'onesb'], [k('pSd')], inc=False)
                S.op('pe', lambda e, o=b['pS'][0:64, h:h + 1], l=b['qp'][si][:, hs], r=b['nb'][jo][:, h:h + 1]:
                     e.matmul(o, l, r, start=False, stop=True), [ks('qp'), ko('nb')], [k('pSd')], inc=(h == 7))
            S.act(b['dd'][:], b['pS'][0:64, 0:8], AF.Abs, [k('pSd')], [k('dd')])
            S.tt('dve', b['dd'][:], b['dd'][:], b['tok'][si][:, 24:32], ALU.max, [k('dd'), ks('tok')], [k('dd')])
            S.op('dve', lambda e, o=b['dd'][:]: e.reciprocal(out=o, in_=o), [k('dd')], [k('dd')])
            for pr in range(4):
                pz = b['pN'][pr % 2]
                pk = f'pN{pr % 2}{d}'
                for hh in range(2):
                    h = pr * 2 + hh
                    hs = slice(h * 64, (h + 1) * 64)
                    S.op('pe', lambda e, o=pz[0:64, hh * 256:(hh + 1) * 256], l=b['S'][si][:, hs], r=b['v'][si][:, h, 0:256]:
                         e.matmul(o, l, r, start=True, stop=False), [ks('S'), ks('v')], [pk], inc=False)
                    S.op('pe', lambda e, o=pz[0:64, hh * 256:(hh + 1) * 256], l=b['qp'][si][:, hs], r=b['Cnb'][jo][:, h, :]:
                         e.matmul(o, l, r, start=False, stop=True), [ks('qp'), ko('Cnb')], [pk], inc=(hh == 1))
                S.tt('dve', b['Hc'][si][:, pr * 2:pr * 2 + 2, :], pz[0:64, :].rearrange("p (h v) -> p h v", h=2),
                     b['dd'][:, pr * 2:pr * 2 + 2].unsqueeze(2).to_broadcast([64, 2, 256]), ALU.mult, [pk, k('dd')], [ks('Hc')])
            S.dma('sp', hdl[d][r0:r0 + 64, :].rearrange("t (h v) -> t h v", h=8), b['Hc'][si][:], [ks('Hc')], ['hd_d'], 'dhd')
            b['ci'] = jn
            if lastc and q < 4:
                S.dma('sp', nC[q, d * 8:(d + 1) * 8].rearrange("h p v -> p h v"), b['Cn'][:], [k('Cn')], ['nC'], 'dout')
                S.dma('sp', nn[q, d * 8:(d + 1) * 8, :].rearrange("h p -> p h"), b['nst'][:], [k('nst')], ['nn'], 'dout', slow=True)
                S.dma('sp', nm[q, d, :].rearrange("(h o) -> h o", o=1), b['mfin'][q % 2][:], [f'mfin{q % 2}{d}'], ['nm'], 'dout')

        seqs = [(q, q * 256, 4) for q in range(4)] + [(4, NP, 64)]
        units = [[], []]
        for (q, base, nch) in seqs:
            ngr = nch // 4
            for d in range(2):
                for gi_ in range(ngr):
                    gg = gi_ if d == 0 else ngr - 1 - gi_
                    for jj in range(4):
                        jc = jj if d == 0 else 3 - jj
                        ci = gi_ * 4 + jj
                        units[d].append((q, base + gg * 256, jc, ci == 0, ci == nch - 1, jj == 0))
        nu = len(units[0])
        S.interleave([lambda d=d: pre(d, units[d][0], 0) for d in range(2)])
        for i in range(nu):
            fns = []
            if i + 1 < nu:
                fns += [lambda d=d, i=i: pre(d, units[d][i + 1], (i + 1) % 2) for d in range(2)]
            fns += [lambda d=d, i=i: post(d, units[d][i], i % 2) for d in range(2)]
            S.interleave(fns)
        S.flush()

    if STOP_AFTER <= 3:
        return nc, es, S

    with ExitStack() as st:
        HN = S.sb(st, [128, D], F32, "HN")
        load_bc(HN[:], head_norm[0:1, :], 'HN')
        gate = []
        for row in range(2):
            g_ = S.sb(st, [128, D], F32, f"gate{row}")
            load_bc(g_[:], mod[0, row:row + 1, 2 * D:3 * D], f'gate{row}')
            gate.append(g_)
        yT = S.sb(st, [128, KD, 1024], BF16, "yT")
        hf = S.sb(st, [128, D], F32, "hf")
        hb = S.sb(st, [128, D], F32, "hb")
        ogt = S.sb(st, [128, D], F32, "ogt")
        yb = [S.sb(st, [128, D], BF16, f"yb{i}") for i in range(2)]
        junk = S.sb(st, [128, 256], BF16, "junk4")
        ss8 = S.sb(st, [128, 8], F32, "ss8")
        ixw = S.sb(st, [128, 1], I32, "ixw")
        ixs = S.sb(st, [128, 1], I32, "ixs")
        ptr = [S.ps(st, [128, 1024], BF16, f"ptr4{i}") for i in range(2)]
        wo = [S.sb(st, [128, KD, 512], BF16, f"wo{i}") for i in range(2)]
        xblk = S.sb(st, [128, 8, D], F32, "xblk")
        po = [S.ps(st, [128, 512], F32, f"po4{i}") for i in range(2)]
        w_out_r = w_out.rearrange("(k p) n -> p k n", p=128)
        ti = 0
        wi_ = 0
        oi = 0

        def gath(dst, src2d, ix, rk, wk_):
            S.op('pool', lambda e, o=dst, s_=src2d, i_=ix: e.indirect_dma_start(
                out=o, out_offset=None, in_=s_, in_offset=bass.IndirectOffsetOnAxis(ap=i_, axis=0)),
                rk, wk_, dma='dg4')

        for blk in range(3):
            row = 0 if blk == 0 else 1
            for tl in range(8):
                b = ti % 2
                ti += 1
                if blk == 0:
                    r0 = tl * 128
                    S.dma('sp', hf[:], hdl[0][r0:r0 + 128, :], ['hd_d'], ['hf'], 'dhf')
                    S.dma('sp', hb[:], hdl[1][r0:r0 + 128, :], ['hd_d'], ['hb'], 'dhb')
                    S.dma('sp', ogt[:], og_d[r0:r0 + 128, :], ['qk_d'], ['ogt'], 'dog')
                    S.dma('sp', xblk[:, tl, :], xp[r0:r0 + 128, :], [], ['xblk'], 'dxb')
                else:
                    wt = (blk - 1) * 8 + tl
                    S.dma('sp', ixw[:], idx_win[wt], [], ['ixw'], 'dix')
                    S.dma('sp', ixs[:], idx_xs[wt], [], ['ixs'], 'dix')
                    gath(hf[:, :], hdl[0][:, :], ixw[:, :], ['ixw', 'hd_d'], ['hf'])
                    gath(hb[:, :], hdl[1][:, :], ixw[:, :], ['ixw', 'hd_d'], ['hb'])
                    gath(ogt[:, :], og_d[:, :], ixw[:, :], ['ixw', 'qk_d'], ['ogt'])
                    gath(xblk[:, tl, :], xs[:, :], ixs[:, :], ['ixs'], ['xblk'])
                S.tt('pool', hf[:], hf[:], hb[:], ALU.add, ['hf', 'hb'], ['hf'])
                for h in range(8):
                    S.act(junk[:], hf[:, h * 256:(h + 1) * 256], AF.Square, ['hf'], ['junk4', 'ss8'],
                          accum=ss8[:, h:h + 1])
                S.ts('dve', ss8[:], ss8[:], 1.0 / 256.0, ALU.mult, ['ss8'], ['ss8'], s2=EPS, op1=ALU.add)
                S.act(ss8[:], ss8[:], AF.Sqrt, ['ss8'], ['ss8'])
                S.op('dve', lambda e, o=ss8[:]: e.reciprocal(out=o, in_=o), ['ss8'], ['ss8'])
                S.tt('dve', hf[:].rearrange("p (h v) -> p h v", h=8), hf[:].rearrange("p (h v) -> p h v", h=8),
                     ss8[:].unsqueeze(2).to_broadcast([128, 8, 256]), ALU.mult, ['hf', 'ss8'], ['hf'])
                S.tt('pool', ogt[:], ogt[:], HN[:], ALU.mult, ['ogt', 'HN'], ['ogt'])
                S.tt('dve', yb[b][:], hf[:], ogt[:], ALU.mult, ['hf', 'ogt'], [f'yb{b}'])
                for half in range(2):
                    for kk in range(8):
                        kx = half * 8 + kk
                        S.tr(ptr[half][:, kk * 128:(kk + 1) * 128], yb[b][:, kx * 128:(kx + 1) * 128], identb[:],
                             [f'yb{b}', 'identb'], [f'ptr4{half}'], inc=(kk == 7))
                    S.copy('act' if half == 0 else 'dve', yT[:, half * 8:(half + 1) * 8, tl * 128:(tl + 1) * 128],
                           ptr[half][:].rearrange("p (k t) -> p k t", k=8), [f'ptr4{half}'], ['yT'])
            for n4 in range(4):
                wb = wi_ % 2
                wi_ += 1
                S.dma('pool', wo[wb][:], w_out_r[:, :, n4 * 512:(n4 + 1) * 512], [], [f'wo{wb}'], f'dwo{wb}')
                for tl in range(8):
                    ob = oi % 2
                    oi += 1
                    for kx in range(KD):
                        S.mm(po[ob][:, :], yT[:, kx, tl * 128:(tl + 1) * 128], wo[wb][:, kx, :], kx == 0, kx == KD - 1,
                             ['yT', f'wo{wb}'], [f'po4{ob}'])
                    S.tt('dve', po[ob][:, :], po[ob][:, :], gate[row][:, n4 * 512:(n4 + 1) * 512], ALU.mult,
                         [f'po4{ob}', f'gate{row}'], [f'po4{ob}'])
                    S.tt('dve', xblk[:, tl, n4 * 512:(n4 + 1) * 512], xblk[:, tl, n4 * 512:(n4 + 1) * 512], po[ob][:, :], ALU.add,
                         ['xblk', f'po4{ob}'], ['xblk'])
            for tl in range(8):
                r0 = blk * 1024 + tl * 128
                S.dma('sp', x1[r0:r0 + 128, :], xblk[:, tl, :], ['xblk'], ['x1'], 'dx1')
        S.flush()

    if STOP_AFTER <= 4:
        return nc, es, S

    def ffn_layer(l, x_rows, nblk, row_of_blk, experts, moe, out_rows, final):
        for blk in range(nblk):
            row = row_of_blk(blk)
            with ExitStack() as st0:
                hT = S.sb(st0, [128, KD, 1024], BF16, "hTf")
                comb = S.sb(st0, [128, 8, 8], F32, "comb")
                with ExitStack() as st:
                    G, Sh = make_GS(st, l, 1, norm_ffn[l:l + 1, :])
                    nb = NormBufs(st)
                    if moe:
                        rt = S.sb(st, [128, KD, 8], F32, "rt")
                        S.dma('sp', rt[:], router.rearrange("(k p) e -> p k e", p=128), [], ['rt'], 'dconst', slow=True)
                        h32T = S.sb(st, [128, KD, 128], F32, "h32T")
                        p32 = [S.ps(st, [128, 512], F32, f"p32{i}") for i in range(2)]
                        plg = S.ps(st, [128, 512], F32, "plg")
                        lg = S.sb(st, [128, 8], F32, "lg")
                        top = S.sb(st, [128, 8], F32, "top")
                        g12 = S.sb(st, [128, 2], F32, "g12")
                        eq = S.sb(st, [128, 8], F32, "eq")
                    for tl in range(8):
                        b = norm_tile(nb, x_rows(blk, tl), G[row], Sh[row], (hT, 'hTf'), tl * 128, keep32=moe)
                        if moe:
                            for qq in range(4):
                                pq = p32[qq % 2]
                                for kk in range(4):
                                    kx = qq * 4 + kk
                                    S.tr(pq[:, kk * 128:(kk + 1) * 128], nb.tmp[b][:, kx * 128:(kx + 1) * 128], identf[:],
                                         [f'ntmp{b}', 'identf'], [f'p32{qq % 2}'], inc=(kk == 3))
                                S.copy('act', h32T[:, qq * 4:(qq + 1) * 4, :], pq[:].rearrange("p (k t) -> p k t", k=4),
                                       [f'p32{qq % 2}'], ['h32T'])
                            for kx in range(KD):
                                S.mm(plg[:, 0:8], h32T[:, kx, :], rt[:, kx, :], kx == 0, kx == KD - 1, ['h32T', 'rt'], ['plg'])
                            S.copy('dve', lg[:], plg[:, 0:8], ['plg'], ['lg'])
                            S.op('dve', lambda e, o=top[:], i=lg[:]: e.max(out=o, in_=i), ['lg'], ['top'])
                            S.tt('dve', g12[:, 0:1], top[:, 0:1], top[:, 1:2], ALU.subtract, ['top'], ['g12'])
                            S.act(g12[:, 0:1], g12[:, 0:1], AF.Sigmoid, ['g12'], ['g12'])
                            S.ts('dve', g12[:, 1:2], g12[:, 0:1], -1.0, ALU.mult, ['g12'], ['g12'], s2=1.0, op1=ALU.add)
                            S.ts('dve', eq[:], lg[:], top[:, 0:1], ALU.is_equal, ['lg', 'top', 'g12'], ['eq'], s2=g12[:, 0:1], op1=ALU.mult)
                            S.ts('dve', comb[:, tl, :], lg[:], top[:, 1:2], ALU.is_equal, ['lg', 'top', 'g12'], ['comb'], s2=g12[:, 1:2], op1=ALU.mult)
                            S.tt('dve', comb[:, tl, :], comb[:, tl, :], eq[:], ALU.add, ['comb', 'eq'], ['comb'])
                    S.flush()
                yacc = S.sb(st0, [128, 8, D], F32, "yacc")
                with ExitStack() as st:
                    actb = S.sb(st, [128, 11, 1024], BF16, "actb")
                    w1c = [S.sb(st, [128, KD, 128], BF16, f"w1c{i}") for i in range(2)]
                    w3c = [S.sb(st, [128, KD, 128], BF16, f"w3c{i}") for i in range(2)]
                    w2p = S.sb(st, [128, 11, 512], BF16, "w2p")
                    sg = [S.sb(st, [128, 512], F32, f"sg{i}") for i in range(2)]
                    pa = [S.ps(st, [128, 512], F32, f"pfa{i}") for i in range(2)]
                    pb = [S.ps(st, [128, 512], F32, f"pfb{i}") for i in range(2)]
                    po = [S.ps(st, [128, 512], F32, f"pfo{i}") for i in range(2)]
                    wi_ = 0
                    pi_ = 0
                    oi = 0
                    first = True
                    for ei, (w1, w3, w2) in enumerate(experts):
                        w1r = w1.rearrange("(k p) n -> p k n", p=128)
                        w3r = w3.rearrange("(k p) n -> p k n", p=128)
                        w2r = w2.rearrange("(f p) n -> p f n", p=128)
                        for qd in range(4):
                            for fc in range(11):
                                f0 = (qd * 11 + fc) * 128
                                wb = wi_ % 2
                                wi_ += 1
                                S.dma('pool', w1c[wb][:], w1r[:, :, f0:f0 + 128], [], [f'w1c{wb}'], f'dw1{wb}')
                                S.dma('pool', w3c[wb][:], w3r[:, :, f0:f0 + 128], [], [f'w3c{wb}'], f'dw3{wb}')
                                for tb in range(2):
                                    p_ = pi_ % 2
                                    pi_ += 1
                                    for kx in range(KD):
                                        S.mm(pa[p_][:, :], w1c[wb][:, kx, :], hT[:, kx, tb * 512:(tb + 1) * 512], kx == 0, kx == KD - 1,
                                             [f'w1c{wb}', 'hTf'], [f'pfa{p_}'])
                                    for kx in range(KD):
                                        S.mm(pb[p_][:, :], w3c[wb][:, kx, :], hT[:, kx, tb * 512:(tb + 1) * 512], kx == 0, kx == KD - 1,
                                             [f'w3c{wb}', 'hTf'], [f'pfb{p_}'])
                                    S.act(sg[p_][:], pa[p_][:, :], AF.Silu, [f'pfa{p_}'], [f'sg{p_}'])
                                    S.tt('dve', actb[:, fc, tb * 512:(tb + 1) * 512], sg[p_][:], pb[p_][:, :], ALU.mult,
                                         [f'sg{p_}', f'pfb{p_}'], ['actb'])
                            for n4 in range(4):
                                S.dma('pool', w2p[:], w2r[:, qd * 11:(qd + 1) * 11, n4 * 512:(n4 + 1) * 512], [], ['w2p'], 'dw2p')
                                for tl in range(8):
                                    ob = oi % 2
                                    oi += 1
                                    for fc in range(11):
                                        S.mm(po[ob][:, :], actb[:, fc, tl * 128:(tl + 1) * 128], w2p[:, fc, :], fc == 0, fc == 10,
                                             ['actb', 'w2p'], [f'pfo{ob}'])
                                    ya = yacc[:, tl, n4 * 512:(n4 + 1) * 512]
                                    if moe:
                                        if first:
                                            S.ts('dve', ya, po[ob][:, :], comb[:, tl, ei:ei + 1], ALU.mult, [f'pfo{ob}', 'comb'], ['yacc'])
                                        else:
                                            S.stt(ya, po[ob][:, :], comb[:, tl, ei:ei + 1], ya, ALU.mult, ALU.add,
                                                  [f'pfo{ob}', 'comb', 'yacc'], ['yacc'])
                                    else:
                                        if first:
                                            S.copy('act', ya, po[ob][:, :], [f'pfo{ob}'], ['yacc'])
                                        else:
                                            S.tt('dve', ya, ya, po[ob][:, :], ALU.add, [f'pfo{ob}', 'yacc'], ['yacc'])
                            first = False
                    S.flush()
                with ExitStack() as st:
                    gt_ = S.sb(st, [128, D], F32, "gateR")
                    load_bc(gt_[:], mod[l, row:row + 1, 5 * D:6 * D], 'gateR')
                    xr = [S.sb(st, [128, D], F32, f"xr{i}") for i in range(2)]
                    if final:
                        NF = S.sb(st, [128, D], F32, "NF")
                        load_bc(NF[:], norm_final[0:1, :], 'NF')
                        junkf = S.sb(st, [128, D], BF16, "junkf")
                        ssf = [S.sb(st, [128, 1], F32, f"ssf{i}") for i in range(2)]
                    for tl in range(8):
                        b = tl % 2
                        S.dma('sp', xr[b][:], x_rows(blk, tl), [], [f'xr{b}'], f'dxr{b}')
                        S.tt('pool', yacc[:, tl, :], yacc[:, tl, :], gt_[:], ALU.mult, ['yacc', 'gateR'], ['yacc'])
                        S.tt('dve', xr[b][:], xr[b][:], yacc[:, tl, :], ALU.add, [f'xr{b}', 'yacc'], [f'xr{b}'])
                        if final:
                            S.act(junkf[:], xr[b][:], AF.Square, [f'xr{b}'], ['junkf', f'ssf{b}'], accum=ssf[b][:])
                            S.ts('dve', ssf[b][:], ssf[b][:], 1.0 / D, ALU.mult, [f'ssf{b}'], [f'ssf{b}'], s2=EPS, op1=ALU.add)
                            S.act(ssf[b][:], ssf[b][:], AF.Sqrt, [f'ssf{b}'], [f'ssf{b}'])
                            S.op('dve', lambda e, o=ssf[b][:]: e.reciprocal(out=o, in_=o), [f'ssf{b}'], [f'ssf{b}'])
                            S.stt(xr[b][:], xr[b][:], ssf[b][:, 0:1], NF[:], ALU.mult, ALU.mult, [f'xr{b}', f'ssf{b}', 'NF'], [f'xr{b}'])
                        S.dma('sp', out_rows(blk, tl), xr[b][:], [f'xr{b}'], ['xout'], 'dxout')
                    S.flush()

    ffn_layer(0, lambda blk, tl: x1[blk * 1024 + tl * 128: blk * 1024 + (tl + 1) * 128, :], 3,
              lambda blk: 0 if blk == 0 else 1, [(ffn_w1, ffn_w3, ffn_w2)], False,
              lambda blk, tl: x2[blk * 1024 + tl * 128: blk * 1024 + (tl + 1) * 128, :], False)
    if STOP_AFTER <= 5:
        return nc, es, S

    with ExitStack() as st:
        hall = S.sb(st, [128, 16, D], BF16, "hall")
        PW = S.sb(st, [128, 16, 512], BF16, "PW")
        S.dma('pool', PW[:], pool_w.rearrange("g (c p) n -> p (g c) n", p=128), [], ['PW'], 'dconst2')
        pmt = S.sb(st, [128, 20, 128], BF16, "pmt")
        ict = S.sb(st, [128, 4, 128], F32, "ict")
        dT = S.sb(st, [128, 16, 128], BF16, "dT")
        psg = S.sb(st, [128, D], F32, "psg")
        gt = S.sb(st, [128, D], F32, "gt6")
        Gt = S.sb(st, [128, D], F32, "G6")
        St = S.sb(st, [128, D], F32, "S6")
        xin = S.sb(st, [128, D], F32, "xin6")
        junk = S.sb(st, [128, D], BF16, "junk6")
        ss = S.sb(st, [128, 1], F32, "ss6")
        tmp = S.sb(st, [128, D], F32, "tmp6")
        idx = S.sb(st, [128, 1], I32, "idx6")
        pd = [S.ps(st, [128, 512], F32, f"pd{i}") for i in range(2)]
        pq = [S.ps(st, [128, 512], F32, f"pq{i}") for i in range(2)]
        load_bc(gt[:], norm_mix[1:2, :], 'gt6')

        def norm6(dst_tile_idx):
            S.act(junk[:], xin[:], AF.Square, ['xin6'], ['junk6', 'ss6'], accum=ss[:])
            S.ts('dve', ss[:], ss[:], 1.0 / D, ALU.mult, ['ss6'], ['ss6'], s2=EPS, op1=ALU.add)
            S.act(ss[:], ss[:], AF.Sqrt, ['ss6'], ['ss6'])
            S.op('dve', lambda e, o=ss[:]: e.reciprocal(out=o, in_=o), ['ss6'], ['ss6'])
            S.stt(tmp[:], xin[:], ss[:, 0:1], Gt[:], ALU.mult, ALU.mult, ['xin6', 'ss6', 'G6'], ['tmp6'])
            S.tt('pool', hall[:, dst_tile_idx, :], tmp[:], St[:], ALU.add, ['tmp6', 'S6'], ['hall'])

        pi_ = 0
        for grp in range(2):
            row = grp
            load_bc(Gt[:], mod[1, row:row + 1, D:2 * D], 'G6')
            load_bc(St[:], mod[1, row:row + 1, 0:D], 'S6')
            S.stt(Gt[:], Gt[:], 1.0, gt[:], ALU.add, ALU.mult, ['G6', 'gt6'], ['G6'])
            load_bc(psg[:], mod[1, row:row + 1, 2 * D:3 * D], 'psg')
            load_bc(tmp[:], pool_scale[0:1, :], 'tmp6')
            S.tt('dve', psg[:], psg[:], tmp[:], ALU.mult, ['psg', 'tmp6'], ['psg'])
            ntile = 8 if grp == 0 else 16
            for t_ in range(ntile):
                if grp == 0:
                    S.dma('sp', xin[:], x2[t_ * 128:(t_ + 1) * 128, :], ['xout'], ['xin6'], 'dxin6')
                else:
                    S.dma('sp', xin[:], x2[NP + t_ * 128:NP + (t_ + 1) * 128, :], ['xout'], ['xin6'], 'dxin6')
                norm6(t_)
            for j in range(8):
                if grp == 0:
                    jo = j % 2
                    for g in range(4):
                        S.dma('pool', pmt[:, g * 2:(g + 1) * 2, :], pm_p[(g * 2 + jo) * 2:(g * 2 + jo) * 2 + 2].rearrange("m p t -> p m t"),
                              [], ['pmt'], 'dpmt')
                        S.dma('sp', ict[:, g, :], ic_p[g * 2 + jo:g * 2 + jo + 1, :].partition_broadcast(128), [], ['ict'], 'dict')
                    S.dma('sp', xin[:], x2[j * 128:(j + 1) * 128, :], ['xout'], ['xin6'], 'dxin6')
                else:
                    mi = 0
                    for g in range(4):
                        nm_ = (3, 3, 5, 9)[g]
                        off = sum((3, 3, 5, 9)[:g])
                        S.dma('pool', pmt[:, off:off + nm_, :], pm_s[g_off(g) + j * nm_: g_off(g) + (j + 1) * nm_].rearrange("m p t -> p m t"),
                              [], ['pmt'], 'dpmt')
                        S.dma('sp', ict[:, g, :], ic_s[g * 8 + j:g * 8 + j + 1, :].partition_broadcast(128), [], ['ict'], 'dict')
                    S.dma('sp', xin[:], x2[NP + (j + 4) * 128:NP + (j + 5) * 128, :], ['xout'], ['xin6'], 'dxin6')
                for g in range(4):
                    p_ = pi_ % 2
                    pi_ += 1
                    if grp == 0:
                        srcs = [((j // 2) * 2 + ji, g * 2 + ji) for ji in range(2)]
                    else:
                        hw_ = (1, 1, 2, 4)[g]
                        off = sum((3, 3, 5, 9)[:g])
                        srcs = [(j + 4 + di, off + di + hw_) for di in range(-hw_, hw_ + 1)]
                    for cc in range(4):
                        for si, (ti_, mi_) in enumerate(srcs):
                            S.op('pe', lambda e, o=pd[p_][:, cc * 128:(cc + 1) * 128], l=hall[:, ti_, g * 512 + cc * 128: g * 512 + (cc + 1) * 128], r=pmt[:, mi_, :],
                                 a=(si == 0), z=(si == len(srcs) - 1): e.matmul(o, l, r, start=a, stop=z),
                                 ['hall', 'pmt'], [f'pd{p_}'], inc=(cc == 3 and si == len(srcs) - 1))
                    S.tt('dve', dT[:, g * 4:(g + 1) * 4, :], pd[p_][:, :].rearrange("p (c t) -> p c t", c=4),
                         ict[:, g, :].unsqueeze(1).to_broadcast([128, 4, 128]), ALU.mult, [f'pd{p_}', 'ict'], ['dT'])
                for g in range(4):
                    p_ = pi_ % 2
                    pi_ += 1
                    for cc in range(4):
                        S.mm(pq[p_][:, :], dT[:, g * 4 + cc, :], PW[:, g * 4 + cc, :], cc == 0, cc == 3, ['dT', 'PW'], [f'pq{p_}'])
                    S.tt('dve', tmp[:, g * 512:(g + 1) * 512], pq[p_][:, :], psg[:, g * 512:(g + 1) * 512], ALU.mult,
                         [f'pq{p_}', 'psg'], ['tmp6'])
                S.tt('pool', xin[:], xin[:], tmp[:], ALU.add, ['xin6', 'tmp6'], ['xin6'])
                r0 = grp * 1024 + j * 128
                S.dma('sp', x3[r0:r0 + 128, :], xin[:], ['xin6'], ['x3'], 'dx3')
        S.flush()
    if STOP_AFTER <= 6:
        return nc, es, S

    ffn_layer(1, lambda blk, tl: x3[blk * 1024 + tl * 128: blk * 1024 + (tl + 1) * 128, :], 2,
              lambda blk: blk, [(moe_w1[e], moe_w3[e], moe_w2[e]) for e in range(8)], True,
              lambda blk, tl: (yp if blk == 0 else ys)[tl * 128:(tl + 1) * 128, :], True)
    return nc, es, S


DBG_OUT = set()
STOP_AFTER = 99


def g_off(g):
    return 8 * sum((3, 3, 5, 9)[:g])


def _pool_consts(p):
    W = (2, 4, 8, 16)
    HW = (1, 1, 2, 4)
    pm_s = np.zeros((160, 128, 128), np.float32)
    ic_s = np.zeros((32, 128), np.float32)
    mi = 0
    for g, w in enumerate(W):
        for j in range(8):
            cnt = np.zeros(128, np.float32)
            mats = {}
            for to in range(128):
                r = 16 * p + 2 * j + to // 64
                cc = to % 64
                rlo, rhi = max(r - w // 2, 0), min(r + w // 2, 64)
                clo, chi = max(cc - w // 2, 0), min(cc + w // 2, 64)
                cnt[to] = (rhi - rlo) * (chi - clo)
                for rr in range(rlo, rhi):
                    wt = (rr - (16 * p - 8)) // 2
                    m = mats.setdefault(wt, np.zeros((128, 128), np.float32))
                    ti0 = ((rr - (16 * p - 8)) % 2) * 64
                    m[ti0 + clo:ti0 + chi, to] += 1.0
                wt_self = j + 4
                m = mats.setdefault(wt_self, np.zeros((128, 128), np.float32))
                m[to, to] -= cnt[to]
            for di in range(-HW[g], HW[g] + 1):
                wt = j + 4 + di
                if wt in mats:
                    pm_s[mi] = mats[wt]
                mi += 1
            ic_s[g * 8 + j] = 1.0 / cnt
    assert mi == 160
    pm_p = np.zeros((16, 128, 128), np.float32)
    ic_p = np.zeros((8, 128), np.float32)
    for g, w in enumerate(W):
        for jo in range(2):
            for to in range(128):
                t = jo * 128 + to
                lo, hi = max(t - w // 2, 0), min(t + w // 2, 256)
                ic_p[g * 2 + jo, to] = 1.0 / (hi - lo)
                for tt in range(lo, hi):
                    pm_p[(g * 2 + jo) * 2 + tt // 128, tt % 128, to] += 1.0
                pm_p[(g * 2 + jo) * 2 + jo, to, to] -= (hi - lo)
    return pm_s, ic_s, pm_p, ic_p


def _core_inputs(c, inp):
    s, p = c // 4, c % 4
    f = np.float32
    m = {}
    m["xp"] = np.ascontiguousarray(inp["x_prompt"][4 * c:4 * c + 4].reshape(NP, D))
    m["xs"] = np.ascontiguousarray(inp["x_sample"][s])
    m["sC"] = np.ascontiguousarray(inp["state_mlstm_C"][s, 0].reshape(16, 128, 256))
    m["snl"] = np.ascontiguousarray(inp["state_mlstm_n"][s, 0].reshape(16, 128).T)
    m["sml"] = np.ascontiguousarray(inp["state_mlstm_m"][s, 0].reshape(2, 8, 1))
    c2 = np.stack([inp["c_ctx"], inp["c"][s]], axis=-1).reshape(KD, 128, 2)
    m["c2"] = np.ascontiguousarray(c2.transpose(1, 0, 2))
    m["w_ada"] = inp["w_ada"]
    m["b_ada"] = inp["b_ada"]
    m["norm_mix"] = inp["norm_mix"]
    m["norm_ffn"] = inp["norm_ffn"]
    m["norm_final"] = inp["norm_final"].reshape(1, D)
    m["w_in"] = inp["mlstm_w_in"][0]
    m["bgl"] = np.ascontiguousarray(inp["mlstm_b_gates"][0].reshape(4, 8).T)
    m["head_norm"] = inp["mlstm_head_norm"].reshape(1, D)
    m["w_out"] = inp["mlstm_w_out"][0]
    m["pool_w"] = inp["pool_w"][0]
    m["pool_scale"] = inp["pool_scale"].reshape(1, D)
    m["ffn_w1"] = inp["ffn_w1"][0]
    m["ffn_w3"] = inp["ffn_w3"][0]
    m["ffn_w2"] = inp["ffn_w2"][0]
    m["router"] = inp["moe_router"][0]
    m["moe_w1"] = inp["moe_w1"][0]
    m["moe_w3"] = inp["moe_w3"][0]
    m["moe_w2"] = inp["moe_w2"][0]
    m["identf"] = np.eye(128, dtype=f)
    cm = np.zeros((2, 64, 64), f)
    si, ti = np.meshgrid(np.arange(64), np.arange(64), indexing="ij")
    cm[0][si > ti] = NEG
    cm[1][si < ti] = NEG
    m["cmask"] = np.ascontiguousarray(np.tile(cm, (1, 1, 8)))
    bm = np.zeros((8, 8, 64), f)
    for h in range(8):
        bm[h, h, :] = 1.0
    m["bmask"] = bm.reshape(8, 512)
    iw = np.zeros((16, 128, 1), np.int32)
    for wt in range(16):
        for i in range(128):
            r = 16 * p - 8 + 2 * wt + i // 64
            r = min(max(r, 0), 63)
            iw[wt, i, 0] = NP + r * 64 + i % 64
    m["idx_win"] = iw
    m["idx_xs"] = iw - NP
    pm_s, ic_s, pm_p, ic_p = _pool_consts(p)
    m["pm_s"], m["ic_s"], m["pm_p"], m["ic_p"] = pm_s, ic_s, pm_p, ic_p
    return {k: np.ascontiguousarray(v) for k, v in m.items()}


def kernel(**inputs):
    inp = {k: np.asarray(v) for k, v in inputs.items()}
    nc, es, S = build_program()
    in_maps = [_core_inputs(c, inp) for c in range(8)]
    res = run_bass_kernel_spmd(nc, in_maps, core_ids=list(range(8)))
    r = res.results
    y_prompt = np.concatenate([r[c]["yp"] for c in range(8)], 0).reshape(32, 256, D).astype(np.float32)
    y_sample = np.stack([np.concatenate([r[4 * s + p]["ys"] for p in range(4)], 0) for s in range(2)], 0).astype(np.float32)
    new_C = np.concatenate([r[c]["nC"] for c in range(8)], 0).reshape(32, 1, 2, 8, 128, 256).astype(np.float32)
    new_n = np.concatenate([r[c]["nn"] for c in range(8)], 0).reshape(32, 1, 2, 8, 128).astype(np.float32)
    new_m = np.concatenate([r[c]["nm"] for c in range(8)], 0).reshape(32, 1, 2, 8).astype(np.float32)
    return (y_prompt, y_sample, new_C, new_n, new_m)
```

```python
import numpy as np
from contextlib import ExitStack
import concourse.bass as bass
import concourse.mybir as mybir
from concourse.bass_utils import run_bass_kernel_spmd

F32 = mybir.dt.float32
BF16 = mybir.dt.bfloat16
I32 = mybir.dt.int32
AF = mybir.ActivationFunctionType
ALU = mybir.AluOpType

D = 2048
KD = 16
DFF = 5632
NFC = 44
NH = 8
NP = 1024
NS = 4096
T0 = NP + NS
T1 = 2048
EPS = 1e-6
NEG = -30000.0
ENG = ('pe', 'act', 'dve', 'pool', 'sp')


class Sched:
    def __init__(self, nc, es):
        self.nc = nc
        self.es = es
        self.eobj = {'pe': nc.tensor, 'act': nc.scalar, 'dve': nc.vector, 'pool': nc.gpsimd, 'sp': nc.sync}
        self.ops = {e: [] for e in ENG}
        self.cnt = {e: 0 for e in ENG}
        self.dcnt = {}
        self.seen = {e: {} for e in ENG}
        self.lastw = {}
        self.readers = {}
        self.sems = {}
        self.pend = {e: ([], []) for e in ENG}
        self.nsb = 0
        self.capture = None

    def sem(self, name):
        if name not in self.sems:
            self.sems[name] = self.es.enter_context(self.nc.semaphore(name))
        return self.sems[name]

    def op(self, eng, fn, reads=(), writes=(), dma=None, inc=True):
        if self.capture is not None:
            self.capture.append((eng, fn, tuple(reads), tuple(writes), dma, inc))
            return None
        waits = {}

        def need(tok):
            s, v = tok
            if waits.get(s, 0) < v:
                waits[s] = v
        for b in reads:
            if b in self.lastw:
                need(self.lastw[b])
        for b in writes:
            if b in self.lastw:
                need(self.lastw[b])
            for r in self.readers.get(b, ()):
                need(r)
        wl = []
        for s, v in waits.items():
            if s in self.dcnt:
                v = 16 * self.dcnt[s]
            if self.seen[eng].get(s, 0) < v:
                self.seen[eng][s] = v
                wl.append((s, v))
        tok = None
        incspec = None
        if dma is not None:
            self.dcnt[dma] = self.dcnt.get(dma, 0) + 1
            tok = (dma, 16 * self.dcnt[dma])
            incspec = (dma, 16)
        elif inc:
            self.cnt[eng] += 1
            tok = ('E' + eng, self.cnt[eng])
            incspec = ('E' + eng, 1)
        self.ops[eng].append((wl, fn, incspec))
        pr, pw = self.pend[eng]
        if tok is None:
            pr.extend(reads)
            pw.extend(writes)
        else:
            if dma is None:
                allr = list(reads) + pr
                allw = list(writes) + pw
                self.pend[eng] = ([], [])
            else:
                allr, allw = reads, writes
            for b in allw:
                self.lastw[b] = tok
                self.readers[b] = []
            for b in allr:
                self.readers.setdefault(b, []).append(tok)
        return tok

    def interleave(self, fns):
        lists = []
        for f in fns:
            self.capture = []
            f()
            lists.append(self.capture)
        self.capture = None
        n = max(len(l) for l in lists) if lists else 0
        for i in range(n):
            for l in lists:
                if i < len(l):
                    self.op(*l[i])

    def flush(self, final=False):
        nc = self.nc
        for s in list(self.dcnt) + ['E' + e for e in ENG]:
            self.sem(s)
        tails = {}
        for e in ENG:
            tl = []
            for e2 in ENG:
                if e2 != e and self.cnt[e2] > self.seen[e].get('E' + e2, 0):
                    tl.append(('E' + e2, self.cnt[e2]))
                    self.seen[e]['E' + e2] = self.cnt[e2]
            for s, c in self.dcnt.items():
                if 16 * c > self.seen[e].get(s, 0):
                    tl.append((s, 16 * c))
                    self.seen[e][s] = 16 * c
            tails[e] = tl
        ops = self.ops
        sems = self.sems

        def run(e, name):
            for wl, fn, incspec in ops[name]:
                for s, v in wl:
                    e.wait_ge(sems[s], v)
                ins = fn(e)
                if incspec is not None:
                    ins.then_inc(sems[incspec[0]], incspec[1])
            for s, v in tails[name]:
                e.wait_ge(sems[s], v)
        with nc.Block() as block:
            @block.tensor
            def _(e):
                run(e, 'pe')

            @block.scalar
            def _(e):
                run(e, 'act')

            @block.vector
            def _(e):
                run(e, 'dve')

            @block.gpsimd
            def _(e):
                run(e, 'pool')

            @block.sync
            def _(e):
                run(e, 'sp')
        self.ops = {e: [] for e in ENG}

    def sb(self, st, shape, dt, name=None):
        self.nsb += 1
        return st.enter_context(self.nc.sbuf_tensor(f"{name or 't'}_{self.nsb}", list(shape), dt))

    def ps(self, st, shape, dt, name=None):
        self.nsb += 1
        return st.enter_context(self.nc.psum_tensor(f"{name or 'p'}_{self.nsb}", list(shape), dt))

    def dma(self, q, out, in_, reads, writes, sem, slow=False):
        if slow:
            return self.op(q, lambda e, o=out, i=in_: e.dma_start(out=o, in_=i, allow_slow_non_contiguous=True),
                           reads, writes, dma=sem)
        return self.op(q, lambda e, o=out, i=in_: e.dma_start(out=o, in_=i), reads, writes, dma=sem)

    def mm(self, out, lhsT, rhs, start, stop, reads, writes):
        return self.op('pe', lambda e, o=out, l=lhsT, r=rhs, a=start, b=stop: e.matmul(o, l, r, start=a, stop=b),
                       reads, writes, inc=stop)

    def tr(self, out, in_, ident, reads, writes, inc=True):
        return self.op('pe', lambda e, o=out, i=in_, d=ident: e.transpose(o, i, d), reads, writes, inc=inc)

    def act(self, out, in_, func, reads, writes, bias=None, scale=None, accum=None, eng='act'):
        def f(e, o=out, i=in_, fu=func, b=bias, s=scale, a=accum):
            kw = {}
            if b is not None:
                kw['bias'] = b
            if s is not None:
                kw['scale'] = s
            if a is not None:
                kw['accum_out'] = a
            return e.activation(o, i, fu, **kw)
        return self.op('act', f, reads, writes)

    def tt(self, eng, out, in0, in1, op, reads, writes):
        return self.op(eng, lambda e, o=out, a=in0, b=in1, p=op: e.tensor_tensor(out=o, in0=a, in1=b, op=p), reads, writes)

    def ts(self, eng, out, in0, s1, op0, reads, writes, s2=None, op1=None):
        def f(e, o=out, a=in0, x1=s1, x2=s2, p0=op0, p1=op1):
            if p1 is None:
                return e.tensor_scalar(out=o, in0=a, scalar1=x1, scalar2=None, op0=p0)
            return e.tensor_scalar(out=o, in0=a, scalar1=x1, scalar2=x2, op0=p0, op1=p1)
        return self.op(eng, f, reads, writes)

    def stt(self, out, in0, scalar, in1, op0, op1, reads, writes):
        return self.op('dve', lambda e, o=out, a=in0, s=scalar, b=in1, p0=op0, p1=op1:
                       e.scalar_tensor_tensor(out=o, in0=a, scalar=s, in1=b, op0=p0, op1=p1), reads, writes)

    def copy(self, eng, out, in_, reads, writes):
        if eng == 'act':
            return self.op('act', lambda e, o=out, i=in_: e.copy(o, i), reads, writes)
        return self.op(eng, lambda e, o=out, i=in_: e.tensor_copy(out=o, in_=i), reads, writes)

    def memset(self, eng, ap, val, writes):
        return self.op(eng, lambda e, a=ap, v=val: e.memset(a, v), (), writes)


def build_program(dbg=False):
    nc = bass.Bass("TRN2", target_bir_lowering=False)
    es = ExitStack()
    S = Sched(nc, es)

    def din(name, shape, dt=F32):
        return nc.dram_tensor(name, list(shape), dt, kind="ExternalInput").ap()

    def dout(name, shape, dt=F32):
        return nc.dram_tensor(name, list(shape), dt, kind="ExternalOutput").ap()

    def dscr(name, shape, dt=F32):
        kind = "ExternalOutput" if (dbg and name in DBG_OUT) else "Internal"
        return nc.dram_tensor(name, list(shape), dt, kind=kind).ap()

    xp = din("xp", [NP, D])
    xs = din("xs", [NS, D])
    sC = din("sC", [16, 128, 256])
    snl = din("snl", [128, 16])
    sml = din("sml", [2, 8, 1])
    c2 = din("c2", [128, KD, 2])
    w_ada = din("w_ada", [2, D, 6 * D])
    b_ada = din("b_ada", [2, 6 * D])
    norm_mix = din("norm_mix", [2, D])
    norm_ffn = din("norm_ffn", [2, D])
    norm_final = din("norm_final", [1, D])
    w_in = din("w_in", [D, 6176])
    bgl = din("bgl", [8, 4])
    head_norm = din("head_norm", [1, D])
    w_out = din("w_out", [D, D])
    pool_w = din("pool_w", [4, 512, 512])
    pool_scale = din("pool_scale", [1, D])
    ffn_w1 = din("ffn_w1", [D, DFF])
    ffn_w3 = din("ffn_w3", [D, DFF])
    ffn_w2 = din("ffn_w2", [DFF, D])
    router = din("router", [D, 8])
    moe_w1 = din("moe_w1", [8, D, DFF])
    moe_w3 = din("moe_w3", [8, D, DFF])
    moe_w2 = din("moe_w2", [8, DFF, D])
    identf_d = din("identf", [128, 128])
    cmask_d = din("cmask", [2, 64, 512])
    bmask_d = din("bmask", [8, 512])
    idx_win = din("idx_win", [16, 128, 1], I32)
    idx_xs = din("idx_xs", [16, 128, 1], I32)
    pm_s = din("pm_s", [160, 128, 128])
    ic_s = din("ic_s", [32, 128])
    pm_p = din("pm_p", [16, 128, 128])
    ic_p = din("ic_p", [8, 128])

    yp = dout("yp", [NP, D])
    ys = dout("ys", [1024, D])
    nC = dout("nC", [4, 16, 128, 256])
    nn = dout("nn", [4, 16, 128])
    nm = dout("nm", [4, 2, 8])

    mod = dscr("mod", [2, 2, 6 * D])
    qT_d = dscr("qT_d", [NH, 128, T0], BF16)
    kT_d = dscr("kT_d", [NH, 128, T0], BF16)
    k_d = dscr("k_d", [T0, 1024], BF16)
    v_d = dscr("v_d", [T0, D], BF16)
    og_d = dscr("og_d", [T0, D])
    gi_d = dscr("gi_d", [4, 8, T0])
    hdl = [dscr("hd0", [T0, D]), dscr("hd1", [T0, D])]
    TW = NP + 2048
    x1 = dscr("x1", [TW, D])
    x2 = dscr("x2", [TW, D])
    x3 = dscr("x3", [T1, D])

    identf = S.sb(es, [128, 128], F32, "identf")
    identb = S.sb(es, [128, 128], BF16, "identb")
    S.dma('sp', identf[:], identf_d[:, :], [], ['identf'], 'dconst')
    S.copy('dve', identb[:], identf[:], ['identf'], ['identb'])

    rowsel = {0: 0, 1: 1}

    def load_bc(tile_ap, src_row_ap, key, sem='dmod'):
        S.dma('sp', tile_ap, src_row_ap.partition_broadcast(128), [], [key], sem)

    with ExitStack() as st:
        c2t = S.sb(st, [128, KD, 2], F32)
        c2s = S.sb(st, [128, KD, 2], BF16)
        S.dma('sp', c2t[:], c2[:, :, :], [], ['c2t'], 'dconst')
        S.act(c2s[:], c2t[:], AF.Silu, ['c2t'], ['c2s'])
        wa = [S.sb(st, [128, KD, 512], BF16, f"wa{i}") for i in range(2)]
        bt = [S.sb(st, [2, 512], F32, f"bt{i}") for i in range(2)]
        mo = [S.sb(st, [2, 512], F32, f"mo{i}") for i in range(2)]
        pa = [S.ps(st, [128, 512], F32, f"pa{i}") for i in range(2)]
        it = 0
        for l in range(2):
            wl = w_ada[l].rearrange("(k p) n -> p k n", p=128)
            for nci in range(24):
                b = it % 2
                it += 1
                n0 = nci * 512
                S.dma('pool', wa[b][:], wl[:, :, n0:n0 + 512], [], [f'wa{b}'], f'dwa{b}')
                S.dma('sp', bt[b][:], b_ada[l:l + 1, n0:n0 + 512].partition_broadcast(2), [], [f'bt{b}'], f'dbt{b}')
                for k in range(KD):
                    S.mm(pa[b][0:2, :], c2s[:, k, :], wa[b][:, k, :], k == 0, k == KD - 1,
                         ['c2s', f'wa{b}'], [f'pa{b}'])
                S.tt('dve', mo[b][:], pa[b][0:2, :], bt[b][:], ALU.add, [f'pa{b}', f'bt{b}'], [f'mo{b}'])
                S.dma('sp', mod[l, :, n0:n0 + 512], mo[b][:], [f'mo{b}'], ['mod'], 'dmodw')
        S.flush()

    def make_GS(st, l, which, gvec_ap):
        base = 0 if which == 0 else 3
        gt = S.sb(st, [128, D], F32, "gt")
        load_bc(gt[:], gvec_ap, 'gt')
        G, Sh = [], []
        for row in range(2):
            g_ = S.sb(st, [128, D], F32, f"G{row}")
            s_ = S.sb(st, [128, D], F32, f"S{row}")
            load_bc(g_[:], mod[l, row:row + 1, (base + 1) * D:(base + 2) * D], f'G{row}')
            load_bc(s_[:], mod[l, row:row + 1, base * D:(base + 1) * D], f'S{row}')
            S.stt(g_[:], g_[:], 1.0, gt[:], ALU.add, ALU.mult, [f'G{row}', 'gt'], [f'G{row}'])
            G.append(g_)
            Sh.append(s_)
        return G, Sh

    class NormBufs:
        def __init__(self, st):
            self.xin = [S.sb(st, [128, D], F32, f"xin{i}") for i in range(2)]
            self.junk = S.sb(st, [128, D], BF16, "junk")
            self.ss = [S.sb(st, [128, 1], F32, f"ss{i}") for i in range(2)]
            self.tmp = [S.sb(st, [128, D], F32, f"ntmp{i}") for i in range(2)]
            self.h = [S.sb(st, [128, D], BF16, f"nh{i}") for i in range(2)]
            self.ptr = [S.ps(st, [128, 1024], BF16, f"ptr{i}") for i in range(2)]
            self.n = 0

    def norm_tile(nb, x_src_ap, G, Sh, hT_dst, col0, loaded=False, xkey=None, keep32=False):
        b = nb.n % 2
        nb.n += 1
        if not loaded:
            S.dma('sp', nb.xin[b][:], x_src_ap, [xkey] if xkey else [], [f'xin{b}'], f'dxin{b}')
        S.act(nb.junk[:], nb.xin[b][:], AF.Square, [f'xin{b}'], ['junk', f'ss{b}'], accum=nb.ss[b][:])
        S.ts('dve', nb.ss[b][:], nb.ss[b][:], 1.0 / D, ALU.mult, [f'ss{b}'], [f'ss{b}'], s2=EPS, op1=ALU.add)
        S.act(nb.ss[b][:], nb.ss[b][:], AF.Sqrt, [f'ss{b}'], [f'ss{b}'])
        S.op('dve', lambda e, o=nb.ss[b][:]: e.reciprocal(out=o, in_=o), [f'ss{b}'], [f'ss{b}'])
        S.stt(nb.tmp[b][:], nb.xin[b][:], nb.ss[b][:, 0:1], G[:], ALU.mult, ALU.mult,
              [f'xin{b}', f'ss{b}', 'G0', 'G1'], [f'ntmp{b}'])
        if keep32:
            S.tt('pool', nb.tmp[b][:], nb.tmp[b][:], Sh[:], ALU.add, [f'ntmp{b}', 'S0', 'S1'], [f'ntmp{b}'])
            S.copy('pool', nb.h[b][:], nb.tmp[b][:], [f'ntmp{b}'], [f'nh{b}'])
        else:
            S.tt('pool', nb.h[b][:], nb.tmp[b][:], Sh[:], ALU.add, [f'ntmp{b}', 'S0', 'S1'], [f'nh{b}'])
        if hT_dst is not None:
            ht, hkey = hT_dst
            for half in range(2):
                for kk in range(8):
                    k = half * 8 + kk
                    S.tr(nb.ptr[half][:, kk * 128:(kk + 1) * 128], nb.h[b][:, k * 128:(k + 1) * 128], identb[:],
                         [f'nh{b}', 'identb'], [f'ptr{half}'], inc=(kk == 7))
                S.copy('act' if half == 0 else 'dve',
                       ht[:, half * 8:(half + 1) * 8, col0:col0 + 128],
                       nb.ptr[half][:].rearrange("p (k t) -> p k t", k=8), [f'ptr{half}'], [hkey])
        return b

    def x0_rows(t0, n):
        if t0 < NP:
            return xp[t0:t0 + n, :]
        return xs[t0 - NP:t0 - NP + n, :]

    with ExitStack() as st:
        G, Sh = make_GS(st, 0, 0, norm_mix[0:1, :])
        nb = NormBufs(st)
        hT = S.sb(st, [128, KD, 1024], BF16, "hT")
        wc = [S.sb(st, [128, KD, 512], BF16, f"wc{i}") for i in range(2)]
        ev = [S.sb(st, [128, 512], BF16, f"ev{i}") for i in range(2)]
        evf = [S.sb(st, [128, 512], F32, f"evf{i}") for i in range(2)]
        pm = [S.ps(st, [128, 512], F32, f"pm{i}") for i in range(2)]
        bg = S.sb(st, [8, 4], F32, "bg")
        gtmp = S.sb(st, [8, 512], F32, "gtmp")
        S.dma('sp', bg[:], bgl[:, :], [], ['bg'], 'dconst')
        S.ts('dve', bg[:], bg[:], 1.0 / 15.0, ALU.mult, ['bg'], ['bg'])
        w_in_r = w_in.rearrange("(k p) n -> p k n", p=128)
        wi = 0
        ei = 0
        for blk in range(T0 // 1024):
            row = 0 if blk == 0 else 1
            for tl in range(8):
                t0 = blk * 1024 + tl * 128
                norm_tile(nb, x0_rows(t0, 128), G[row], Sh[row], (hT, 'hT'), tl * 128)
            for cg in range(13):
                wb = wi % 2
                wi += 1
                ncol = 512 if cg < 12 else 32
                S.dma('pool', wc[wb][:, :, 0:ncol], w_in_r[:, :, cg * 512:cg * 512 + ncol], [], [f'wc{wb}'], f'dwc{wb}')
                if cg < 4:
                    for hh in range(4):
                        head = (cg % 2) * 4 + hh
                        for tb in range(2):
                            pb = ei % 2
                            ei += 1
                            for k in range(KD):
                                S.mm(pm[pb][:, :], wc[wb][:, k, hh * 128:(hh + 1) * 128], hT[:, k, tb * 512:(tb + 1) * 512],
                                     k == 0, k == KD - 1, [f'wc{wb}', 'hT'], [f'pm{pb}'])
                            dst = qT_d if cg < 2 else kT_d
                            if cg < 2:
                                S.copy('act', ev[pb][:], pm[pb][:], [f'pm{pb}'], [f'ev{pb}'])
                            else:
                                S.op('act', lambda e, o=ev[pb][:], i=pm[pb][:]: e.mul(o, i, 128.0 ** -0.5),
                                     [f'pm{pb}'], [f'ev{pb}'])
                            c0 = blk * 1024 + tb * 512
                            S.dma('sp', dst[head, :, c0:c0 + 512], ev[pb][:], [f'ev{pb}'], ['qk_d'], 'dqkw')
                if 2 <= cg < 12:
                    for tl in range(8):
                        pb = ei % 2
                        ei += 1
                        for k in range(KD):
                            S.mm(pm[pb][:, :], hT[:, k, tl * 128:(tl + 1) * 128], wc[wb][:, k, :],
                                 k == 0, k == KD - 1, [f'wc{wb}', 'hT'], [f'pm{pb}'])
                        r0 = blk * 1024 + tl * 128
                        if cg < 4:
                            S.op('act', lambda e, o=ev[pb][:], i=pm[pb][:]: e.mul(o, i, 128.0 ** -0.5),
                                 [f'pm{pb}'], [f'ev{pb}'])
                            S.dma('sp', k_d[r0:r0 + 128, (cg - 2) * 512:(cg - 1) * 512], ev[pb][:], [f'ev{pb}'], ['qk_d'], 'dqkw')
                        elif cg < 8:
                            S.copy('dve', ev[pb][:], pm[pb][:], [f'pm{pb}'], [f'ev{pb}'])
                            S.dma('sp', v_d[r0:r0 + 128, (cg - 4) * 512:(cg - 3) * 512], ev[pb][:], [f'ev{pb}'], ['qk_d'], 'dqkw')
                        else:
                            S.act(evf[pb][:], pm[pb][:], AF.Sigmoid, [f'pm{pb}'], [f'evf{pb}'])
                            S.dma('sp', og_d[r0:r0 + 128, (cg - 8) * 512:(cg - 7) * 512], evf[pb][:], [f'evf{pb}'], ['qk_d'], 'dqkw')
                if cg == 12:
                    for grp in range(4):
                        for tb in range(2):
                            pb = ei % 2
                            ei += 1
                            for k in range(KD):
                                S.mm(pm[pb][0:8, :], wc[wb][:, k, grp * 8:(grp + 1) * 8], hT[:, k, tb * 512:(tb + 1) * 512],
                                     k == 0, k == KD - 1, [f'wc{wb}', 'hT'], [f'pm{pb}'])
                            S.act(gtmp[:], pm[pb][0:8, :], AF.Tanh, [f'pm{pb}', 'bg'], ['gtmp'],
                                  bias=bg[:, grp:grp + 1], scale=1.0 / 15.0)
                            if grp % 2 == 0:
                                S.ts('dve', evf[pb][0:8, :], gtmp[:], 15.0, ALU.mult, ['gtmp'], [f'evf{pb}'])
                            else:
                                S.act(gtmp[:], gtmp[:], AF.Exp, ['gtmp'], ['gtmp'], scale=-15.0)
                                S.act(gtmp[:], gtmp[:], AF.Ln, ['gtmp'], ['gtmp'], bias=1.0)
                                S.ts('dve', evf[pb][0:8, :], gtmp[:], -1.0, ALU.mult, ['gtmp'], [f'evf{pb}'])
                            c0 = blk * 1024 + tb * 512
                            S.dma('sp', gi_d[grp, :, c0:c0 + 512], evf[pb][0:8, :], [f'evf{pb}'], ['qk_d'], 'dqkw')
        S.flush()


    if STOP_AFTER <= 2:
        return nc, es, S

    with ExitStack() as st:
        ones8 = S.sb(st, [8, 128], F32, "ones8")
        bmask = S.sb(st, [8, 512], F32, "bmask")
        S.memset('dve', ones8[:], 1.0, ['ones8'])
        onesb = S.sb(st, [128, 1], BF16, "onesb")
        S.memset('dve', onesb[:], 1.0, ['onesb'])
        S.dma('sp', bmask[:], bmask_d[:, :], [], ['bmask'], 'dconst')
        bmask3 = bmask[:].rearrange("p (h t) -> p h t", h=8)
        cm = []
        B = []
        for d in range(2):
            c_ = S.sb(st, [64, 512], F32, f"cm{d}")
            S.dma('sp', c_[:], cmask_d[d, :, :], [], [f'cm{d}'], 'dconst')
            cm.append(c_)
            b = {}
            b['I'] = S.sb(st, [8, 256], F32, f"I{d}")
            b['F'] = S.sb(st, [8, 256], F32, f"F{d}")
            b['qT'] = S.sb(st, [128, 8, 256], BF16, f"qT{d}")
            b['kT'] = S.sb(st, [128, 8, 256], BF16, f"kT{d}")
            for nm_ in ('b', 'c', 'M', 'nM', 'wk', 'wi', 'fl', 't'):
                b[nm_] = S.sb(st, [8, 64], F32, f"r{nm_}{d}")
            b['m'] = [S.sb(st, [8, 1], F32, f"m{i}{d}") for i in range(2)]
            b['mfin'] = [S.sb(st, [8, 1], F32, f"mfin{i}{d}") for i in range(2)]
            b['BD1'] = S.sb(st, [8, 512], F32, f"BD1{d}")
            b['BD2'] = S.sb(st, [8, 512], F32, f"BD2{d}")
            b['dg'] = S.sb(st, [8, 8], F32, f"dg{d}")
            b['E'] = S.sb(st, [64, 512], F32, f"E{d}")
            for nm_, shp, dt_ in (('v', [64, 8, 257], BF16), ('k', [64, 8, 128], BF16), ('tok', [64, 32], F32),
                                  ('S', [64, 512], BF16), ('qp', [128, 512], BF16), ('kw', [64, 8, 128], BF16),
                                  ('decb', [128, 8], F32), ('Hc', [64, 8, 256], F32)):
                b[nm_] = [S.sb(st, shp, dt_, f"{nm_}{i}{d}") for i in range(2)]
            b['Cn'] = S.sb(st, [128, 8, 256], F32, f"Cn{d}")
            b['nst'] = S.sb(st, [128, 8], F32, f"nst{d}")
            b['Cnb'] = [S.sb(st, [128, 8, 256], BF16, f"Cnb{i}{d}") for i in range(2)]
            b['nb'] = [S.sb(st, [128, 8], BF16, f"nb{i}{d}") for i in range(2)]
            b['dd'] = S.sb(st, [64, 8], F32, f"dd{d}")
            b['pA'] = S.ps(st, [128, 512], F32, f"pA{d}")
            b['pS'] = S.ps(st, [128, 512], F32, f"pS{d}")
            b['pN'] = [S.ps(st, [128, 512], F32, f"pN{i}{d}") for i in range(2)]
            b['ci'] = 0
            b['mi'] = 0
            for i in range(2):
                S.memset('pool', b['v'][i][:, :, 256:257], 1.0, [f'v{i}{d}'])
            B.append(b)

        def K_(n, d):
            return f'{n}{d}'

        def group_load(d, g0):
            b = B[d]
            S.dma('sp', b['I'][:], gi_d[2 * d, :, g0:g0 + 256], ['qk_d'], [K_('I', d)], K_('dgl', d))
            S.dma('sp', b['F'][:], gi_d[2 * d + 1, :, g0:g0 + 256], ['qk_d'], [K_('F', d)], K_('dgl', d))
            S.dma('sp', b['qT'][:], qT_d[:, :, g0:g0 + 256].rearrange("h p t -> p h t"), ['qk_d'], [K_('qT', d)], K_('dgl', d))
            S.dma('sp', b['kT'][:], kT_d[:, :, g0:g0 + 256].rearrange("h p t -> p h t"), ['qk_d'], [K_('kT', d)], K_('dgl', d))

        def pre(d, u, si):
            q, g0, jc, first, lastc, newgrp = u
            b = B[d]
            r0 = g0 + jc * 64
            cs = slice(jc * 64, jc * 64 + 64)
            last = 63 if d == 0 else 0
            k = lambda n: K_(n, d)
            ks = lambda n: f'{n}{si}{d}'

            def rv(ap):
                return ap[:, ::-1] if d == 1 else ap
            if first:
                if q < 4:
                    S.memset('pool', b['m'][b['mi']][:], 0.0, [k('m')])
                else:
                    S.dma('sp', b['m'][b['mi']][:], sml[d], [], [k('m')], K_('dst', d))
            if newgrp:
                group_load(d, g0)
            mcur = b['m'][b['mi']]
            mnew = b['m'][1 - b['mi']]
            S.dma('sp', b['v'][si][:, :, 0:256], v_d[r0:r0 + 64, :].rearrange("t (h v) -> t h v", h=8), ['qk_d'], [ks('v')], ks('dcl'))
            S.dma('sp', b['k'][si][:], k_d[r0:r0 + 64, :].rearrange("t (h v) -> t h v", h=8), ['qk_d'], [ks('k')], ks('dcl'))
            S.op('dve', lambda e, o=rv(b['b'][:]), a=ones8[:, 0:64], x=rv(b['F'][:, cs]):
                 e.tensor_tensor_scan(out=o, data0=a, data1=x, initial=0.0, op0=ALU.mult, op1=ALU.add),
                 ['ones8', k('F')], [k('b')])
            S.tt('dve', b['c'][:], b['I'][:, cs], b['b'][:], ALU.subtract, [k('I'), k('b')], [k('c')])
            S.op('dve', lambda e, o=rv(b['M'][:]), a=ones8[:, 0:64], x=rv(b['c'][:]), i=mcur[:, 0:1]:
                 e.tensor_tensor_scan(out=o, data0=a, data1=x, initial=i, op0=ALU.mult, op1=ALU.max),
                 ['ones8', k('c'), k('m')], [k('M')])
            S.ts('dve', b['nM'][:], b['M'][:], -1.0, ALU.mult, [k('M')], [k('nM')])
            S.act(b['wk'][:], b['c'][:], AF.Exp, [k('c'), k('nM')], [k('wk')], bias=b['nM'][:, last:last + 1])
            S.act(b['wi'][:], b['nM'][:], AF.Exp, [k('nM'), k('m')], [k('wi')], bias=mcur[:, 0:1])
            S.tt('dve', b['t'][:], b['nM'][:], b['b'][:], ALU.subtract, [k('nM'), k('b')], [k('t')])
            S.act(b['fl'][:], b['t'][:], AF.Exp, [k('t')], [k('fl')])
            S.tt('dve', mnew[:], b['b'][:, last:last + 1], b['M'][:, last:last + 1], ALU.add, [k('b'), k('M'), k('m')], [k('m')])
            if lastc and q < 4:
                S.copy('dve', b['mfin'][q % 2][:], mnew[:], [k('m')], [f'mfin{q % 2}{d}'])
            S.tt('dve', b['BD1'][:].rearrange("p (h t) -> p h t", h=8), bmask3,
                 b['nM'][:].unsqueeze(1).to_broadcast([8, 8, 64]), ALU.mult, ['bmask', k('nM')], [k('BD1')])
            S.tt('dve', b['BD2'][:].rearrange("p (h t) -> p h t", h=8), bmask3,
                 b['wi'][:].unsqueeze(1).to_broadcast([8, 8, 64]), ALU.mult, ['bmask', k('wi')], [k('BD2')])
            S.ts('dve', b['dg'][:], identf[0:8, 0:8], b['wi'][:, last:last + 1], ALU.mult, ['identf', k('wi')], [k('dg')])
            for j, nm_ in enumerate(('c', 'wk', 'wi', 'fl')):
                S.tr(b['pA'][0:64, j * 8:(j + 1) * 8], b[nm_][:], identf[0:8, 0:8], [k(nm_), 'identf'], [k('pA')], inc=False)
            S.mm(b['pA'][:, 32:40], ones8[:, :], b['dg'][:], True, True, ['ones8', k('dg')], [k('pA')])
            S.copy('act', b['tok'][si][:], b['pA'][0:64, 0:32], [k('pA')], [ks('tok')])
            S.copy('act', b['decb'][si][:], b['pA'][:, 32:40], [k('pA')], [ks('decb')])
            S.mm(b['pA'][0:64, :], ones8[:, 0:64], b['BD1'][:], True, False, ['ones8', k('BD1')], [k('pA')])
            S.mm(b['pA'][0:64, :], b['c'][:], bmask[:], False, False, [k('c'), 'bmask'], [k('pA')])
            S.mm(b['pA'][0:64, :], identf[0:64, 0:64], cm[d][:], False, True, ['identf', f'cm{d}'], [k('pA')])
            S.act(b['E'][:], b['pA'][0:64, :], AF.Exp, [k('pA')], [k('E')])
            for h in range(8):
                S.op('pe', lambda e, o=b['pA'][0:64, h * 64:(h + 1) * 64], l=b['kT'][:, h, cs], r=b['qT'][:, h, cs]:
                     e.matmul(o, l, r, start=True, stop=True), [k('kT'), k('qT')], [k('pA')], inc=(h == 7))
            S.tt('dve', b['S'][si][:], b['pA'][0:64, :], b['E'][:], ALU.mult, [k('pA'), k('E')], [ks('S')])
            S.mm(b['pA'][:, :], ones8[:, :], b['BD2'][:], True, True, ['ones8', k('BD2')], [k('pA')])
            S.tt('dve', b['qp'][si][:].rearrange("p (h t) -> p h t", h=8), b['qT'][:, :, cs],
                 b['pA'][:, :].rearrange("p (h t) -> p h t", h=8), ALU.mult, [k('qT'), k('pA')], [ks('qp')])
            S.tt('pool', b['kw'][si][:], b['k'][si][:], b['tok'][si][:, 8:16].unsqueeze(2).to_broadcast([64, 8, 128]), ALU.mult,
                 [ks('k'), ks('tok')], [ks('kw')])
            b['mi'] = 1 - b['mi']

        def post(d, u, si):
            q, g0, jc, first, lastc, newgrp = u
            b = B[d]
            r0 = g0 + jc * 64
            k = lambda n: K_(n, d)
            ks = lambda n: f'{n}{si}{d}'
            jo = b['ci']
            jn = 1 - jo
            ko = lambda n: f'{n}{jo}{d}'
            kn = lambda n: f'{n}{jn}{d}'
            if first:
                if q < 4:
                    S.memset('pool', b['Cn'][:], 0.0, [k('Cn')])
                    S.memset('pool', b['nst'][:], 0.0, [k('nst')])
                else:
                    S.dma('sp', b['Cn'][:], sC[d * 8:(d + 1) * 8].rearrange("h p v -> p h v"), [], [k('Cn')], K_('dst', d))
                    S.dma('sp', b['nst'][:], snl[:, d * 8:(d + 1) * 8], [], [k('nst')], K_('dst', d))
                S.copy('pool', b['Cnb'][jo][:], b['Cn'][:], [k('Cn')], [ko('Cnb')])
                S.copy('pool', b['nb'][jo][:], b['nst'][:], [k('nst')], [ko('nb')])
            for h in range(8):
                S.act(b['Cn'][:, h, :], b['Cn'][:, h, :], AF.Copy, [k('Cn'), ks('decb')], [k('Cn')], scale=b['decb'][si][:, h:h + 1])
            for h in range(8):
                S.op('pe', lambda e, o=b['pS'][:, 8 + h:9 + h], l=b['kw'][si][:, h, :], r=onesb[0:64, 0:1]:
                     e.matmul(o, l, r, start=True, stop=True), [ks('kw'), 'onesb'], [k('pSn')], inc=(h == 7))
            S.tt('dve', b['nst'][:], b['nst'][:], b['decb'][si][:], ALU.mult, [k('nst'), ks('decb')], [k('nst')])
            S.tt('dve', b['nst'][:], b['nst'][:], b['pS'][:, 8:16], ALU.add, [k('nst'), k('pSn')], [k('nst')])
            for pr in range(4):
                pz = b['pN'][pr % 2]
                pk = f'pN{pr % 2}{d}'
                for hh in range(2):
                    h = pr * 2 + hh
                    S.op('pe', lambda e, o=pz[:, hh * 256:(hh + 1) * 256], l=b['kw'][si][:, h, :], r=b['v'][si][:, h, 0:256]:
                         e.matmul(o, l, r, start=True, stop=True), [ks('kw'), ks('v')], [pk], inc=(hh == 1))
                S.tt('dve', b['Cn'][:, pr * 2:pr * 2 + 2, :], b['Cn'][:, pr * 2:pr * 2 + 2, :],
                     pz[:, :].rearrange("p (h v) -> p h v", h=2), ALU.add, [k('Cn'), pk], [k('Cn')])
            S.copy('pool', b['Cnb'][jn][:], b['Cn'][:], [k('Cn')], [kn('Cnb')])
            S.copy('pool', b['nb'][jn][:], b['nst'][:], [k('nst')], [kn('nb')])
            for h in range(8):
                hs = slice(h * 64, (h + 1) * 64)
                S.op('pe', lambda e, o=b['pS'][0:64, h:h + 1], l=b['S'][si][:, hs], r=onesb[0:64, 0:1]:
                     e.matmul(o, l, r, start=True, stop=False), [ks('S'), 'onesb'], [k('pSd')], inc=False)
                S.op('pe', lambda e, o=b['pS'][0:64, h:h + 1], l=b['qp'][si][:, hs], r=b['nb'][jo][:, h:h + 1]:
                     e.matmul(o, l, r, start=False, stop=True), [ks('qp'), ko('nb')], [k('pSd')], inc=(h == 7))
            S.act(b['dd'][:], b['pS'][0:64, 0:8], AF.Abs, [k('pSd')], [k('dd')])
            S.tt('dve', b['dd'][:], b['dd'][:], b['tok'][si][:, 24:32], ALU.max, [k('dd'), ks('tok')], [k('dd')])
            S.op('dve', lambda e, o=b['dd'][:]: e.reciprocal(out=o, in_=o), [k('dd')], [k('dd')])
            for pr in range(4):
                pz = b['pN'][pr % 2]
                pk = f'pN{pr % 2}{d}'
                for hh in range(2):
                    h = pr * 2 + hh
                    hs = slice(h * 64, (h + 1) * 64)
                    S.op('pe', lambda e, o=pz[0:64, hh * 256:(hh + 1) * 256], l=b['S'][si][:, hs], r=b['v'][si][:, h, 0:256]:
                         e.matmul(o, l, r, start=True, stop=False), [ks('S'), ks('v')], [pk], inc=False)
                    S.op('pe', lambda e, o=pz[0:64, hh * 256:(hh + 1) * 256], l=b['qp'][si][:, hs], r=b['Cnb'][jo][:, h, :]:
                         e.matmul(o, l, r, start=False, stop=True), [ks('qp'), ko('Cnb')], [pk], inc=(hh == 1))
                for hh in range(2):
                    h = pr * 2 + hh
                    S.act(b['Hc'][si][:, h, :], pz[0:64, hh * 256:(hh + 1) * 256], AF.Copy, [pk, k('dd')], [ks('Hc')],
                          scale=b['dd'][:, h:h + 1])
            S.dma('sp', hdl[d][r0:r0 + 64, :].rearrange("t (h v) -> t h v", h=8), b['Hc'][si][:], [ks('Hc')], ['hd_d'], 'dhd')
            b['ci'] = jn
            if lastc and q < 4:
                S.dma('sp', nC[q, d * 8:(d + 1) * 8].rearrange("h p v -> p h v"), b['Cn'][:], [k('Cn')], ['nC'], 'dout')
                S.dma('sp', nn[q, d * 8:(d + 1) * 8, :].rearrange("h p -> p h"), b['nst'][:], [k('nst')], ['nn'], 'dout', slow=True)
                S.dma('sp', nm[q, d, :].rearrange("(h o) -> h o", o=1), b['mfin'][q % 2][:], [f'mfin{q % 2}{d}'], ['nm'], 'dout')

        seqs = [(q, q * 256, 4) for q in range(4)] + [(4, NP, 64)]
        units = [[], []]
        for (q, base, nch) in seqs:
            ngr = nch // 4
            for d in range(2):
                for gi_ in range(ngr):
                    gg = gi_ if d == 0 else ngr - 1 - gi_
                    for jj in range(4):
                        jc = jj if d == 0 else 3 - jj
                        ci = gi_ * 4 + jj
                        units[d].append((q, base + gg * 256, jc, ci == 0, ci == nch - 1, jj == 0))
        nu = len(units[0])
        S.interleave([lambda d=d: pre(d, units[d][0], 0) for d in range(2)])
        for i in range(nu):
            fns = []
            if i + 1 < nu:
                fns += [lambda d=d, i=i: pre(d, units[d][i + 1], (i + 1) % 2) for d in range(2)]
            fns += [lambda d=d, i=i: post(d, units[d][i], i % 2) for d in range(2)]
            S.interleave(fns)
        S.flush()

    if STOP_AFTER <= 3:
        return nc, es, S

    with ExitStack() as st:
        HN = S.sb(st, [128, D], F32, "HN")
        load_bc(HN[:], head_norm[0:1, :], 'HN')
        gate = []
        for row in range(2):
            g_ = S.sb(st, [128, D], F32, f"gate{row}")
            load_bc(g_[:], mod[0, row:row + 1, 2 * D:3 * D], f'gate{row}')
            gate.append(g_)
        yT = S.sb(st, [128, KD, 1024], BF16, "yT")
        hf = S.sb(st, [128, D], F32, "hf")
        hb = S.sb(st, [128, D], F32, "hb")
        ogt = S.sb(st, [128, D], F32, "ogt")
        yb = [S.sb(st, [128, D], BF16, f"yb{i}") for i in range(2)]
        junk = S.sb(st, [128, 256], BF16, "junk4")
        ss8 = S.sb(st, [128, 8], F32, "ss8")
        ixw = S.sb(st, [128, 1], I32, "ixw")
        ixs = S.sb(st, [128, 1], I32, "ixs")
        ptr = [S.ps(st, [128, 1024], BF16, f"ptr4{i}") for i in range(2)]
        wo = [S.sb(st, [128, KD, 512], BF16, f"wo{i}") for i in range(2)]
        xblk = S.sb(st, [128, 8, D], F32, "xblk")
        po = [S.ps(st, [128, 512], F32, f"po4{i}") for i in range(2)]
        w_out_r = w_out.rearrange("(k p) n -> p k n", p=128)
        ti = 0
        wi_ = 0
        oi = 0

        def gath(dst, src2d, ix, rk, wk_):
            S.op('pool', lambda e, o=dst, s_=src2d, i_=ix: e.indirect_dma_start(
                out=o, out_offset=None, in_=s_, in_offset=bass.IndirectOffsetOnAxis(ap=i_, axis=0)),
                rk, wk_, dma='dg4')

        for blk in range(3):
            row = 0 if blk == 0 else 1
            for tl in range(8):
                b = ti % 2
                ti += 1
                if blk == 0:
                    r0 = tl * 128
                    S.dma('sp', hf[:], hdl[0][r0:r0 + 128, :], ['hd_d'], ['hf'], 'dhf')
                    S.dma('sp', hb[:], hdl[1][r0:r0 + 128, :], ['hd_d'], ['hb'], 'dhb')
                    S.dma('sp', ogt[:], og_d[r0:r0 + 128, :], ['qk_d'], ['ogt'], 'dog')
                    S.dma('sp', xblk[:, tl, :], xp[r0:r0 + 128, :], [], ['xblk'], 'dxb')
                else:
                    wt = (blk - 1) * 8 + tl
                    S.dma('sp', ixw[:], idx_win[wt], [], ['ixw'], 'dix')
                    S.dma('sp', ixs[:], idx_xs[wt], [], ['ixs'], 'dix')
                    gath(hf[:, :], hdl[0][:, :], ixw[:, :], ['ixw', 'hd_d'], ['hf'])
                    gath(hb[:, :], hdl[1][:, :], ixw[:, :], ['ixw', 'hd_d'], ['hb'])
                    gath(ogt[:, :], og_d[:, :], ixw[:, :], ['ixw', 'qk_d'], ['ogt'])
                    gath(xblk[:, tl, :], xs[:, :], ixs[:, :], ['ixs'], ['xblk'])
                S.tt('pool', hf[:], hf[:], hb[:], ALU.add, ['hf', 'hb'], ['hf'])
                for h in range(8):
                    S.act(junk[:], hf[:, h * 256:(h + 1) * 256], AF.Square, ['hf'], ['junk4', 'ss8'],
                          accum=ss8[:, h:h + 1])
                S.ts('dve', ss8[:], ss8[:], 1.0 / 256.0, ALU.mult, ['ss8'], ['ss8'], s2=EPS, op1=ALU.add)
                S.act(ss8[:], ss8[:], AF.Sqrt, ['ss8'], ['ss8'])
                S.op('dve', lambda e, o=ss8[:]: e.reciprocal(out=o, in_=o), ['ss8'], ['ss8'])
                S.tt('dve', hf[:].rearrange("p (h v) -> p h v", h=8), hf[:].rearrange("p (h v) -> p h v", h=8),
                     ss8[:].unsqueeze(2).to_broadcast([128, 8, 256]), ALU.mult, ['hf', 'ss8'], ['hf'])
                S.tt('pool', ogt[:], ogt[:], HN[:], ALU.mult, ['ogt', 'HN'], ['ogt'])
                S.tt('dve', yb[b][:], hf[:], ogt[:], ALU.mult, ['hf', 'ogt'], [f'yb{b}'])
                for half in range(2):
                    for kk in range(8):
                        kx = half * 8 + kk
                        S.tr(ptr[half][:, kk * 128:(kk + 1) * 128], yb[b][:, kx * 128:(kx + 1) * 128], identb[:],
                             [f'yb{b}', 'identb'], [f'ptr4{half}'], inc=(kk == 7))
                    S.copy('act' if half == 0 else 'dve', yT[:, half * 8:(half + 1) * 8, tl * 128:(tl + 1) * 128],
                           ptr[half][:].rearrange("p (k t) -> p k t", k=8), [f'ptr4{half}'], ['yT'])
            for n4 in range(4):
                wb = wi_ % 2
                wi_ += 1
                S.dma('pool', wo[wb][:], w_out_r[:, :, n4 * 512:(n4 + 1) * 512], [], [f'wo{wb}'], f'dwo{wb}')
                for tl in range(8):
                    ob = oi % 2
                    oi += 1
                    for kx in range(KD):
                        S.mm(po[ob][:, :], yT[:, kx, tl * 128:(tl + 1) * 128], wo[wb][:, kx, :], kx == 0, kx == KD - 1,
                             ['yT', f'wo{wb}'], [f'po4{ob}'])
                    S.tt('dve', po[ob][:, :], po[ob][:, :], gate[row][:, n4 * 512:(n4 + 1) * 512], ALU.mult,
                         [f'po4{ob}', f'gate{row}'], [f'po4{ob}'])
                    S.tt('dve', xblk[:, tl, n4 * 512:(n4 + 1) * 512], xblk[:, tl, n4 * 512:(n4 + 1) * 512], po[ob][:, :], ALU.add,
                         ['xblk', f'po4{ob}'], ['xblk'])
            for tl in range(8):
                r0 = blk * 1024 + tl * 128
                S.dma('sp', x1[r0:r0 + 128, :], xblk[:, tl, :], ['xblk'], ['x1'], 'dx1')
        S.flush()

    if STOP_AFTER <= 4:
        return nc, es, S

    def ffn_layer(l, x_rows, nblk, row_of_blk, experts, moe, out_rows, final):
        for blk in range(nblk):
            row = row_of_blk(blk)
            with ExitStack() as st0:
                hT = S.sb(st0, [128, KD, 1024], BF16, "hTf")
                comb = S.sb(st0, [128, 8, 8], F32, "comb")
                with ExitStack() as st:
                    G, Sh = make_GS(st, l, 1, norm_ffn[l:l + 1, :])
                    nb = NormBufs(st)
                    if moe:
                        rt = S.sb(st, [128, KD, 8], F32, "rt")
                        S.dma('sp', rt[:], router.rearrange("(k p) e -> p k e", p=128), [], ['rt'], 'dconst', slow=True)
                        h32T = S.sb(st, [128, KD, 128], F32, "h32T")
                        p32 = [S.ps(st, [128, 512], F32, f"p32{i}") for i in range(2)]
                        plg = S.ps(st, [128, 512], F32, "plg")
                        lg = S.sb(st, [128, 8], F32, "lg")
                        top = S.sb(st, [128, 8], F32, "top")
                        g12 = S.sb(st, [128, 2], F32, "g12")
                        eq = S.sb(st, [128, 8], F32, "eq")
                    for tl in range(8):
                        b = norm_tile(nb, x_rows(blk, tl), G[row], Sh[row], (hT, 'hTf'), tl * 128, keep32=moe)
                        if moe:
                            for qq in range(4):
                                pq = p32[qq % 2]
                                for kk in range(4):
                                    kx = qq * 4 + kk
                                    S.tr(pq[:, kk * 128:(kk + 1) * 128], nb.tmp[b][:, kx * 128:(kx + 1) * 128], identf[:],
                                         [f'ntmp{b}', 'identf'], [f'p32{qq % 2}'], inc=(kk == 3))
                                S.copy('act', h32T[:, qq * 4:(qq + 1) * 4, :], pq[:].rearrange("p (k t) -> p k t", k=4),
                                       [f'p32{qq % 2}'], ['h32T'])
                            for kx in range(KD):
                                S.mm(plg[:, 0:8], h32T[:, kx, :], rt[:, kx, :], kx == 0, kx == KD - 1, ['h32T', 'rt'], ['plg'])
                            S.copy('dve', lg[:], plg[:, 0:8], ['plg'], ['lg'])
                            S.op('dve', lambda e, o=top[:], i=lg[:]: e.max(out=o, in_=i), ['lg'], ['top'])
                            S.tt('dve', g12[:, 0:1], top[:, 0:1], top[:, 1:2], ALU.subtract, ['top'], ['g12'])
                            S.act(g12[:, 0:1], g12[:, 0:1], AF.Sigmoid, ['g12'], ['g12'])
                            S.ts('dve', g12[:, 1:2], g12[:, 0:1], -1.0, ALU.mult, ['g12'], ['g12'], s2=1.0, op1=ALU.add)
                            S.ts('dve', eq[:], lg[:], top[:, 0:1], ALU.is_equal, ['lg', 'top', 'g12'], ['eq'], s2=g12[:, 0:1], op1=ALU.mult)
                            S.ts('dve', comb[:, tl, :], lg[:], top[:, 1:2], ALU.is_equal, ['lg', 'top', 'g12'], ['comb'], s2=g12[:, 1:2], op1=ALU.mult)
                            S.tt('dve', comb[:, tl, :], comb[:, tl, :], eq[:], ALU.add, ['comb', 'eq'], ['comb'])
                    S.flush()
                yacc = S.sb(st0, [128, 8, D], F32, "yacc")
                with ExitStack() as st:
                    actb = S.sb(st, [128, 11, 1024], BF16, "actb")
                    w1c = [S.sb(st, [128, KD, 128], BF16, f"w1c{i}") for i in range(2)]
                    w3c = [S.sb(st, [128, KD, 128], BF16, f"w3c{i}") for i in range(2)]
                    w2p = [S.sb(st, [128, 11, 512], BF16, f"w2p{i}") for i in range(2)]
                    w2i = 0
                    sg = [S.sb(st, [128, 512], F32, f"sg{i}") for i in range(2)]
                    pa = [S.ps(st, [128, 512], F32, f"pfa{i}") for i in range(2)]
                    pb = [S.ps(st, [128, 512], F32, f"pfb{i}") for i in range(2)]
                    po = [S.ps(st, [128, 512], F32, f"pfo{i}") for i in range(2)]
                    wi_ = 0
                    pi_ = 0
                    oi = 0
                    first = True
                    for ei, (w1, w3, w2) in enumerate(experts):
                        w1r = w1.rearrange("(k p) n -> p k n", p=128)
                        w3r = w3.rearrange("(k p) n -> p k n", p=128)
                        w2r = w2.rearrange("(f p) n -> p f n", p=128)
                        for qd in range(4):
                            for fc in range(11):
                                f0 = (qd * 11 + fc) * 128
                                wb = wi_ % 2
                                wi_ += 1
                                S.dma('pool', w1c[wb][:], w1r[:, :, f0:f0 + 128], [], [f'w1c{wb}'], f'dw1{wb}')
                                S.dma('pool', w3c[wb][:], w3r[:, :, f0:f0 + 128], [], [f'w3c{wb}'], f'dw3{wb}')
                                for tb in range(2):
                                    p_ = pi_ % 2
                                    pi_ += 1
                                    for kx in range(KD):
                                        S.mm(pa[p_][:, :], w1c[wb][:, kx, :], hT[:, kx, tb * 512:(tb + 1) * 512], kx == 0, kx == KD - 1,
                                             [f'w1c{wb}', 'hTf'], [f'pfa{p_}'])
                                    for kx in range(KD):
                                        S.mm(pb[p_][:, :], w3c[wb][:, kx, :], hT[:, kx, tb * 512:(tb + 1) * 512], kx == 0, kx == KD - 1,
                                             [f'w3c{wb}', 'hTf'], [f'pfb{p_}'])
                                    S.act(sg[p_][:], pa[p_][:, :], AF.Silu, [f'pfa{p_}'], [f'sg{p_}'])
                                    S.tt('dve', actb[:, fc, tb * 512:(tb + 1) * 512], sg[p_][:], pb[p_][:, :], ALU.mult,
                                         [f'sg{p_}', f'pfb{p_}'], ['actb'])
                            for n4 in range(4):
                                w2b = w2i % 2
                                w2i += 1
                                S.dma('pool', w2p[w2b][:], w2r[:, qd * 11:(qd + 1) * 11, n4 * 512:(n4 + 1) * 512], [], [f'w2p{w2b}'], f'dw2p{w2b}')
                                for tl in range(8):
                                    ob = oi % 2
                                    oi += 1
                                    for fc in range(11):
                                        S.mm(po[ob][:, :], actb[:, fc, tl * 128:(tl + 1) * 128], w2p[w2b][:, fc, :], fc == 0, fc == 10,
                                             ['actb', f'w2p{w2b}'], [f'pfo{ob}'])
                                    ya = yacc[:, tl, n4 * 512:(n4 + 1) * 512]
                                    if moe:
                                        if first:
                                            S.ts('dve', ya, po[ob][:, :], comb[:, tl, ei:ei + 1], ALU.mult, [f'pfo{ob}', 'comb'], ['yacc'])
                                        else:
                                            S.stt(ya, po[ob][:, :], comb[:, tl, ei:ei + 1], ya, ALU.mult, ALU.add,
                                                  [f'pfo{ob}', 'comb', 'yacc'], ['yacc'])
                                    else:
                                        if first:
                                            S.copy('act', ya, po[ob][:, :], [f'pfo{ob}'], ['yacc'])
                                        else:
                                            S.tt('dve', ya, ya, po[ob][:, :], ALU.add, [f'pfo{ob}', 'yacc'], ['yacc'])
                            first = False
                    S.flush()
                with ExitStack() as st:
                    gt_ = S.sb(st, [128, D], F32, "gateR")
                    load_bc(gt_[:], mod[l, row:row + 1, 5 * D:6 * D], 'gateR')
                    xr = [S.sb(st, [128, D], F32, f"xr{i}") for i in range(2)]
                    if final:
                        NF = S.sb(st, [128, D], F32, "NF")
                        load_bc(NF[:], norm_final[0:1, :], 'NF')
                        junkf = S.sb(st, [128, D], BF16, "junkf")
                        ssf = [S.sb(st, [128, 1], F32, f"ssf{i}") for i in range(2)]
                    for tl in range(8):
                        b = tl % 2
                        S.dma('sp', xr[b][:], x_rows(blk, tl), [], [f'xr{b}'], f'dxr{b}')
                        S.tt('pool', yacc[:, tl, :], yacc[:, tl, :], gt_[:], ALU.mult, ['yacc', 'gateR'], ['yacc'])
                        S.tt('dve', xr[b][:], xr[b][:], yacc[:, tl, :], ALU.add, [f'xr{b}', 'yacc'], [f'xr{b}'])
                        if final:
                            S.act(junkf[:], xr[b][:], AF.Square, [f'xr{b}'], ['junkf', f'ssf{b}'], accum=ssf[b][:])
                            S.ts('dve', ssf[b][:], ssf[b][:], 1.0 / D, ALU.mult, [f'ssf{b}'], [f'ssf{b}'], s2=EPS, op1=ALU.add)
                            S.act(ssf[b][:], ssf[b][:], AF.Sqrt, [f'ssf{b}'], [f'ssf{b}'])
                            S.op('dve', lambda e, o=ssf[b][:]: e.reciprocal(out=o, in_=o), [f'ssf{b}'], [f'ssf{b}'])
                            S.stt(xr[b][:], xr[b][:], ssf[b][:, 0:1], NF[:], ALU.mult, ALU.mult, [f'xr{b}', f'ssf{b}', 'NF'], [f'xr{b}'])
                        S.dma('sp', out_rows(blk, tl), xr[b][:], [f'xr{b}'], ['xout'], 'dxout')
                    S.flush()

    ffn_layer(0, lambda blk, tl: x1[blk * 1024 + tl * 128: blk * 1024 + (tl + 1) * 128, :], 3,
              lambda blk: 0 if blk == 0 else 1, [(ffn_w1, ffn_w3, ffn_w2)], False,
              lambda blk, tl: x2[blk * 1024 + tl * 128: blk * 1024 + (tl + 1) * 128, :], False)
    if STOP_AFTER <= 5:
        return nc, es, S

    with ExitStack() as st:
        hall = S.sb(st, [128, 16, D], BF16, "hall")
        PW = S.sb(st, [128, 16, 512], BF16, "PW")
        S.dma('pool', PW[:], pool_w.rearrange("g (c p) n -> p (g c) n", p=128), [], ['PW'], 'dconst2')
        pmt = S.sb(st, [128, 20, 128], BF16, "pmt")
        ict = S.sb(st, [128, 4, 128], F32, "ict")
        dT = S.sb(st, [128, 16, 128], BF16, "dT")
        psg = S.sb(st, [128, D], F32, "psg")
        gt = S.sb(st, [128, D], F32, "gt6")
        Gt = S.sb(st, [128, D], F32, "G6")
        St = S.sb(st, [128, D], F32, "S6")
        xin = S.sb(st, [128, D], F32, "xin6")
        junk = S.sb(st, [128, D], BF16, "junk6")
        ss = S.sb(st, [128, 1], F32, "ss6")
        tmp = S.sb(st, [128, D], F32, "tmp6")
        idx = S.sb(st, [128, 1], I32, "idx6")
        pd = [S.ps(st, [128, 512], F32, f"pd{i}") for i in range(2)]
        pq = [S.ps(st, [128, 512], F32, f"pq{i}") for i in range(2)]
        load_bc(gt[:], norm_mix[1:2, :], 'gt6')

        def norm6(dst_tile_idx):
            S.act(junk[:], xin[:], AF.Square, ['xin6'], ['junk6', 'ss6'], accum=ss[:])
            S.ts('dve', ss[:], ss[:], 1.0 / D, ALU.mult, ['ss6'], ['ss6'], s2=EPS, op1=ALU.add)
            S.act(ss[:], ss[:], AF.Sqrt, ['ss6'], ['ss6'])
            S.op('dve', lambda e, o=ss[:]: e.reciprocal(out=o, in_=o), ['ss6'], ['ss6'])
            S.stt(tmp[:], xin[:], ss[:, 0:1], Gt[:], ALU.mult, ALU.mult, ['xin6', 'ss6', 'G6'], ['tmp6'])
            S.tt('pool', hall[:, dst_tile_idx, :], tmp[:], St[:], ALU.add, ['tmp6', 'S6'], ['hall'])

        pi_ = 0
        for grp in range(2):
            row = grp
            load_bc(Gt[:], mod[1, row:row + 1, D:2 * D], 'G6')
            load_bc(St[:], mod[1, row:row + 1, 0:D], 'S6')
            S.stt(Gt[:], Gt[:], 1.0, gt[:], ALU.add, ALU.mult, ['G6', 'gt6'], ['G6'])
            load_bc(psg[:], mod[1, row:row + 1, 2 * D:3 * D], 'psg')
            load_bc(tmp[:], pool_scale[0:1, :], 'tmp6')
            S.tt('dve', psg[:], psg[:], tmp[:], ALU.mult, ['psg', 'tmp6'], ['psg'])
            ntile = 8 if grp == 0 else 16
            for t_ in range(ntile):
                if grp == 0:
                    S.dma('sp', xin[:], x2[t_ * 128:(t_ + 1) * 128, :], ['xout'], ['xin6'], 'dxin6')
                else:
                    S.dma('sp', xin[:], x2[NP + t_ * 128:NP + (t_ + 1) * 128, :], ['xout'], ['xin6'], 'dxin6')
                norm6(t_)
            for j in range(8):
                if grp == 0:
                    jo = j % 2
                    for g in range(4):
                        S.dma('pool', pmt[:, g * 2:(g + 1) * 2, :], pm_p[(g * 2 + jo) * 2:(g * 2 + jo) * 2 + 2].rearrange("m p t -> p m t"),
                              [], ['pmt'], 'dpmt')
                        S.dma('sp', ict[:, g, :], ic_p[g * 2 + jo:g * 2 + jo + 1, :].partition_broadcast(128), [], ['ict'], 'dict')
                    S.dma('sp', xin[:], x2[j * 128:(j + 1) * 128, :], ['xout'], ['xin6'], 'dxin6')
                else:
                    mi = 0
                    for g in range(4):
                        nm_ = (3, 3, 5, 9)[g]
                        off = sum((3, 3, 5, 9)[:g])
                        S.dma('pool', pmt[:, off:off + nm_, :], pm_s[g_off(g) + j * nm_: g_off(g) + (j + 1) * nm_].rearrange("m p t -> p m t"),
                              [], ['pmt'], 'dpmt')
                        S.dma('sp', ict[:, g, :], ic_s[g * 8 + j:g * 8 + j + 1, :].partition_broadcast(128), [], ['ict'], 'dict')
                    S.dma('sp', xin[:], x2[NP + (j + 4) * 128:NP + (j + 5) * 128, :], ['xout'], ['xin6'], 'dxin6')
                for g in range(4):
                    p_ = pi_ % 2
                    pi_ += 1
                    if grp == 0:
                        srcs = [((j // 2) * 2 + ji, g * 2 + ji) for ji in range(2)]
                    else:
                        hw_ = (1, 1, 2, 4)[g]
                        off = sum((3, 3, 5, 9)[:g])
                        srcs = [(j + 4 + di, off + di + hw_) for di in range(-hw_, hw_ + 1)]
                    for cc in range(4):
                        for si, (ti_, mi_) in enumerate(srcs):
                            S.op('pe', lambda e, o=pd[p_][:, cc * 128:(cc + 1) * 128], l=hall[:, ti_, g * 512 + cc * 128: g * 512 + (cc + 1) * 128], r=pmt[:, mi_, :],
                                 a=(si == 0), z=(si == len(srcs) - 1): e.matmul(o, l, r, start=a, stop=z),
                                 ['hall', 'pmt'], [f'pd{p_}'], inc=(cc == 3 and si == len(srcs) - 1))
                    S.tt('dve', dT[:, g * 4:(g + 1) * 4, :], pd[p_][:, :].rearrange("p (c t) -> p c t", c=4),
                         ict[:, g, :].unsqueeze(1).to_broadcast([128, 4, 128]), ALU.mult, [f'pd{p_}', 'ict'], ['dT'])
                for g in range(4):
                    p_ = pi_ % 2
                    pi_ += 1
                    for cc in range(4):
                        S.mm(pq[p_][:, :], dT[:, g * 4 + cc, :], PW[:, g * 4 + cc, :], cc == 0, cc == 3, ['dT', 'PW'], [f'pq{p_}'])
                    S.tt('dve', tmp[:, g * 512:(g + 1) * 512], pq[p_][:, :], psg[:, g * 512:(g + 1) * 512], ALU.mult,
                         [f'pq{p_}', 'psg'], ['tmp6'])
                S.tt('pool', xin[:], xin[:], tmp[:], ALU.add, ['xin6', 'tmp6'], ['xin6'])
                r0 = grp * 1024 + j * 128
                S.dma('sp', x3[r0:r0 + 128, :], xin[:], ['xin6'], ['x3'], 'dx3')
        S.flush()
    if STOP_AFTER <= 6:
        return nc, es, S

    ffn_layer(1, lambda blk, tl: x3[blk * 1024 + tl * 128: blk * 1024 + (tl + 1) * 128, :], 2,
              lambda blk: blk, [(moe_w1[e], moe_w3[e], moe_w2[e]) for e in range(8)], True,
              lambda blk, tl: (yp if blk == 0 else ys)[tl * 128:(tl + 1) * 128, :], True)
    return nc, es, S


DBG_OUT = set()
STOP_AFTER = 99


def g_off(g):
    return 8 * sum((3, 3, 5, 9)[:g])


def _pool_consts(p):
    W = (2, 4, 8, 16)
    HW = (1, 1, 2, 4)
    pm_s = np.zeros((160, 128, 128), np.float32)
    ic_s = np.zeros((32, 128), np.float32)
    mi = 0
    for g, w in enumerate(W):
        for j in range(8):
            cnt = np.zeros(128, np.float32)
            mats = {}
            for to in range(128):
                r = 16 * p + 2 * j + to // 64
                cc = to % 64
                rlo, rhi = max(r - w // 2, 0), min(r + w // 2, 64)
                clo, chi = max(cc - w // 2, 0), min(cc + w // 2, 64)
                cnt[to] = (rhi - rlo) * (chi - clo)
                for rr in range(rlo, rhi):
                    wt = (rr - (16 * p - 8)) // 2
                    m = mats.setdefault(wt, np.zeros((128, 128), np.float32))
                    ti0 = ((rr - (16 * p - 8)) % 2) * 64
                    m[ti0 + clo:ti0 + chi, to] += 1.0
                wt_self = j + 4
                m = mats.setdefault(wt_self, np.zeros((128, 128), np.float32))
                m[to, to] -= cnt[to]
            for di in range(-HW[g], HW[g] + 1):
                wt = j + 4 + di
                if wt in mats:
                    pm_s[mi] = mats[wt]
                mi += 1
            ic_s[g * 8 + j] = 1.0 / cnt
    assert mi == 160
    pm_p = np.zeros((16, 128, 128), np.float32)
    ic_p = np.zeros((8, 128), np.float32)
    for g, w in enumerate(W):
        for jo in range(2):
            for to in range(128):
                t = jo * 128 + to
                lo, hi = max(t - w // 2, 0), min(t + w // 2, 256)
                ic_p[g * 2 + jo, to] = 1.0 / (hi - lo)
                for tt in range(lo, hi):
                    pm_p[(g * 2 + jo) * 2 + tt // 128, tt % 128, to] += 1.0
                pm_p[(g * 2 + jo) * 2 + jo, to, to] -= (hi - lo)
    return pm_s, ic_s, pm_p, ic_p


def _core_inputs(c, inp):
    s, p = c // 4, c % 4
    f = np.float32
    m = {}
    m["xp"] = np.ascontiguousarray(inp["x_prompt"][4 * c:4 * c + 4].reshape(NP, D))
    m["xs"] = np.ascontiguousarray(inp["x_sample"][s])
    m["sC"] = np.ascontiguousarray(inp["state_mlstm_C"][s, 0].reshape(16, 128, 256))
    m["snl"] = np.ascontiguousarray(inp["state_mlstm_n"][s, 0].reshape(16, 128).T)
    m["sml"] = np.ascontiguousarray(inp["state_mlstm_m"][s, 0].reshape(2, 8, 1))
    c2 = np.stack([inp["c_ctx"], inp["c"][s]], axis=-1).reshape(KD, 128, 2)
    m["c2"] = np.ascontiguousarray(c2.transpose(1, 0, 2))
    m["w_ada"] = inp["w_ada"]
    m["b_ada"] = inp["b_ada"]
    m["norm_mix"] = inp["norm_mix"]
    m["norm_ffn"] = inp["norm_ffn"]
    m["norm_final"] = inp["norm_final"].reshape(1, D)
    m["w_in"] = inp["mlstm_w_in"][0]
    m["bgl"] = np.ascontiguousarray(inp["mlstm_b_gates"][0].reshape(4, 8).T)
    m["head_norm"] = inp["mlstm_head_norm"].reshape(1, D)
    m["w_out"] = inp["mlstm_w_out"][0]
    m["pool_w"] = inp["pool_w"][0]
    m["pool_scale"] = inp["pool_scale"].reshape(1, D)
    m["ffn_w1"] = inp["ffn_w1"][0]
    m["ffn_w3"] = inp["ffn_w3"][0]
    m["ffn_w2"] = inp["ffn_w2"][0]
    m["router"] = inp["moe_router"][0]
    m["moe_w1"] = inp["moe_w1"][0]
    m["moe_w3"] = inp["moe_w3"][0]
    m["moe_w2"] = inp["moe_w2"][0]
    m["identf"] = np.eye(128, dtype=f)
    cm = np.zeros((2, 64, 64), f)
    si, ti = np.meshgrid(np.arange(64), np.arange(64), indexing="ij")
    cm[0][si > ti] = NEG
    cm[1][si < ti] = NEG
    m["cmask"] = np.ascontiguousarray(np.tile(cm, (1, 1, 8)))
    bm = np.zeros((8, 8, 64), f)
    for h in range(8):
        bm[h, h, :] = 1.0
    m["bmask"] = bm.reshape(8, 512)
    iw = np.zeros((16, 128, 1), np.int32)
    for wt in range(16):
        for i in range(128):
            r = 16 * p - 8 + 2 * wt + i // 64
            r = min(max(r, 0), 63)
            iw[wt, i, 0] = NP + r * 64 + i % 64
    m["idx_win"] = iw
    m["idx_xs"] = iw - NP
    pm_s, ic_s, pm_p, ic_p = _pool_consts(p)
    m["pm_s"], m["ic_s"], m["pm_p"], m["ic_p"] = pm_s, ic_s, pm_p, ic_p
    return {k: np.ascontiguousarray(v) for k, v in m.items()}


def kernel(**inputs):
    inp = {k: np.asarray(v) for k, v in inputs.items()}
    nc, es, S = build_program()
    in_maps = [_core_inputs(c, inp) for c in range(8)]
    res = run_bass_kernel_spmd(nc, in_maps, core_ids=list(range(8)))
    r = res.results
    y_prompt = np.concatenate([r[c]["yp"] for c in range(8)], 0).reshape(32, 256, D).astype(np.float32)
    y_sample = np.stack([np.concatenate([r[4 * s + p]["ys"] for p in range(4)], 0) for s in range(2)], 0).astype(np.float32)
    new_C = np.concatenate([r[c]["nC"] for c in range(8)], 0).reshape(32, 1, 2, 8, 128, 256).astype(np.float32)
    new_n = np.concatenate([r[c]["nn"] for c in range(8)], 0).reshape(32, 1, 2, 8, 128).astype(np.float32)
    new_m = np.concatenate([r[c]["nm"] for c in range(8)], 0).reshape(32, 1, 2, 8).astype(np.float32)
    return (y_prompt, y_sample, new_C, new_n, new_m)
```

```python
import numpy as np
from contextlib import ExitStack
import concourse.bass as bass
import concourse.mybir as mybir
from concourse.bass_utils import run_bass_kernel_spmd

F32 = mybir.dt.float32
BF16 = mybir.dt.bfloat16
I32 = mybir.dt.int32
AF = mybir.ActivationFunctionType
ALU = mybir.AluOpType

D = 2048
KD = 16
DFF = 5632
NFC = 44
NH = 8
NP = 1024
NS = 4096
T0 = NP + NS
T1 = 2048
EPS = 1e-6
NEG = -30000.0
ENG = ('pe', 'act', 'dve', 'pool', 'sp')


class Sched:
    def __init__(self, nc, es):
        self.nc = nc
        self.es = es
        self.eobj = {'pe': nc.tensor, 'act': nc.scalar, 'dve': nc.vector, 'pool': nc.gpsimd, 'sp': nc.sync}
        self.ops = {e: [] for e in ENG}
        self.cnt = {e: 0 for e in ENG}
        self.dcnt = {}
        self.seen = {e: {} for e in ENG}
        self.lastw = {}
        self.readers = {}
        self.sems = {}
        self.pend = {e: ([], []) for e in ENG}
        self.nsb = 0
        self.capture = None

    def sem(self, name):
        if name not in self.sems:
            self.sems[name] = self.es.enter_context(self.nc.semaphore(name))
        return self.sems[name]

    def op(self, eng, fn, reads=(), writes=(), dma=None, inc=True):
        if self.capture is not None:
            self.capture.append((eng, fn, tuple(reads), tuple(writes), dma, inc))
            return None
        waits = {}

        def need(tok):
            s, v = tok
            if waits.get(s, 0) < v:
                waits[s] = v
        for b in reads:
            if b in self.lastw:
                need(self.lastw[b])
        for b in writes:
            if b in self.lastw:
                need(self.lastw[b])
            for r in self.readers.get(b, ()):
                need(r)
        wl = []
        for s, v in waits.items():
            if s in self.dcnt:
                v = 16 * self.dcnt[s]
            if self.seen[eng].get(s, 0) < v:
                self.seen[eng][s] = v
                wl.append((s, v))
        tok = None
        incspec = None
        if dma is not None:
            self.dcnt[dma] = self.dcnt.get(dma, 0) + 1
            tok = (dma, 16 * self.dcnt[dma])
            incspec = (dma, 16)
        elif inc:
            self.cnt[eng] += 1
            tok = ('E' + eng, self.cnt[eng])
            incspec = ('E' + eng, 1)
        self.ops[eng].append((wl, fn, incspec))
        pr, pw = self.pend[eng]
        if tok is None:
            pr.extend(reads)
            pw.extend(writes)
        else:
            if dma is None:
                allr = list(reads) + pr
                allw = list(writes) + pw
                self.pend[eng] = ([], [])
            else:
                allr, allw = reads, writes
            for b in allw:
                self.lastw[b] = tok
                self.readers[b] = []
            for b in allr:
                self.readers.setdefault(b, []).append(tok)
        return tok

    def interleave(self, fns):
        lists = []
        for f in fns:
            self.capture = []
            f()
            lists.append(self.capture)
        self.capture = None
        n = max(len(l) for l in lists) if lists else 0
        for i in range(n):
            for l in lists:
                if i < len(l):
                    self.op(*l[i])

    def flush(self, final=False):
        nc = self.nc
        for s in list(self.dcnt) + ['E' + e for e in ENG]:
            self.sem(s)
        tails = {}
        for e in ENG:
            tl = []
            for e2 in ENG:
                if e2 != e and self.cnt[e2] > self.seen[e].get('E' + e2, 0):
                    tl.append(('E' + e2, self.cnt[e2]))
                    self.seen[e]['E' + e2] = self.cnt[e2]
            for s, c in self.dcnt.items():
                if 16 * c > self.seen[e].get(s, 0):
                    tl.append((s, 16 * c))
                    self.seen[e][s] = 16 * c
            tails[e] = tl
        ops = self.ops
        sems = self.sems

        def run(e, name):
            for wl, fn, incspec in ops[name]:
                for s, v in wl:
                    e.wait_ge(sems[s], v)
                ins = fn(e)
                if incspec is not None:
                    ins.then_inc(sems[incspec[0]], incspec[1])
            for s, v in tails[name]:
                e.wait_ge(sems[s], v)
        with nc.Block() as block:
            @block.tensor
            def _(e):
                run(e, 'pe')

            @block.scalar
            def _(e):
                run(e, 'act')

            @block.vector
            def _(e):
                run(e, 'dve')

            @block.gpsimd
            def _(e):
                run(e, 'pool')

            @block.sync
            def _(e):
                run(e, 'sp')
        self.ops = {e: [] for e in ENG}

    def sb(self, st, shape, dt, name=None):
        self.nsb += 1
        return st.enter_context(self.nc.sbuf_tensor(f"{name or 't'}_{self.nsb}", list(shape), dt))

    def ps(self, st, shape, dt, name=None):
        self.nsb += 1
        return st.enter_context(self.nc.psum_tensor(f"{name or 'p'}_{self.nsb}", list(shape), dt))

    def dma(self, q, out, in_, reads, writes, sem, slow=False):
        if slow:
            return self.op(q, lambda e, o=out, i=in_: e.dma_start(out=o, in_=i, allow_slow_non_contiguous=True),
                           reads, writes, dma=sem)
        return self.op(q, lambda e, o=out, i=in_: e.dma_start(out=o, in_=i), reads, writes, dma=sem)

    def mm(self, out, lhsT, rhs, start, stop, reads, writes):
        return self.op('pe', lambda e, o=out, l=lhsT, r=rhs, a=start, b=stop: e.matmul(o, l, r, start=a, stop=b),
                       reads, writes, inc=stop)

    def tr(self, out, in_, ident, reads, writes, inc=True):
        return self.op('pe', lambda e, o=out, i=in_, d=ident: e.transpose(o, i, d), reads, writes, inc=inc)

    def act(self, out, in_, func, reads, writes, bias=None, scale=None, accum=None, eng='act'):
        def f(e, o=out, i=in_, fu=func, b=bias, s=scale, a=accum):
            kw = {}
            if b is not None:
                kw['bias'] = b
            if s is not None:
                kw['scale'] = s
            if a is not None:
                kw['accum_out'] = a
            return e.activation(o, i, fu, **kw)
        return self.op('act', f, reads, writes)

    def tt(self, eng, out, in0, in1, op, reads, writes):
        return self.op(eng, lambda e, o=out, a=in0, b=in1, p=op: e.tensor_tensor(out=o, in0=a, in1=b, op=p), reads, writes)

    def ts(self, eng, out, in0, s1, op0, reads, writes, s2=None, op1=None):
        def f(e, o=out, a=in0, x1=s1, x2=s2, p0=op0, p1=op1):
            if p1 is None:
                return e.tensor_scalar(out=o, in0=a, scalar1=x1, scalar2=None, op0=p0)
            return e.tensor_scalar(out=o, in0=a, scalar1=x1, scalar2=x2, op0=p0, op1=p1)
        return self.op(eng, f, reads, writes)

    def stt(self, out, in0, scalar, in1, op0, op1, reads, writes):
        return self.op('dve', lambda e, o=out, a=in0, s=scalar, b=in1, p0=op0, p1=op1:
                       e.scalar_tensor_tensor(out=o, in0=a, scalar=s, in1=b, op0=p0, op1=p1), reads, writes)

    def copy(self, eng, out, in_, reads, writes):
        if eng == 'act':
            return self.op('act', lambda e, o=out, i=in_: e.copy(o, i), reads, writes)
        return self.op(eng, lambda e, o=out, i=in_: e.tensor_copy(out=o, in_=i), reads, writes)

    def memset(self, eng, ap, val, writes):
        return self.op(eng, lambda e, a=ap, v=val: e.memset(a, v), (), writes)


def build_program(dbg=False):
    nc = bass.Bass("TRN2", target_bir_lowering=False)
    es = ExitStack()
    S = Sched(nc, es)

    def din(name, shape, dt=F32):
        return nc.dram_tensor(name, list(shape), dt, kind="ExternalInput").ap()

    def dout(name, shape, dt=F32):
        return nc.dram_tensor(name, list(shape), dt, kind="ExternalOutput").ap()

    def dscr(name, shape, dt=F32):
        kind = "ExternalOutput" if (dbg and name in DBG_OUT) else "Internal"
        return nc.dram_tensor(name, list(shape), dt, kind=kind).ap()

    xp = din("xp", [NP, D])
    xs = din("xs", [NS, D])
    sC = din("sC", [16, 128, 256])
    snl = din("snl", [128, 16])
    sml = din("sml", [2, 8, 1])
    c2 = din("c2", [128, KD, 2])
    w_ada = din("w_ada", [2, D, 6 * D])
    b_ada = din("b_ada", [2, 6 * D])
    norm_mix = din("norm_mix", [2, D])
    norm_ffn = din("norm_ffn", [2, D])
    norm_final = din("norm_final", [1, D])
    w_in = din("w_in", [D, 6176])
    bgl = din("bgl", [8, 4])
    head_norm = din("head_norm", [1, D])
    w_out = din("w_out", [D, D])
    pool_w = din("pool_w", [4, 512, 512])
    pool_scale = din("pool_scale", [1, D])
    ffn_w1 = din("ffn_w1", [D, DFF])
    ffn_w3 = din("ffn_w3", [D, DFF])
    ffn_w2 = din("ffn_w2", [DFF, D])
    router = din("router", [D, 8])
    moe_w1 = din("moe_w1", [8, D, DFF])
    moe_w3 = din("moe_w3", [8, D, DFF])
    moe_w2 = din("moe_w2", [8, DFF, D])
    identf_d = din("identf", [128, 128])
    cmask_d = din("cmask", [2, 64, 512])
    bmask_d = din("bmask", [8, 512])
    idx_win = din("idx_win", [16, 128, 1], I32)
    idx_xs = din("idx_xs", [16, 128, 1], I32)
    pm_s = din("pm_s", [160, 128, 128])
    ic_s = din("ic_s", [32, 128])
    pm_p = din("pm_p", [16, 128, 128])
    ic_p = din("ic_p", [8, 128])

    yp = dout("yp", [NP, D])
    ys = dout("ys", [1024, D])
    nC = dout("nC", [4, 16, 128, 256])
    nn = dout("nn", [4, 16, 128])
    nm = dout("nm", [4, 2, 8])

    mod = dscr("mod", [2, 2, 6 * D])
    qT_d = dscr("qT_d", [NH, 128, T0], BF16)
    kT_d = dscr("kT_d", [NH, 128, T0], BF16)
    k_d = dscr("k_d", [T0, 1024], BF16)
    v_d = dscr("v_d", [T0, D], BF16)
    og_d = dscr("og_d", [T0, D])
    gi_d = dscr("gi_d", [4, 8, T0])
    hdl = [dscr("hd0", [T0, D]), dscr("hd1", [T0, D])]
    TW = NP + 2048
    x1 = dscr("x1", [TW, D])
    x2 = dscr("x2", [TW, D])
    x3 = dscr("x3", [T1, D])

    identf = S.sb(es, [128, 128], F32, "identf")
    identb = S.sb(es, [128, 128], BF16, "identb")
    S.dma('sp', identf[:], identf_d[:, :], [], ['identf'], 'dconst')
    S.copy('dve', identb[:], identf[:], ['identf'], ['identb'])

    rowsel = {0: 0, 1: 1}

    def load_bc(tile_ap, src_row_ap, key, sem='dmod'):
        S.dma('sp', tile_ap, src_row_ap.partition_broadcast(128), [], [key], sem)

    with ExitStack() as st:
        c2t = S.sb(st, [128, KD, 2], F32)
        c2s = S.sb(st, [128, KD, 2], BF16)
        S.dma('sp', c2t[:], c2[:, :, :], [], ['c2t'], 'dconst')
        S.act(c2s[:], c2t[:], AF.Silu, ['c2t'], ['c2s'])
        wa = [S.sb(st, [128, KD, 512], BF16, f"wa{i}") for i in range(2)]
        bt = [S.sb(st, [2, 512], F32, f"bt{i}") for i in range(2)]
        mo = [S.sb(st, [2, 512], F32, f"mo{i}") for i in range(2)]
        pa = [S.ps(st, [128, 512], F32, f"pa{i}") for i in range(2)]
        it = 0
        for l in range(2):
            wl = w_ada[l].rearrange("(k p) n -> p k n", p=128)
            for nci in range(24):
                b = it % 2
                it += 1
                n0 = nci * 512
                S.dma('pool', wa[b][:], wl[:, :, n0:n0 + 512], [], [f'wa{b}'], f'dwa{b}')
                S.dma('sp', bt[b][:], b_ada[l:l + 1, n0:n0 + 512].partition_broadcast(2), [], [f'bt{b}'], f'dbt{b}')
                for k in range(KD):
                    S.mm(pa[b][0:2, :], c2s[:, k, :], wa[b][:, k, :], k == 0, k == KD - 1,
                         ['c2s', f'wa{b}'], [f'pa{b}'])
                S.tt('dve', mo[b][:], pa[b][0:2, :], bt[b][:], ALU.add, [f'pa{b}', f'bt{b}'], [f'mo{b}'])
                S.dma('sp', mod[l, :, n0:n0 + 512], mo[b][:], [f'mo{b}'], ['mod'], 'dmodw')
        S.flush()

    def make_GS(st, l, which, gvec_ap):
        base = 0 if which == 0 else 3
        gt = S.sb(st, [128, D], F32, "gt")
        load_bc(gt[:], gvec_ap, 'gt')
        G, Sh = [], []
        for row in range(2):
            g_ = S.sb(st, [128, D], F32, f"G{row}")
            s_ = S.sb(st, [128, D], F32, f"S{row}")
            load_bc(g_[:], mod[l, row:row + 1, (base + 1) * D:(base + 2) * D], f'G{row}')
            load_bc(s_[:], mod[l, row:row + 1, base * D:(base + 1) * D], f'S{row}')
            S.stt(g_[:], g_[:], 1.0, gt[:], ALU.add, ALU.mult, [f'G{row}', 'gt'], [f'G{row}'])
            G.append(g_)
            Sh.append(s_)
        return G, Sh

    class NormBufs:
        def __init__(self, st):
            self.xin = [S.sb(st, [128, D], F32, f"xin{i}") for i in range(2)]
            self.junk = S.sb(st, [128, D], BF16, "junk")
            self.ss = [S.sb(st, [128, 1], F32, f"ss{i}") for i in range(2)]
            self.tmp = [S.sb(st, [128, D], F32, f"ntmp{i}") for i in range(2)]
            self.h = [S.sb(st, [128, D], BF16, f"nh{i}") for i in range(2)]
            self.ptr = [S.ps(st, [128, 1024], BF16, f"ptr{i}") for i in range(2)]
            self.n = 0

    def norm_tile(nb, x_src_ap, G, Sh, hT_dst, col0, loaded=False, xkey=None, keep32=False):
        b = nb.n % 2
        nb.n += 1
        if not loaded:
            S.dma('sp', nb.xin[b][:], x_src_ap, [xkey] if xkey else [], [f'xin{b}'], f'dxin{b}')
        S.act(nb.junk[:], nb.xin[b][:], AF.Square, [f'xin{b}'], ['junk', f'ss{b}'], accum=nb.ss[b][:])
        S.ts('dve', nb.ss[b][:], nb.ss[b][:], 1.0 / D, ALU.mult, [f'ss{b}'], [f'ss{b}'], s2=EPS, op1=ALU.add)
        S.act(nb.ss[b][:], nb.ss[b][:], AF.Sqrt, [f'ss{b}'], [f'ss{b}'])
        S.op('dve', lambda e, o=nb.ss[b][:]: e.reciprocal(out=o, in_=o), [f'ss{b}'], [f'ss{b}'])
        S.stt(nb.tmp[b][:], nb.xin[b][:], nb.ss[b][:, 0:1], G[:], ALU.mult, ALU.mult,
              [f'xin{b}', f'ss{b}', 'G0', 'G1'], [f'ntmp{b}'])
        if keep32:
            S.tt('pool', nb.tmp[b][:], nb.tmp[b][:], Sh[:], ALU.add, [f'ntmp{b}', 'S0', 'S1'], [f'ntmp{b}'])
            S.copy('pool', nb.h[b][:], nb.tmp[b][:], [f'ntmp{b}'], [f'nh{b}'])
        else:
            S.tt('pool', nb.h[b][:], nb.tmp[b][:], Sh[:], ALU.add, [f'ntmp{b}', 'S0', 'S1'], [f'nh{b}'])
        if hT_dst is not None:
            ht, hkey = hT_dst
            for half in range(2):
                for kk in range(8):
                    k = half * 8 + kk
                    S.tr(nb.ptr[half][:, kk * 128:(kk + 1) * 128], nb.h[b][:, k * 128:(k + 1) * 128], identb[:],
                         [f'nh{b}', 'identb'], [f'ptr{half}'], inc=(kk == 7))
                S.copy('act' if half == 0 else 'dve',
                       ht[:, half * 8:(half + 1) * 8, col0:col0 + 128],
                       nb.ptr[half][:].rearrange("p (k t) -> p k t", k=8), [f'ptr{half}'], [hkey])
        return b

    def x0_rows(t0, n):
        if t0 < NP:
            return xp[t0:t0 + n, :]
        return xs[t0 - NP:t0 - NP + n, :]

    with ExitStack() as st:
        G, Sh = make_GS(st, 0, 0, norm_mix[0:1, :])
        nb = NormBufs(st)
        hT = S.sb(st, [128, KD, 1024], BF16, "hT")
        wc = [S.sb(st, [128, KD, 512], BF16, f"wc{i}") for i in range(2)]
        ev = [S.sb(st, [128, 512], BF16, f"ev{i}") for i in range(2)]
        evf = [S.sb(st, [128, 512], F32, f"evf{i}") for i in range(2)]
        pm = [S.ps(st, [128, 512], F32, f"pm{i}") for i in range(2)]
        bg = S.sb(st, [8, 4], F32, "bg")
        gtmp = S.sb(st, [8, 512], F32, "gtmp")
        S.dma('sp', bg[:], bgl[:, :], [], ['bg'], 'dconst')
        S.ts('dve', bg[:], bg[:], 1.0 / 15.0, ALU.mult, ['bg'], ['bg'])
        w_in_r = w_in.rearrange("(k p) n -> p k n", p=128)
        wi = 0
        ei = 0
        for blk in range(T0 // 1024):
            row = 0 if blk == 0 else 1
            for tl in range(8):
                t0 = blk * 1024 + tl * 128
                norm_tile(nb, x0_rows(t0, 128), G[row], Sh[row], (hT, 'hT'), tl * 128)
            for cg in range(13):
                wb = wi % 2
                wi += 1
                ncol = 512 if cg < 12 else 32
                S.dma('pool', wc[wb][:, :, 0:ncol], w_in_r[:, :, cg * 512:cg * 512 + ncol], [], [f'wc{wb}'], f'dwc{wb}')
                if cg < 4:
                    for hh in range(4):
                        head = (cg % 2) * 4 + hh
                        for tb in range(2):
                            pb = ei % 2
                            ei += 1
                            for k in range(KD):
                                S.mm(pm[pb][:, :], wc[wb][:, k, hh * 128:(hh + 1) * 128], hT[:, k, tb * 512:(tb + 1) * 512],
                                     k == 0, k == KD - 1, [f'wc{wb}', 'hT'], [f'pm{pb}'])
                            dst = qT_d if cg < 2 else kT_d
                            if cg < 2:
                                S.copy('act', ev[pb][:], pm[pb][:], [f'pm{pb}'], [f'ev{pb}'])
                            else:
                                S.op('act', lambda e, o=ev[pb][:], i=pm[pb][:]: e.mul(o, i, 128.0 ** -0.5),
                                     [f'pm{pb}'], [f'ev{pb}'])
                            c0 = blk * 1024 + tb * 512
                            S.dma('sp', dst[head, :, c0:c0 + 512], ev[pb][:], [f'ev{pb}'], ['qk_d'], 'dqkw')
                if 2 <= cg < 12:
                    for tl in range(8):
                        pb = ei % 2
                        ei += 1
                        for k in range(KD):
                            S.mm(pm[pb][:, :], hT[:, k, tl * 128:(tl + 1) * 128], wc[wb][:, k, :],
                                 k == 0, k == KD - 1, [f'wc{wb}', 'hT'], [f'pm{pb}'])
                        r0 = blk * 1024 + tl * 128
                        if cg < 4:
                            S.op('act', lambda e, o=ev[pb][:], i=pm[pb][:]: e.mul(o, i, 128.0 ** -0.5),
                                 [f'pm{pb}'], [f'ev{pb}'])
                            S.dma('sp', k_d[r0:r0 + 128, (cg - 2) * 512:(cg - 1) * 512], ev[pb][:], [f'ev{pb}'], ['qk_d'], 'dqkw')
                        elif cg < 8:
                            S.copy('dve', ev[pb][:], pm[pb][:], [f'pm{pb}'], [f'ev{pb}'])
                            S.dma('sp', v_d[r0:r0 + 128, (cg - 4) * 512:(cg - 3) * 512], ev[pb][:], [f'ev{pb}'], ['qk_d'], 'dqkw')
                        else:
                            S.act(evf[pb][:], pm[pb][:], AF.Sigmoid, [f'pm{pb}'], [f'evf{pb}'])
                            S.dma('sp', og_d[r0:r0 + 128, (cg - 8) * 512:(cg - 7) * 512], evf[pb][:], [f'evf{pb}'], ['qk_d'], 'dqkw')
                if cg == 12:
                    for grp in range(4):
                        for tb in range(2):
                            pb = ei % 2
                            ei += 1
                            for k in range(KD):
                                S.mm(pm[pb][0:8, :], wc[wb][:, k, grp * 8:(grp + 1) * 8], hT[:, k, tb * 512:(tb + 1) * 512],
                                     k == 0, k == KD - 1, [f'wc{wb}', 'hT'], [f'pm{pb}'])
                            S.act(gtmp[:], pm[pb][0:8, :], AF.Tanh, [f'pm{pb}', 'bg'], ['gtmp'],
                                  bias=bg[:, grp:grp + 1], scale=1.0 / 15.0)
                            if grp % 2 == 0:
                                S.ts('dve', evf[pb][0:8, :], gtmp[:], 15.0, ALU.mult, ['gtmp'], [f'evf{pb}'])
                            else:
                                S.act(gtmp[:], gtmp[:], AF.Exp, ['gtmp'], ['gtmp'], scale=-15.0)
                                S.act(gtmp[:], gtmp[:], AF.Ln, ['gtmp'], ['gtmp'], bias=1.0)
                                S.ts('dve', evf[pb][0:8, :], gtmp[:], -1.0, ALU.mult, ['gtmp'], [f'evf{pb}'])
                            c0 = blk * 1024 + tb * 512
                            S.dma('sp', gi_d[grp, :, c0:c0 + 512], evf[pb][0:8, :], [f'evf{pb}'], ['qk_d'], 'dqkw')
        S.flush()


    if STOP_AFTER <= 2:
        return nc, es, S

    with ExitStack() as st:
        ones8 = S.sb(st, [8, 128], F32, "ones8")
        bmask = S.sb(st, [8, 512], F32, "bmask")
        S.memset('dve', ones8[:], 1.0, ['ones8'])
        onesb = S.sb(st, [128, 1], BF16, "onesb")
        S.memset('dve', onesb[:], 1.0, ['onesb'])
        S.dma('sp', bmask[:], bmask_d[:, :], [], ['bmask'], 'dconst')
        bmask3 = bmask[:].rearrange("p (h t) -> p h t", h=8)
        cm = []
        B = []
        for d in range(2):
            c_ = S.sb(st, [64, 512], F32, f"cm{d}")
            S.dma('sp', c_[:], cmask_d[d, :, :], [], [f'cm{d}'], 'dconst')
            cm.append(c_)
            b = {}
            b['I'] = S.sb(st, [8, 256], F32, f"I{d}")
            b['F'] = S.sb(st, [8, 256], F32, f"F{d}")
            b['qT'] = S.sb(st, [128, 8, 256], BF16, f"qT{d}")
            b['kT'] = S.sb(st, [128, 8, 256], BF16, f"kT{d}")
            for nm_ in ('b', 'c', 'M', 'nM', 'wk', 'wi', 'fl', 't'):
                b[nm_] = S.sb(st, [8, 64], F32, f"r{nm_}{d}")
            b['m'] = [S.sb(st, [8, 1], F32, f"m{i}{d}") for i in range(2)]
            b['mfin'] = [S.sb(st, [8, 1], F32, f"mfin{i}{d}") for i in range(2)]
            b['BD1'] = S.sb(st, [8, 512], F32, f"BD1{d}")
            b['BD2'] = S.sb(st, [8, 512], F32, f"BD2{d}")
            b['dg'] = S.sb(st, [8, 8], F32, f"dg{d}")
            b['E'] = S.sb(st, [64, 512], F32, f"E{d}")
            for nm_, shp, dt_ in (('v', [64, 8, 257], BF16), ('k', [64, 8, 128], BF16), ('tok', [64, 32], F32),
                                  ('S', [64, 512], BF16), ('qp', [128, 512], BF16), ('kw', [64, 8, 128], BF16),
                                  ('decb', [128, 8], F32), ('Hc', [64, 8, 256], F32)):
                b[nm_] = [S.sb(st, shp, dt_, f"{nm_}{i}{d}") for i in range(2)]
            b['Cn'] = S.sb(st, [128, 8, 256], F32, f"Cn{d}")
            b['nst'] = S.sb(st, [128, 8], F32, f"nst{d}")
            b['Cnb'] = [S.sb(st, [128, 8, 256], BF16, f"Cnb{i}{d}") for i in range(2)]
            b['nb'] = [S.sb(st, [128, 8], BF16, f"nb{i}{d}") for i in range(2)]
            b['dd'] = S.sb(st, [64, 8], F32, f"dd{d}")
            b['pA'] = S.ps(st, [128, 512], F32, f"pA{d}")
            b['pS'] = S.ps(st, [128, 512], F32, f"pS{d}")
            b['pN'] = [S.ps(st, [128, 512], F32, f"pN{i}{d}") for i in range(2)]
            b['ci'] = 0
            b['mi'] = 0
            for i in range(2):
                S.memset('pool', b['v'][i][:, :, 256:257], 1.0, [f'v{i}{d}'])
            B.append(b)

        def K_(n, d):
            return f'{n}{d}'

        def group_load(d, g0):
            b = B[d]
            S.dma('sp', b['I'][:], gi_d[2 * d, :, g0:g0 + 256], ['qk_d'], [K_('I', d)], K_('dgl', d))
            S.dma('sp', b['F'][:], gi_d[2 * d + 1, :, g0:g0 + 256], ['qk_d'], [K_('F', d)], K_('dgl', d))
            S.dma('sp', b['qT'][:], qT_d[:, :, g0:g0 + 256].rearrange("h p t -> p h t"), ['qk_d'], [K_('qT', d)], K_('dgl', d))
            S.dma('sp', b['kT'][:], kT_d[:, :, g0:g0 + 256].rearrange("h p t -> p h t"), ['qk_d'], [K_('kT', d)], K_('dgl', d))

        def pre(d, u, si):
            q, g0, jc, first, lastc, newgrp = u
            b = B[d]
            r0 = g0 + jc * 64
            cs = slice(jc * 64, jc * 64 + 64)
            last = 63 if d == 0 else 0
            k = lambda n: K_(n, d)
            ks = lambda n: f'{n}{si}{d}'

            def rv(ap):
                return ap[:, ::-1] if d == 1 else ap
            if first:
                if q < 4:
                    S.memset('pool', b['m'][b['mi']][:], 0.0, [k('m')])
                else:
                    S.dma('sp', b['m'][b['mi']][:], sml[d], [], [k('m')], K_('dst', d))
            if newgrp:
                group_load(d, g0)
            mcur = b['m'][b['mi']]
            mnew = b['m'][1 - b['mi']]
            S.dma('sp', b['v'][si][:, :, 0:256], v_d[r0:r0 + 64, :].rearrange("t (h v) -> t h v", h=8), ['qk_d'], [ks('v')], ks('dcl'))
            S.dma('sp', b['k'][si][:], k_d[r0:r0 + 64, :].rearrange("t (h v) -> t h v", h=8), ['qk_d'], [ks('k')], ks('dcl'))
            S.op('dve', lambda e, o=rv(b['b'][:]), a=ones8[:, 0:64], x=rv(b['F'][:, cs]):
                 e.tensor_tensor_scan(out=o, data0=a, data1=x, initial=0.0, op0=ALU.mult, op1=ALU.add),
                 ['ones8', k('F')], [k('b')])
            S.tt('dve', b['c'][:], b['I'][:, cs], b['b'][:], ALU.subtract, [k('I'), k('b')], [k('c')])
            S.op('dve', lambda e, o=rv(b['M'][:]), a=ones8[:, 0:64], x=rv(b['c'][:]), i=mcur[:, 0:1]:
                 e.tensor_tensor_scan(out=o, data0=a, data1=x, initial=i, op0=ALU.mult, op1=ALU.max),
                 ['ones8', k('c'), k('m')], [k('M')])
            S.ts('dve', b['nM'][:], b['M'][:], -1.0, ALU.mult, [k('M')], [k('nM')])
            S.act(b['wk'][:], b['c'][:], AF.Exp, [k('c'), k('nM')], [k('wk')], bias=b['nM'][:, last:last + 1])
            S.act(b['wi'][:], b['nM'][:], AF.Exp, [k('nM'), k('m')], [k('wi')], bias=mcur[:, 0:1])
            S.tt('dve', b['t'][:], b['nM'][:], b['b'][:], ALU.subtract, [k('nM'), k('b')], [k('t')])
            S.act(b['fl'][:], b['t'][:], AF.Exp, [k('t')], [k('fl')])
            S.tt('dve', mnew[:], b['b'][:, last:last + 1], b['M'][:, last:last + 1], ALU.add, [k('b'), k('M'), k('m')], [k('m')])
            if lastc and q < 4:
                S.copy('dve', b['mfin'][q % 2][:], mnew[:], [k('m')], [f'mfin{q % 2}{d}'])
            S.tt('dve', b['BD1'][:].rearrange("p (h t) -> p h t", h=8), bmask3,
                 b['nM'][:].unsqueeze(1).to_broadcast([8, 8, 64]), ALU.mult, ['bmask', k('nM')], [k('BD1')])
            S.tt('dve', b['BD2'][:].rearrange("p (h t) -> p h t", h=8), bmask3,
                 b['wi'][:].unsqueeze(1).to_broadcast([8, 8, 64]), ALU.mult, ['bmask', k('wi')], [k('BD2')])
            S.ts('dve', b['dg'][:], identf[0:8, 0:8], b['wi'][:, last:last + 1], ALU.mult, ['identf', k('wi')], [k('dg')])
            for j, nm_ in enumerate(('c', 'wk', 'wi', 'fl')):
                S.tr(b['pA'][0:64, j * 8:(j + 1) * 8], b[nm_][:], identf[0:8, 0:8], [k(nm_), 'identf'], [k('pA')], inc=False)
            S.mm(b['pA'][:, 32:40], ones8[:, :], b['dg'][:], True, True, ['ones8', k('dg')], [k('pA')])
            S.copy('act', b['tok'][si][:], b['pA'][0:64, 0:32], [k('pA')], [ks('tok')])
            S.copy('act', b['decb'][si][:], b['pA'][:, 32:40], [k('pA')], [ks('decb')])
            S.mm(b['pA'][0:64, :], ones8[:, 0:64], b['BD1'][:], True, False, ['ones8', k('BD1')], [k('pA')])
            S.mm(b['pA'][0:64, :], b['c'][:], bmask[:], False, False, [k('c'), 'bmask'], [k('pA')])
            S.mm(b['pA'][0:64, :], identf[0:64, 0:64], cm[d][:], False, True, ['identf', f'cm{d}'], [k('pA')])
            S.act(b['E'][:], b['pA'][0:64, :], AF.Exp, [k('pA')], [k('E')])
            for h in range(8):
                S.op('pe', lambda e, o=b['pA'][0:64, h * 64:(h + 1) * 64], l=b['kT'][:, h, cs], r=b['qT'][:, h, cs]:
                     e.matmul(o, l, r, start=True, stop=True), [k('kT'), k('qT')], [k('pA')], inc=(h == 7))
            S.tt('dve', b['S'][si][:], b['pA'][0:64, :], b['E'][:], ALU.mult, [k('pA'), k('E')], [ks('S')])
            S.mm(b['pA'][:, :], ones8[:, :], b['BD2'][:], True, True, ['ones8', k('BD2')], [k('pA')])
            S.tt('dve', b['qp'][si][:].rearrange("p (h t) -> p h t", h=8), b['qT'][:, :, cs],
                 b['pA'][:, :].rearrange("p (h t) -> p h t", h=8), ALU.mult, [k('qT'), k('pA')], [ks('qp')])
            S.tt('pool', b['kw'][si][:], b['k'][si][:], b['tok'][si][:, 8:16].unsqueeze(2).to_broadcast([64, 8, 128]), ALU.mult,
                 [ks('k'), ks('tok')], [ks('kw')])
            b['mi'] = 1 - b['mi']

        def post(d, u, si):
            q, g0, jc, first, lastc, newgrp = u
            b = B[d]
            r0 = g0 + jc * 64
            k = lambda n: K_(n, d)
            ks = lambda n: f'{n}{si}{d}'
            jo = b['ci']
            jn = 1 - jo
            ko = lambda n: f'{n}{jo}{d}'
            kn = lambda n: f'{n}{jn}{d}'
            if first:
                if q < 4:
                    S.memset('pool', b['Cn'][:], 0.0, [k('Cn')])
                    S.memset('pool', b['nst'][:], 0.0, [k('nst')])
                else:
                    S.dma('sp', b['Cn'][:], sC[d * 8:(d + 1) * 8].rearrange("h p v -> p h v"), [], [k('Cn')], K_('dst', d))
                    S.dma('sp', b['nst'][:], snl[:, d * 8:(d + 1) * 8], [], [k('nst')], K_('dst', d))
                S.copy('pool', b['Cnb'][jo][:], b['Cn'][:], [k('Cn')], [ko('Cnb')])
                S.copy('pool', b['nb'][jo][:], b['nst'][:], [k('nst')], [ko('nb')])
            S.tt('dve', b['Cn'][:], b['Cn'][:], b['decb'][si][:].unsqueeze(2).to_broadcast([128, 8, 256]), ALU.mult,
                 [k('Cn'), ks('decb')], [k('Cn')])
            for h in range(8):
                S.op('pe', lambda e, o=b['pS'][:, 8 + h:9 + h], l=b['kw'][si][:, h, :], r=onesb[0:64, 0:1]:
                     e.matmul(o, l, r, start=True, stop=True), [ks('kw'), 'onesb'], [k('pSn')], inc=(h == 7))
            S.tt('dve', b['nst'][:], b['nst'][:], b['decb'][si][:], ALU.mult, [k('nst'), ks('decb')], [k('nst')])
            S.tt('dve', b['nst'][:], b['nst'][:], b['pS'][:, 8:16], ALU.add, [k('nst'), k('pSn')], [k('nst')])
            for pr in range(4):
                pz = b['pN'][pr % 2]
                pk = f'pN{pr % 2}{d}'
                for hh in range(2):
                    h = pr * 2 + hh
                    S.op('pe', lambda e, o=pz[:, hh * 256:(hh + 1) * 256], l=b['kw'][si][:, h, :], r=b['v'][si][:, h, 0:256]:
                         e.matmul(o, l, r, start=True, stop=True), [ks('kw'), ks('v')], [pk], inc=(hh == 1))
                S.tt('dve', b['Cn'][:, pr * 2:pr * 2 + 2, :], b['Cn'][:, pr * 2:pr * 2 + 2, :],
                     pz[:, :].rearrange("p (h v) -> p h v", h=2), ALU.add, [k('Cn'), pk], [k('Cn')])
            S.copy('pool', b['Cnb'][jn][:], b['Cn'][:], [k('Cn')], [kn('Cnb')])
            S.copy('pool', b['nb'][jn][:], b['nst'][:], [k('nst')], [kn('nb')])
            for h in range(8):
                hs = slice(h * 64, (h + 1) * 64)
                S.op('pe', lambda e, o=b['pS'][0:64, h:h + 1], l=b['S'][si][:, hs], r=onesb[0:64, 0:1]:
                     e.matmul(o, l, r, start=True, stop=False), [ks('S'), 'onesb'], [k('pSd')], inc=False)
                S.op('pe', lambda e, o=b['pS'][0:64, h:h + 1], l=b['qp'][si][:, hs], r=b['nb'][jo][:, h:h + 1]:
                     e.matmul(o, l, r, start=False, stop=True), [ks('qp'), ko('nb')], [k('pSd')], inc=(h == 7))
            S.act(b['dd'][:], b['pS'][0:64, 0:8], AF.Abs, [k('pSd')], [k('dd')])
            S.tt('dve', b['dd'][:], b['dd'][:], b['tok'][si][:, 24:32], ALU.max, [k('dd'), ks('tok')], [k('dd')])
            S.op('dve', lambda e, o=b['dd'][:]: e.reciprocal(out=o, in_=o), [k('dd')], [k('dd')])
            for pr in range(4):
                pz = b['pN'][pr % 2]
                pk = f'pN{pr % 2}{d}'
                for hh in range(2):
                    h = pr * 2 + hh
                    hs = slice(h * 64, (h + 1) * 64)
                    S.op('pe', lambda e, o=pz[0:64, hh * 256:(hh + 1) * 256], l=b['S'][si][:, hs], r=b['v'][si][:, h, 0:256]:
                         e.matmul(o, l, r, start=True, stop=False), [ks('S'), ks('v')], [pk], inc=False)
                    S.op('pe', lambda e, o=pz[0:64, hh * 256:(hh + 1) * 256], l=b['qp'][si][:, hs], r=b['Cnb'][jo][:, h, :]:
                         e.matmul(o, l, r, start=False, stop=True), [ks('qp'), ko('Cnb')], [pk], inc=(hh == 1))
                S.tt('dve', b['Hc'][si][:, pr * 2:pr * 2 + 2, :], pz[0:64, :].rearrange("p (h v) -> p h v", h=2),
                     b['dd'][:, pr * 2:pr * 2 + 2].unsqueeze(2).to_broadcast([64, 2, 256]), ALU.mult, [pk, k('dd')], [ks('Hc')])
            S.dma('sp', hdl[d][r0:r0 + 64, :].rearrange("t (h v) -> t h v", h=8), b['Hc'][si][:], [ks('Hc')], ['hd_d'], 'dhd')
            b['ci'] = jn
            if lastc and q < 4:
                S.dma('sp', nC[q, d * 8:(d + 1) * 8].rearrange("h p v -> p h v"), b['Cn'][:], [k('Cn')], ['nC'], 'dout')
                S.dma('sp', nn[q, d * 8:(d + 1) * 8, :].rearrange("h p -> p h"), b['nst'][:], [k('nst')], ['nn'], 'dout', slow=True)
                S.dma('sp', nm[q, d, :].rearrange("(h o) -> h o", o=1), b['mfin'][q % 2][:], [f'mfin{q % 2}{d}'], ['nm'], 'dout')

        seqs = [(q, q * 256, 4) for q in range(4)] + [(4, NP, 64)]
        units = [[], []]
        for (q, base, nch) in seqs:
            ngr = nch // 4
            for d in range(2):
                for gi_ in range(ngr):
                    gg = gi_ if d == 0 else ngr - 1 - gi_
                    for jj in range(4):
                        jc = jj if d == 0 else 3 - jj
                        ci = gi_ * 4 + jj
                        units[d].append((q, base + gg * 256, jc, ci == 0, ci == nch - 1, jj == 0))
        nu = len(units[0])
        S.interleave([lambda d=d: pre(d, units[d][0], 0) for d in range(2)])
        for i in range(nu):
            fns = []
            if i + 1 < nu:
                fns += [lambda d=d, i=i: pre(d, units[d][i + 1], (i + 1) % 2) for d in range(2)]
            fns += [lambda d=d, i=i: post(d, units[d][i], i % 2) for d in range(2)]
            S.interleave(fns)
        S.flush()

    if STOP_AFTER <= 3:
        return nc, es, S

    with ExitStack() as st:
        HN = S.sb(st, [128, D], F32, "HN")
        load_bc(HN[:], head_norm[0:1, :], 'HN')
        gate = []
        for row in range(2):
            g_ = S.sb(st, [128, D], F32, f"gate{row}")
            load_bc(g_[:], mod[0, row:row + 1, 2 * D:3 * D], f'gate{row}')
            gate.append(g_)
        yT = S.sb(st, [128, KD, 1024], BF16, "yT")
        hf = S.sb(st, [128, D], F32, "hf")
        hb = S.sb(st, [128, D], F32, "hb")
        ogt = S.sb(st, [128, D], F32, "ogt")
        yb = [S.sb(st, [128, D], BF16, f"yb{i}") for i in range(2)]
        junk = S.sb(st, [128, 256], BF16, "junk4")
        ss8 = S.sb(st, [128, 8], F32, "ss8")
        ixw = S.sb(st, [128, 1], I32, "ixw")
        ixs = S.sb(st, [128, 1], I32, "ixs")
        ptr = [S.ps(st, [128, 1024], BF16, f"ptr4{i}") for i in range(2)]
        wo = [S.sb(st, [128, KD, 512], BF16, f"wo{i}") for i in range(2)]
        xblk = S.sb(st, [128, 8, D], F32, "xblk")
        po = [S.ps(st, [128, 512], F32, f"po4{i}") for i in range(2)]
        w_out_r = w_out.rearrange("(k p) n -> p k n", p=128)
        ti = 0
        wi_ = 0
        oi = 0

        def gath(dst, src2d, ix, rk, wk_):
            S.op('pool', lambda e, o=dst, s_=src2d, i_=ix: e.indirect_dma_start(
                out=o, out_offset=None, in_=s_, in_offset=bass.IndirectOffsetOnAxis(ap=i_, axis=0)),
                rk, wk_, dma='dg4')

        for blk in range(3):
            row = 0 if blk == 0 else 1
            for tl in range(8):
                b = ti % 2
                ti += 1
                if blk == 0:
                    r0 = tl * 128
                    S.dma('sp', hf[:], hdl[0][r0:r0 + 128, :], ['hd_d'], ['hf'], 'dhf')
                    S.dma('sp', hb[:], hdl[1][r0:r0 + 128, :], ['hd_d'], ['hb'], 'dhb')
                    S.dma('sp', ogt[:], og_d[r0:r0 + 128, :], ['qk_d'], ['ogt'], 'dog')
                    S.dma('sp', xblk[:, tl, :], xp[r0:r0 + 128, :], [], ['xblk'], 'dxb')
                else:
                    wt = (blk - 1) * 8 + tl
                    S.dma('sp', ixw[:], idx_win[wt], [], ['ixw'], 'dix')
                    S.dma('sp', ixs[:], idx_xs[wt], [], ['ixs'], 'dix')
                    gath(hf[:, :], hdl[0][:, :], ixw[:, :], ['ixw', 'hd_d'], ['hf'])
                    gath(hb[:, :], hdl[1][:, :], ixw[:, :], ['ixw', 'hd_d'], ['hb'])
                    gath(ogt[:, :], og_d[:, :], ixw[:, :], ['ixw', 'qk_d'], ['ogt'])
                    gath(xblk[:, tl, :], xs[:, :], ixs[:, :], ['ixs'], ['xblk'])
                S.tt('pool', hf[:], hf[:], hb[:], ALU.add, ['hf', 'hb'], ['hf'])
                for h in range(8):
                    S.act(junk[:], hf[:, h * 256:(h + 1) * 256], AF.Square, ['hf'], ['junk4', 'ss8'],
                          accum=ss8[:, h:h + 1])
                S.ts('dve', ss8[:], ss8[:], 1.0 / 256.0, ALU.mult, ['ss8'], ['ss8'], s2=EPS, op1=ALU.add)
                S.act(ss8[:], ss8[:], AF.Sqrt, ['ss8'], ['ss8'])
                S.op('dve', lambda e, o=ss8[:]: e.reciprocal(out=o, in_=o), ['ss8'], ['ss8'])
                S.tt('dve', hf[:].rearrange("p (h v) -> p h v", h=8), hf[:].rearrange("p (h v) -> p h v", h=8),
                     ss8[:].unsqueeze(2).to_broadcast([128, 8, 256]), ALU.mult, ['hf', 'ss8'], ['hf'])
                S.tt('pool', ogt[:], ogt[:], HN[:], ALU.mult, ['ogt', 'HN'], ['ogt'])
                S.tt('dve', yb[b][:], hf[:], ogt[:], ALU.mult, ['hf', 'ogt'], [f'yb{b}'])
                for half in range(2):
                    for kk in range(8):
                        kx = half * 8 + kk
                        S.tr(ptr[half][:, kk * 128:(kk + 1) * 128], yb[b][:, kx * 128:(kx + 1) * 128], identb[:],
                             [f'yb{b}', 'identb'], [f'ptr4{half}'], inc=(kk == 7))
                    S.copy('act' if half == 0 else 'dve', yT[:, half * 8:(half + 1) * 8, tl * 128:(tl + 1) * 128],
                           ptr[half][:].rearrange("p (k t) -> p k t", k=8), [f'ptr4{half}'], ['yT'])
            for n4 in range(4):
                wb = wi_ % 2
                wi_ += 1
                S.dma('pool', wo[wb][:], w_out_r[:, :, n4 * 512:(n4 + 1) * 512], [], [f'wo{wb}'], f'dwo{wb}')
                for tl in range(8):
                    ob = oi % 2
                    oi += 1
                    for kx in range(KD):
                        S.mm(po[ob][:, :], yT[:, kx, tl * 128:(tl + 1) * 128], wo[wb][:, kx, :], kx == 0, kx == KD - 1,
                             ['yT', f'wo{wb}'], [f'po4{ob}'])
                    S.tt('dve', po[ob][:, :], po[ob][:, :], gate[row][:, n4 * 512:(n4 + 1) * 512], ALU.mult,
                         [f'po4{ob}', f'gate{row}'], [f'po4{ob}'])
                    S.tt('dve', xblk[:, tl, n4 * 512:(n4 + 1) * 512], xblk[:, tl, n4 * 512:(n4 + 1) * 512], po[ob][:, :], ALU.add,
                         ['xblk', f'po4{ob}'], ['xblk'])
            for tl in range(8):
                r0 = blk * 1024 + tl * 128
                S.dma('sp', x1[r0:r0 + 128, :], xblk[:, tl, :], ['xblk'], ['x1'], 'dx1')
        S.flush()

    if STOP_AFTER <= 4:
        return nc, es, S

    def ffn_layer(l, x_rows, nblk, row_of_blk, experts, moe, out_rows, final):
        for blk in range(nblk):
            row = row_of_blk(blk)
            with ExitStack() as st0:
                hT = S.sb(st0, [128, KD, 1024], BF16, "hTf")
                comb = S.sb(st0, [128, 8, 8], F32, "comb")
                with ExitStack() as st:
                    G, Sh = make_GS(st, l, 1, norm_ffn[l:l + 1, :])
                    nb = NormBufs(st)
                    if moe:
                        rt = S.sb(st, [128, KD, 8], F32, "rt")
                        S.dma('sp', rt[:], router.rearrange("(k p) e -> p k e", p=128), [], ['rt'], 'dconst', slow=True)
                        h32T = S.sb(st, [128, KD, 128], F32, "h32T")
                        p32 = [S.ps(st, [128, 512], F32, f"p32{i}") for i in range(2)]
                        plg = S.ps(st, [128, 512], F32, "plg")
                        lg = S.sb(st, [128, 8], F32, "lg")
                        top = S.sb(st, [128, 8], F32, "top")
                        g12 = S.sb(st, [128, 2], F32, "g12")
                        eq = S.sb(st, [128, 8], F32, "eq")
                    for tl in range(8):
                        b = norm_tile(nb, x_rows(blk, tl), G[row], Sh[row], (hT, 'hTf'), tl * 128, keep32=moe)
                        if moe:
                            for qq in range(4):
                                pq = p32[qq % 2]
                                for kk in range(4):
                                    kx = qq * 4 + kk
                                    S.tr(pq[:, kk * 128:(kk + 1) * 128], nb.tmp[b][:, kx * 128:(kx + 1) * 128], identf[:],
                                         [f'ntmp{b}', 'identf'], [f'p32{qq % 2}'], inc=(kk == 3))
                                S.copy('act', h32T[:, qq * 4:(qq + 1) * 4, :], pq[:].rearrange("p (k t) -> p k t", k=4),
                                       [f'p32{qq % 2}'], ['h32T'])
                            for kx in range(KD):
                                S.mm(plg[:, 0:8], h32T[:, kx, :], rt[:, kx, :], kx == 0, kx == KD - 1, ['h32T', 'rt'], ['plg'])
                            S.copy('dve', lg[:], plg[:, 0:8], ['plg'], ['lg'])
                            S.op('dve', lambda e, o=top[:], i=lg[:]: e.max(out=o, in_=i), ['lg'], ['top'])
                            S.tt('dve', g12[:, 0:1], top[:, 0:1], top[:, 1:2], ALU.subtract, ['top'], ['g12'])
                            S.act(g12[:, 0:1], g12[:, 0:1], AF.Sigmoid, ['g12'], ['g12'])
                            S.ts('dve', g12[:, 1:2], g12[:, 0:1], -1.0, ALU.mult, ['g12'], ['g12'], s2=1.0, op1=ALU.add)
                            S.ts('dve', eq[:], lg[:], top[:, 0:1], ALU.is_equal, ['lg', 'top', 'g12'], ['eq'], s2=g12[:, 0:1], op1=ALU.mult)
                            S.ts('dve', comb[:, tl, :], lg[:], top[:, 1:2], ALU.is_equal, ['lg', 'top', 'g12'], ['comb'], s2=g12[:, 1:2], op1=ALU.mult)
                            S.tt('dve', comb[:, tl, :], comb[:, tl, :], eq[:], ALU.add, ['comb', 'eq'], ['comb'])
                    S.flush()
                yacc = S.sb(st0, [128, 8, D], F32, "yacc")
                with ExitStack() as st:
                    actb = S.sb(st, [128, 11, 1024], BF16, "actb")
                    w1c = [S.sb(st, [128, KD, 128], BF16, f"w1c{i}") for i in range(2)]
                    w3c = [S.sb(st, [128, KD, 128], BF16, f"w3c{i}") for i in range(2)]
                    w2p = [S.sb(st, [128, 11, 512], BF16, f"w2p{i}") for i in range(2)]
                    w2i = 0
                    sg = [S.sb(st, [128, 512], F32, f"sg{i}") for i in range(2)]
                    pa = [S.ps(st, [128, 512], F32, f"pfa{i}") for i in range(2)]
                    pb = [S.ps(st, [128, 512], F32, f"pfb{i}") for i in range(2)]
                    po = [S.ps(st, [128, 512], F32, f"pfo{i}") for i in range(2)]
                    wi_ = 0
                    pi_ = 0
                    oi = 0
                    first = True
                    for ei, (w1, w3, w2) in enumerate(experts):
                        w1r = w1.rearrange("(k p) n -> p k n", p=128)
                        w3r = w3.rearrange("(k p) n -> p k n", p=128)
                        w2r = w2.rearrange("(f p) n -> p f n", p=128)
                        for qd in range(4):
                            for fc in range(11):
                                f0 = (qd * 11 + fc) * 128
                                wb = wi_ % 2
                                wi_ += 1
                                S.dma('pool', w1c[wb][:], w1r[:, :, f0:f0 + 128], [], [f'w1c{wb}'], f'dw1{wb}')
                                S.dma('pool', w3c[wb][:], w3r[:, :, f0:f0 + 128], [], [f'w3c{wb}'], f'dw3{wb}')
                                for tb in range(2):
                                    p_ = pi_ % 2
                                    pi_ += 1
                                    for kx in range(KD):
                                        S.mm(pa[p_][:, :], w1c[wb][:, kx, :], hT[:, kx, tb * 512:(tb + 1) * 512], kx == 0, kx == KD - 1,
                                             [f'w1c{wb}', 'hTf'], [f'pfa{p_}'])
                                    for kx in range(KD):
                                        S.mm(pb[p_][:, :], w3c[wb][:, kx, :], hT[:, kx, tb * 512:(tb + 1) * 512], kx == 0, kx == KD - 1,
                                             [f'w3c{wb}', 'hTf'], [f'pfb{p_}'])
                                    S.act(sg[p_][:], pa[p_][:, :], AF.Silu, [f'pfa{p_}'], [f'sg{p_}'])
                                    S.tt('dve', actb[:, fc, tb * 512:(tb + 1) * 512], sg[p_][:], pb[p_][:, :], ALU.mult,
                                         [f'sg{p_}', f'pfb{p_}'], ['actb'])
                            for n4 in range(4):
                                w2b = w2i % 2
                                w2i += 1
                                S.dma('pool', w2p[w2b][:], w2r[:, qd * 11:(qd + 1) * 11, n4 * 512:(n4 + 1) * 512], [], [f'w2p{w2b}'], f'dw2p{w2b}')
                                for tl in range(8):
                                    ob = oi % 2
                                    oi += 1
                                    for fc in range(11):
                                        S.mm(po[ob][:, :], actb[:, fc, tl * 128:(tl + 1) * 128], w2p[w2b][:, fc, :], fc == 0, fc == 10,
                                             ['actb', f'w2p{w2b}'], [f'pfo{ob}'])
                                    ya = yacc[:, tl, n4 * 512:(n4 + 1) * 512]
                                    if moe:
                                        if first:
                                            S.ts('dve', ya, po[ob][:, :], comb[:, tl, ei:ei + 1], ALU.mult, [f'pfo{ob}', 'comb'], ['yacc'])
                                        else:
                                            S.stt(ya, po[ob][:, :], comb[:, tl, ei:ei + 1], ya, ALU.mult, ALU.add,
                                                  [f'pfo{ob}', 'comb', 'yacc'], ['yacc'])
                                    else:
                                        if first:
                                            S.copy('act', ya, po[ob][:, :], [f'pfo{ob}'], ['yacc'])
                                        else:
                                            S.tt('dve', ya, ya, po[ob][:, :], ALU.add, [f'pfo{ob}', 'yacc'], ['yacc'])
                            first = False
                    S.flush()
                with ExitStack() as st:
                    gt_ = S.sb(st, [128, D], F32, "gateR")
                    load_bc(gt_[:], mod[l, row:row + 1, 5 * D:6 * D], 'gateR')
                    xr = [S.sb(st, [128, D], F32, f"xr{i}") for i in range(2)]
                    if final:
                        NF = S.sb(st, [128, D], F32, "NF")
                        load_bc(NF[:], norm_final[0:1, :], 'NF')
                        junkf = S.sb(st, [128, D], BF16, "junkf")
                        ssf = [S.sb(st, [128, 1], F32, f"ssf{i}") for i in range(2)]
                    for tl in range(8):
                        b = tl % 2
                        S.dma('sp', xr[b][:], x_rows(blk, tl), [], [f'xr{b}'], f'dxr{b}')
                        S.tt('pool', yacc[:, tl, :], yacc[:, tl, :], gt_[:], ALU.mult, ['yacc', 'gateR'], ['yacc'])
                        S.tt('dve', xr[b][:], xr[b][:], yacc[:, tl, :], ALU.add, [f'xr{b}', 'yacc'], [f'xr{b}'])
                        if final:
                            S.act(junkf[:], xr[b][:], AF.Square, [f'xr{b}'], ['junkf', f'ssf{b}'], accum=ssf[b][:])
                            S.ts('dve', ssf[b][:], ssf[b][:], 1.0 / D, ALU.mult, [f'ssf{b}'], [f'ssf{b}'], s2=EPS, op1=ALU.add)
                            S.act(ssf[b][:], ssf[b][:], AF.Sqrt, [f'ssf{b}'], [f'ssf{b}'])
                            S.op('dve', lambda e, o=ssf[b][:]: e.reciprocal(out=o, in_=o), [f'ssf{b}'], [f'ssf{b}'])
                            S.stt(xr[b][:], xr[b][:], ssf[b][:, 0:1], NF[:], ALU.mult, ALU.mult, [f'xr{b}', f'ssf{b}', 'NF'], [f'xr{b}'])
                        S.dma('sp', out_rows(blk, tl), xr[b][:], [f'xr{b}'], ['xout'], 'dxout')
                    S.flush()

    ffn_layer(0, lambda blk, tl: x1[blk * 1024 + tl * 128: blk * 1024 + (tl + 1) * 128, :], 3,
              lambda blk: 0 if blk == 0 else 1, [(ffn_w1, ffn_w3, ffn_w2)], False,
              lambda blk, tl: x2[blk * 1024 + tl * 128: blk * 1024 + (tl + 1) * 128, :], False)
    if STOP_AFTER <= 5:
        return nc, es, S

    with ExitStack() as st:
        hall = S.sb(st, [128, 16, D], BF16, "hall")
        PW = S.sb(st, [128, 16, 512], BF16, "PW")
        S.dma('pool', PW[:], pool_w.rearrange("g (c p) n -> p (g c) n", p=128), [], ['PW'], 'dconst2')
        pmt = S.sb(st, [128, 20, 128], BF16, "pmt")
        ict = S.sb(st, [128, 4, 128], F32, "ict")
        dT = S.sb(st, [128, 16, 128], BF16, "dT")
        psg = S.sb(st, [128, D], F32, "psg")
        gt = S.sb(st, [128, D], F32, "gt6")
        Gt = S.sb(st, [128, D], F32, "G6")
        St = S.sb(st, [128, D], F32, "S6")
        xin = S.sb(st, [128, D], F32, "xin6")
        junk = S.sb(st, [128, D], BF16, "junk6")
        ss = S.sb(st, [128, 1], F32, "ss6")
        tmp = S.sb(st, [128, D], F32, "tmp6")
        idx = S.sb(st, [128, 1], I32, "idx6")
        pd = [S.ps(st, [128, 512], F32, f"pd{i}") for i in range(2)]
        pq = [S.ps(st, [128, 512], F32, f"pq{i}") for i in range(2)]
        load_bc(gt[:], norm_mix[1:2, :], 'gt6')

        def norm6(dst_tile_idx):
            S.act(junk[:], xin[:], AF.Square, ['xin6'], ['junk6', 'ss6'], accum=ss[:])
            S.ts('dve', ss[:], ss[:], 1.0 / D, ALU.mult, ['ss6'], ['ss6'], s2=EPS, op1=ALU.add)
            S.act(ss[:], ss[:], AF.Sqrt, ['ss6'], ['ss6'])
            S.op('dve', lambda e, o=ss[:]: e.reciprocal(out=o, in_=o), ['ss6'], ['ss6'])
            S.stt(tmp[:], xin[:], ss[:, 0:1], Gt[:], ALU.mult, ALU.mult, ['xin6', 'ss6', 'G6'], ['tmp6'])
            S.tt('pool', hall[:, dst_tile_idx, :], tmp[:], St[:], ALU.add, ['tmp6', 'S6'], ['hall'])

        pi_ = 0
        for grp in range(2):
            row = grp
            load_bc(Gt[:], mod[1, row:row + 1, D:2 * D], 'G6')
            load_bc(St[:], mod[1, row:row + 1, 0:D], 'S6')
            S.stt(Gt[:], Gt[:], 1.0, gt[:], ALU.add, ALU.mult, ['G6', 'gt6'], ['G6'])
            load_bc(psg[:], mod[1, row:row + 1, 2 * D:3 * D], 'psg')
            load_bc(tmp[:], pool_scale[0:1, :], 'tmp6')
            S.tt('dve', psg[:], psg[:], tmp[:], ALU.mult, ['psg', 'tmp6'], ['psg'])
            ntile = 8 if grp == 0 else 16
            for t_ in range(ntile):
                if grp == 0:
                    S.dma('sp', xin[:], x2[t_ * 128:(t_ + 1) * 128, :], ['xout'], ['xin6'], 'dxin6')
                else:
                    S.dma('sp', xin[:], x2[NP + t_ * 128:NP + (t_ + 1) * 128, :], ['xout'], ['xin6'], 'dxin6')
                norm6(t_)
            for j in range(8):
                if grp == 0:
                    jo = j % 2
                    for g in range(4):
                        S.dma('pool', pmt[:, g * 2:(g + 1) * 2, :], pm_p[(g * 2 + jo) * 2:(g * 2 + jo) * 2 + 2].rearrange("m p t -> p m t"),
                              [], ['pmt'], 'dpmt')
                        S.dma('sp', ict[:, g, :], ic_p[g * 2 + jo:g * 2 + jo + 1, :].partition_broadcast(128), [], ['ict'], 'dict')
                    S.dma('sp', xin[:], x2[j * 128:(j + 1) * 128, :], ['xout'], ['xin6'], 'dxin6')
                else:
                    mi = 0
                    for g in range(4):
                        nm_ = (3, 3, 5, 9)[g]
                        off = sum((3, 3, 5, 9)[:g])
                        S.dma('pool', pmt[:, off:off + nm_, :], pm_s[g_off(g) + j * nm_: g_off(g) + (j + 1) * nm_].rearrange("m p t -> p m t"),
                              [], ['pmt'], 'dpmt')
                        S.dma('sp', ict[:, g, :], ic_s[g * 8 + j:g * 8 + j + 1, :].partition_broadcast(128), [], ['ict'], 'dict')
                    S.dma('sp', xin[:], x2[NP + (j + 4) * 128:NP + (j + 5) * 128, :], ['xout'], ['xin6'], 'dxin6')
                for g in range(4):
                    p_ = pi_ % 2
                    pi_ += 1
                    if grp == 0:
                        srcs = [((j // 2) * 2 + ji, g * 2 + ji) for ji in range(2)]
                    else:
                        hw_ = (1, 1, 2, 4)[g]
                        off = sum((3, 3, 5, 9)[:g])
                        srcs = [(j + 4 + di, off + di + hw_) for di in range(-hw_, hw_ + 1)]
                    for cc in range(4):
                        for si, (ti_, mi_) in enumerate(srcs):
                            S.op('pe', lambda e, o=pd[p_][:, cc * 128:(cc + 1) * 128], l=hall[:, ti_, g * 512 + cc * 128: g * 512 + (cc + 1) * 128], r=pmt[:, mi_, :],
                                 a=(si == 0), z=(si == len(srcs) - 1): e.matmul(o, l, r, start=a, stop=z),
                                 ['hall', 'pmt'], [f'pd{p_}'], inc=(cc == 3 and si == len(srcs) - 1))
                    S.tt('dve', dT[:, g * 4:(g + 1) * 4, :], pd[p_][:, :].rearrange("p (c t) -> p c t", c=4),
                         ict[:, g, :].unsqueeze(1).to_broadcast([128, 4, 128]), ALU.mult, [f'pd{p_}', 'ict'], ['dT'])
                for g in range(4):
                    p_ = pi_ % 2
                    pi_ += 1
                    for cc in range(4):
                        S.mm(pq[p_][:, :], dT[:, g * 4 + cc, :], PW[:, g * 4 + cc, :], cc == 0, cc == 3, ['dT', 'PW'], [f'pq{p_}'])
                    S.tt('dve', tmp[:, g * 512:(g + 1) * 512], pq[p_][:, :], psg[:, g * 512:(g + 1) * 512], ALU.mult,
                         [f'pq{p_}', 'psg'], ['tmp6'])
                S.tt('pool', xin[:], xin[:], tmp[:], ALU.add, ['xin6', 'tmp6'], ['xin6'])
                r0 = grp * 1024 + j * 128
                S.dma('sp', x3[r0:r0 + 128, :], xin[:], ['xin6'], ['x3'], 'dx3')
        S.flush()
    if STOP_AFTER <= 6:
        return nc, es, S

    ffn_layer(1, lambda blk, tl: x3[blk * 1024 + tl * 128: blk * 1024 + (tl + 1) * 128, :], 2,
              lambda blk: blk, [(moe_w1[e], moe_w3[e], moe_w2[e]) for e in range(8)], True,
              lambda blk, tl: (yp if blk == 0 else ys)[tl * 128:(tl + 1) * 128, :], True)
    return nc, es, S


DBG_OUT = set()
STOP_AFTER = 99


def g_off(g):
    return 8 * sum((3, 3, 5, 9)[:g])


def _pool_consts(p):
    W = (2, 4, 8, 16)
    HW = (1, 1, 2, 4)
    pm_s = np.zeros((160, 128, 128), np.float32)
    ic_s = np.zeros((32, 128), np.float32)
    mi = 0
    for g, w in enumerate(W):
        for j in range(8):
            cnt = np.zeros(128, np.float32)
            mats = {}
            for to in range(128):
                r = 16 * p + 2 * j + to // 64
                cc = to % 64
                rlo, rhi = max(r - w // 2, 0), min(r + w // 2, 64)
                clo, chi = max(cc - w // 2, 0), min(cc + w // 2, 64)
                cnt[to] = (rhi - rlo) * (chi - clo)
                for rr in range(rlo, rhi):
                    wt = (rr - (16 * p - 8)) // 2
                    m = mats.setdefault(wt, np.zeros((128, 128), np.float32))
                    ti0 = ((rr - (16 * p - 8)) % 2) * 64
                    m[ti0 + clo:ti0 + chi, to] += 1.0
                wt_self = j + 4
                m = mats.setdefault(wt_self, np.zeros((128, 128), np.float32))
                m[to, to] -= cnt[to]
            for di in range(-HW[g], HW[g] + 1):
                wt = j + 4 + di
                if wt in mats:
                    pm_s[mi] = mats[wt]
                mi += 1
            ic_s[g * 8 + j] = 1.0 / cnt
    assert mi == 160
    pm_p = np.zeros((16, 128, 128), np.float32)
    ic_p = np.zeros((8, 128), np.float32)
    for g, w in enumerate(W):
        for jo in range(2):
            for to in range(128):
                t = jo * 128 + to
                lo, hi = max(t - w // 2, 0), min(t + w // 2, 256)
                ic_p[g * 2 + jo, to] = 1.0 / (hi - lo)
                for tt in range(lo, hi):
                    pm_p[(g * 2 + jo) * 2 + tt // 128, tt % 128, to] += 1.0
                pm_p[(g * 2 + jo) * 2 + jo, to, to] -= (hi - lo)
    return pm_s, ic_s, pm_p, ic_p


def _core_inputs(c, inp):
    s, p = c // 4, c % 4
    f = np.float32
    m = {}
    m["xp"] = np.ascontiguousarray(inp["x_prompt"][4 * c:4 * c + 4].reshape(NP, D))
    m["xs"] = np.ascontiguousarray(inp["x_sample"][s])
    m["sC"] = np.ascontiguousarray(inp["state_mlstm_C"][s, 0].reshape(16, 128, 256))
    m["snl"] = np.ascontiguousarray(inp["state_mlstm_n"][s, 0].reshape(16, 128).T)
    m["sml"] = np.ascontiguousarray(inp["state_mlstm_m"][s, 0].reshape(2, 8, 1))
    c2 = np.stack([inp["c_ctx"], inp["c"][s]], axis=-1).reshape(KD, 128, 2)
    m["c2"] = np.ascontiguousarray(c2.transpose(1, 0, 2))
    m["w_ada"] = inp["w_ada"]
    m["b_ada"] = inp["b_ada"]
    m["norm_mix"] = inp["norm_mix"]
    m["norm_ffn"] = inp["norm_ffn"]
    m["norm_final"] = inp["norm_final"].reshape(1, D)
    m["w_in"] = inp["mlstm_w_in"][0]
    m["bgl"] = np.ascontiguousarray(inp["mlstm_b_gates"][0].reshape(4, 8).T)
    m["head_norm"] = inp["mlstm_head_norm"].reshape(1, D)
    m["w_out"] = inp["mlstm_w_out"][0]
    m["pool_w"] = inp["pool_w"][0]
    m["pool_scale"] = inp["pool_scale"].reshape(1, D)
    m["ffn_w1"] = inp["ffn_w1"][0]
    m["ffn_w3"] = inp["ffn_w3"][0]
    m["ffn_w2"] = inp["ffn_w2"][0]
    m["router"] = inp["moe_router"][0]
    m["moe_w1"] = inp["moe_w1"][0]
    m["moe_w3"] = inp["moe_w3"][0]
    m["moe_w2"] = inp["moe_w2"][0]
    m["identf"] = np.eye(128, dtype=f)
    cm = np.zeros((2, 64, 64), f)
    si, ti = np.meshgrid(np.arange(64), np.arange(64), indexing="ij")
    cm[0][si > ti] = NEG
    cm[1][si < ti] = NEG
    m["cmask"] = np.ascontiguousarray(np.tile(cm, (1, 1, 8)))
    bm = np.zeros((8, 8, 64), f)
    for h in range(8):
        bm[h, h, :] = 1.0
    m["bmask"] = bm.reshape(8, 512)
    iw = np.zeros((16, 128, 1), np.int32)
    for wt in range(16):
        for i in range(128):
            r = 16 * p - 8 + 2 * wt + i // 64
            r = min(max(r, 0), 63)
            iw[wt, i, 0] = NP + r * 64 + i % 64
    m["idx_win"] = iw
    m["idx_xs"] = iw - NP
    pm_s, ic_s, pm_p, ic_p = _pool_consts(p)
    m["pm_s"], m["ic_s"], m["pm_p"], m["ic_p"] = pm_s, ic_s, pm_p, ic_p
    return {k: np.ascontiguousarray(v) for k, v in m.items()}


def kernel(**inputs):
    inp = {k: np.asarray(v) for k, v in inputs.items()}
    nc, es, S = build_program()
    in_maps = [_core_inputs(c, inp) for c in range(8)]
    res = run_bass_kernel_spmd(nc, in_maps, core_ids=list(range(8)))
    r = res.results
    y_prompt = np.concatenate([r[c]["yp"] for c in range(8)], 0).reshape(32, 256, D).astype(np.float32)
    y_sample = np.stack([np.concatenate([r[4 * s + p]["ys"] for p in range(4)], 0) for s in range(2)], 0).astype(np.float32)
    new_C = np.concatenate([r[c]["nC"] for c in range(8)], 0).reshape(32, 1, 2, 8, 128, 256).astype(np.float32)
    new_n = np.concatenate([r[c]["nn"] for c in range(8)], 0).reshape(32, 1, 2, 8, 128).astype(np.float32)
    new_m = np.concatenate([r[c]["nm"] for c in range(8)], 0).reshape(32, 1, 2, 8).astype(np.float32)
    return (y_prompt, y_sample, new_C, new_n, new_m)
```
